# Optimizing a Trainium2 kernel written in Bass

```python
import jax, jax.numpy as jnp
from jax import lax
import numpy as np

D_MODEL = 1024
BATCH = 8
SEQ = 2048
DEPTH = 2

CTX_LEN = 256
GRID_W = 64
D_CONV = 512
HGRN_HEADS = 4
HGRN_HEAD_DIM = 128
D_HGRN = HGRN_HEADS * HGRN_HEAD_DIM
D_MIX = D_CONV + D_HGRN
CONV_KERNEL = 31
CHUNK = 64
N_EXPERTS = 16
N_GROUPS = 4
EXPERTS_PER_GROUP = N_EXPERTS // N_GROUPS
TOP_K = 2
D_EXPERT = 1024
EPS = 1e-6
D_IN = 2 * D_CONV + 5 * D_HGRN
SPLITS = [D_CONV, 2 * D_CONV, 2 * D_CONV + D_HGRN, 2 * D_CONV + 2 * D_HGRN,
          2 * D_CONV + 3 * D_HGRN, 2 * D_CONV + 4 * D_HGRN]

kernel_name = "hybrid_conv_hgrn2_moe_dit"


def rms_norm(x, g):
    xf = x.astype(jnp.float32)
    y = xf * lax.rsqrt(jnp.mean(xf * xf, axis=-1, keepdims=True) + EPS)
    return (y * g.astype(jnp.float32)).astype(x.dtype)


def layer_norm(x, g, b):
    xf = x.astype(jnp.float32)
    mu = jnp.mean(xf, axis=-1, keepdims=True)
    var = jnp.mean(jnp.square(xf - mu), axis=-1, keepdims=True)
    y = (xf - mu) * lax.rsqrt(var + EPS)
    return (y * g.astype(jnp.float32) + b.astype(jnp.float32)).astype(x.dtype)


def modulate(x, g, shift, scale):
    return rms_norm(x, g) * (1 + scale) + shift


def depthwise_conv(u, w, b):
    y = lax.conv_general_dilated(
        u, w[:, None, :].astype(u.dtype), window_strides=(1,),
        padding=[(CONV_KERNEL // 2, CONV_KERNEL // 2)],
        dimension_numbers=('NWC', 'WIO', 'NWC'), feature_group_count=u.shape[-1])
    return y + b.astype(u.dtype)


def conformer_conv(a, gate, w, b, ln_g, ln_b, orient):
    u = a * jax.nn.sigmoid(gate)
    bn, L, C = u.shape
    if orient is None:
        y = depthwise_conv(u, w, b)
    else:
        rows = L // GRID_W
        grid = u.reshape(bn, rows, GRID_W, C)
        if orient == 'col':
            grid = grid.transpose(0, 2, 1, 3)
        n1, n2 = grid.shape[1], grid.shape[2]
        y = depthwise_conv(grid.reshape(bn * n1, n2, C), w, b).reshape(bn, n1, n2, C)
        if orient == 'col':
            y = y.transpose(0, 2, 1, 3)
        y = y.reshape(bn, L, C)
    return jax.nn.silu(layer_norm(y, ln_g, ln_b))


def split_heads(t):
    bn, L, _ = t.shape
    return t.reshape(bn, L, HGRN_HEADS, HGRN_HEAD_DIM).transpose(0, 2, 1, 3)


def flip(t):
    return t[:, :, ::-1]


def lower_bounds(lb_param):
    p = jax.nn.softmax(lb_param.astype(jnp.float32), axis=0)
    return jnp.cumsum(p, axis=0) - p[0]


def forget_gate(f_raw, lb):
    f = lb + (1.0 - lb) * jax.nn.sigmoid(split_heads(f_raw).astype(jnp.float32))
    return jnp.log(f), 1.0 - f


def chunk_scan(q, k, v, logf, s0):
    q, k, v, logf = (t.astype(jnp.float32) for t in (q, k, v, logf))
    bn, h, L, dk = q.shape
    n = L // CHUNK

    def to_chunks(t):
        return jnp.moveaxis(t.reshape(bn, h, n, CHUNK, t.shape[-1]), 2, 0)

    tri = jnp.tril(jnp.ones((CHUNK, CHUNK), dtype=bool))[:, :, None]

    def step(S, inp):
        qc, kc, vc, gc = inp
        b = jnp.cumsum(gc, axis=2)
        o_inter = jnp.einsum('bhtd,bhdv->bhtv', qc * jnp.exp(b), S)
        diff = b[:, :, :, None, :] - b[:, :, None, :, :]
        decay = jnp.exp(jnp.where(tri, diff, -jnp.inf))
        att = jnp.einsum('bhtsd,bhsd->bhts', qc[:, :, :, None, :] * decay, kc)
        o = o_inter + jnp.einsum('bhts,bhsv->bhtv', att, vc)
        b_last = b[:, :, -1:, :]
        S = jnp.exp(b_last[:, :, 0, :, None]) * S + jnp.einsum(
            'bhsd,bhsv->bhdv', kc * jnp.exp(b_last - b), vc)
        return S, o

    S, o = lax.scan(step, s0, (to_chunks(q), to_chunks(k), to_chunks(v), to_chunks(logf)))
    o = jnp.moveaxis(o, 0, 2).reshape(bn, h, L, v.shape[-1])
    return o, S


def final_state(k, v, logf):
    b = jnp.cumsum(logf.astype(jnp.float32), axis=2)
    kd = k.astype(jnp.float32) * jnp.exp(b[:, :, -1:, :] - b)
    return jnp.einsum('bhsd,bhsv->bhdv', kd, v.astype(jnp.float32))


def hgrn_readout(o, og, norm_g, dtype):
    y = rms_norm(o, norm_g) * jax.nn.silu(split_heads(og).astype(jnp.float32))
    bn, h, L, dh = y.shape
    return y.transpose(0, 2, 1, 3).reshape(bn, L, h * dh).astype(dtype)


def moe(h, router_w, router_bias, w_gate, w_up, w_down):
    s = jax.nn.sigmoid(h.astype(jnp.float32) @ router_w.astype(jnp.float32))
    sb = s + router_bias.astype(jnp.float32)
    grp = sb.reshape(-1, N_GROUPS, EXPERTS_PER_GROUP)
    group_score = lax.top_k(grp, TOP_K)[0].sum(-1)
    best = jnp.argmax(group_score, axis=-1)
    in_group = (jnp.arange(N_EXPERTS) // EXPERTS_PER_GROUP)[None, :] == best[:, None]
    _, idx = lax.top_k(jnp.where(in_group, sb, -jnp.inf), TOP_K)
    w = jnp.take_along_axis(s, idx, axis=-1)
    w = w / jnp.sum(w, axis=-1, keepdims=True)
    combine = jnp.sum(jax.nn.one_hot(idx, N_EXPERTS, dtype=jnp.float32) * w[..., None], axis=1)
    combine = combine.astype(h.dtype)
    out = jnp.zeros_like(h)
    for e in range(N_EXPERTS):
        y = (jax.nn.silu(h @ w_gate[e]) * (h @ w_up[e])) @ w_down[e]
        out = out + combine[:, e:e + 1] * y
    return out


def setup_inputs(seed: int = 0) -> dict:
    key = jax.random.key(seed)
    ks = jax.random.split(key, 24)
    f32 = jnp.float32

    def nrm(k, shape, scale):
        return jax.random.normal(k, shape, f32) * scale

    return {
        "x": nrm(ks[0], (BATCH, SEQ, D_MODEL), 1.0),
        "c": nrm(ks[1], (BATCH, D_MODEL), 1.0),
        "ctx": nrm(ks[2], (BATCH, CTX_LEN, D_MODEL), 1.0),
        "c_ctx": nrm(ks[3], (D_MODEL,), 1.0),
        "w_ada": nrm(ks[4], (DEPTH, D_MODEL, 6 * D_MODEL), 0.5 * D_MODEL ** -0.5),
        "b_ada": nrm(ks[5], (DEPTH, 6 * D_MODEL), 0.02),
        "norm1_g": 1.0 + nrm(ks[6], (DEPTH, D_MODEL), 0.05),
        "norm2_g": 1.0 + nrm(ks[7], (DEPTH, D_MODEL), 0.05),
        "w_in": nrm(ks[8], (DEPTH, D_MODEL, D_IN), D_MODEL ** -0.5),
        "conv_w": nrm(ks[9], (DEPTH, CONV_KERNEL, D_CONV), CONV_KERNEL ** -0.5),
        "conv_b": nrm(ks[10], (DEPTH, D_CONV), 0.02),
        "conv_ln_g": 1.0 + nrm(ks[11], (DEPTH, D_CONV), 0.05),
        "conv_ln_b": nrm(ks[12], (DEPTH, D_CONV), 0.02),
        "lb_fwd": nrm(ks[13], (DEPTH, D_HGRN), 0.5),
        "lb_bwd": nrm(ks[14], (DEPTH, D_HGRN), 0.5),
        "hgrn_norm_g": 1.0 + nrm(ks[15], (DEPTH, HGRN_HEAD_DIM), 0.05),
        "w_out": nrm(ks[16], (DEPTH, D_MIX, D_MODEL), D_MIX ** -0.5),
        "router_w": nrm(ks[17], (D_MODEL, N_EXPERTS), D_MODEL ** -0.5),
        "router_bias": nrm(ks[18], (N_EXPERTS,), 0.01),
        "w_gate": nrm(ks[19], (DEPTH, N_EXPERTS, D_MODEL, D_EXPERT), D_MODEL ** -0.5),
        "w_up": nrm(ks[20], (DEPTH, N_EXPERTS, D_MODEL, D_EXPERT), D_MODEL ** -0.5),
        "w_down": nrm(ks[21], (DEPTH, N_EXPERTS, D_EXPERT, D_MODEL), D_EXPERT ** -0.5),
        "final_norm_g": 1.0 + nrm(ks[22], (D_MODEL,), 0.05),
    }


def reference(x, c, ctx, c_ctx, w_ada, b_ada, norm1_g, norm2_g, w_in, conv_w, conv_b,
              conv_ln_g, conv_ln_b, lb_fwd, lb_bwd, hgrn_norm_g, w_out, router_w,
              router_bias, w_gate, w_up, w_down, final_norm_g):
    bn, L, D = x.shape
    lbs_f = lower_bounds(lb_fwd)
    lbs_b = lower_bounds(lb_bwd)
    s_zero = jnp.zeros((bn, HGRN_HEADS, HGRN_HEAD_DIM, HGRN_HEAD_DIM), jnp.float32)

    for l in range(DEPTH):
        last = l == DEPTH - 1
        lb_f = lbs_f[l].reshape(HGRN_HEADS, 1, HGRN_HEAD_DIM)
        lb_b = lbs_b[l].reshape(HGRN_HEADS, 1, HGRN_HEAD_DIM)

        mod_x = jax.nn.silu(c) @ w_ada[l] + b_ada[l]
        mod_c = jax.nn.silu(c_ctx) @ w_ada[l] + b_ada[l]
        sh1, sc1, g1, sh2, sc2, g2 = jnp.split(mod_x[:, None, :], 6, axis=-1)
        sh1c, sc1c, g1c, sh2c, sc2c, g2c = jnp.split(mod_c, 6, axis=-1)

        hx = modulate(x, norm1_g[l], sh1, sc1)
        hc = modulate(ctx, norm1_g[l], sh1c, sc1c)
        ax, gx, qx, ix, ffx, fbx, ogx = jnp.split(hx @ w_in[l], SPLITS, axis=-1)

        if last:
            ic, ffc, fbc = jnp.split(hc @ w_in[l][:, SPLITS[2]:SPLITS[5]], 3, axis=-1)
            i_c = split_heads(ic)
            lf_cf, k_cf = forget_gate(ffc, lb_f)
            lf_cb, k_cb = forget_gate(fbc, lb_b)
            S_f = final_state(k_cf, i_c, lf_cf)
            S_b = final_state(flip(k_cb), flip(i_c), flip(lf_cb))
        else:
            ac, gc, qc, ic, ffc, fbc, ogc = jnp.split(hc @ w_in[l], SPLITS, axis=-1)
            q_c = split_heads(jax.nn.silu(qc))
            i_c = split_heads(ic)
            lf_cf, k_cf = forget_gate(ffc, lb_f)
            lf_cb, k_cb = forget_gate(fbc, lb_b)
            o_cf, S_f = chunk_scan(q_c, k_cf, i_c, lf_cf, s_zero)
            o_cb, S_b = chunk_scan(flip(q_c), flip(k_cb), flip(i_c), flip(lf_cb), s_zero)
            y_hc = hgrn_readout(o_cf + flip(o_cb), ogc, hgrn_norm_g[l], ctx.dtype)
            y_cc = conformer_conv(ac, gc, conv_w[l], conv_b[l], conv_ln_g[l], conv_ln_b[l], None)
            ctx = ctx + g1c * (jnp.concatenate([y_cc, y_hc], axis=-1) @ w_out[l])

        q_x = split_heads(jax.nn.silu(qx))
        i_x = split_heads(ix)
        lf_xf, k_xf = forget_gate(ffx, lb_f)
        lf_xb, k_xb = forget_gate(fbx, lb_b)
        o_xf, _ = chunk_scan(q_x, k_xf, i_x, lf_xf, S_f)
        o_xb, _ = chunk_scan(flip(q_x), flip(k_xb), flip(i_x), flip(lf_xb), S_b)
        y_hx = hgrn_readout(o_xf + flip(o_xb), ogx, hgrn_norm_g[l], x.dtype)

        orient = 'row' if l % 2 == 0 else 'col'
        y_cx = conformer_conv(ax, gx, conv_w[l], conv_b[l], conv_ln_g[l], conv_ln_b[l], orient)
        x = x + g1 * (jnp.concatenate([y_cx, y_hx], axis=-1) @ w_out[l])

        hx2 = modulate(x, norm2_g[l], sh2, sc2).reshape(bn * L, D)
        if last:
            out = moe(hx2, router_w, router_bias, w_gate[l], w_up[l], w_down[l])
            x = x + g2 * out.reshape(bn, L, D)
        else:
            hc2 = modulate(ctx, norm2_g[l], sh2c, sc2c).reshape(bn * CTX_LEN, D)
            out = moe(jnp.concatenate([hx2, hc2], axis=0), router_w, router_bias,
                      w_gate[l], w_up[l], w_down[l])
            x = x + g2 * out[:bn * L].reshape(bn, L, D)
            ctx = ctx + g2c * out[bn * L:].reshape(bn, CTX_LEN, D)

    return rms_norm(x, final_norm_g)
```

```python
import numpy as np
from contextlib import ExitStack, suppress
import concourse.bass as bass
import concourse.mybir as mybir
from concourse.bass_utils import run_bass_kernel_spmd

F32, BF16 = mybir.dt.float32, mybir.dt.bfloat16
AF = mybir.ActivationFunctionType
ALU = mybir.AluOpType
AX = mybir.AxisListType

D = 1024
NX = 2048
NCX = 256
NT = NX + NCX
DEPTH = 2
NE = 16
EPS = 1e-6
CH = 32
NCHK = NT // CH
TT = [(0, 512), (512, 512), (1024, 512), (1536, 512), (2048, 256)]
BIG = 1.0e4
MOE_PIPE = True

VOFF = {}
_o = 0
for _n, _w in (("n1g", 16), ("n2g", 16), ("fing", 8), ("convb", 8), ("lng", 8), ("lnb", 8),
               ("hg", 2), ("lbf", 8), ("lbb", 8), ("convw", 2 * 4 * 31), ("rbias", 16)):
    VOFF[_n] = _o
    _o += _w
NV = _o
COFF = {"ident": 0, "maskF": 128, "maskB": 256, "smask": 384}
NCST = 384 + 512 + 4


class Res:
    __slots__ = ("w", "r")

    def __init__(self):
        self.w = None
        self.r = {}


class Stream:
    def __init__(self, eng, sem, key):
        self.eng, self.sem, self.key = eng, sem, key
        self.count = 0
        self.waited = {}


class DSem:
    def __init__(self, handle, key):
        self.handle, self.key, self.count = handle, key, 0


class Sched:
    def __init__(self, nc, es):
        self.nc, self.es = nc, es
        self.semh = {}
        self.streams = {}
        for name, eng in (("pe", nc.tensor), ("act", nc.scalar), ("dve", nc.vector),
                          ("pool", nc.gpsimd), ("sp", nc.sync)):
            h = es.enter_context(nc.semaphore("s_" + name))
            self.semh[name] = h
            self.streams[name] = Stream(eng, h, name)
        self.dsems = []

    def res(self):
        return Res()

    def grid(self, *dims):
        if len(dims) == 1:
            return [Res() for _ in range(dims[0])]
        return [self.grid(*dims[1:]) for _ in range(dims[0])]

    def dsem(self):
        key = f"d{len(self.dsems)}"
        h = self.es.enter_context(self.nc.semaphore("s_" + key))
        self.semh[key] = h
        d = DSem(h, key)
        self.dsems.append(d)
        return d

    def _deps(self, st, reads, writes):
        deps = {}

        def add(tok, same_ok):
            if tok is None:
                return
            k, v = tok
            if k == st.key and not same_ok:
                return
            if deps.get(k, 0) < v:
                deps[k] = v
        for r in reads:
            add(r.w, True)
        for w in writes:
            add(w.w, False)
            for k, v in w.r.items():
                add((k, v), False)
        for k, v in deps.items():
            if st.waited.get(k, 0) < v:
                st.waited[k] = v
                st.eng.wait_ge(self.semh[k], v)

    def _mark(self, key, val, reads, writes):
        for r in reads:
            r.r[key] = val
        for w in writes:
            w.w = (key, val)
            w.r = {}

    def op(self, sname, fn, reads=(), writes=()):
        st = self.streams[sname]
        self._deps(st, reads, writes)
        st.count += 1
        fn().then_inc(st.sem, 1)
        self._mark(st.key, st.count, reads, writes)

    def group(self, sname, fns, reads=(), writes=()):
        st = self.streams[sname]
        self._deps(st, reads, writes)
        for fn in fns[:-1]:
            fn()
        st.count += 1
        fns[-1]().then_inc(st.sem, 1)
        self._mark(st.key, st.count, reads, writes)

    def dma(self, sname, out, in_, ds, reads=(), writes=()):
        st = self.streams[sname]
        self._deps(st, reads, writes)
        ds.count += 16
        st.eng.dma_start(out=out, in_=in_).then_inc(ds.handle, 16)
        self._mark(ds.key, ds.count, reads, writes)

    def barrier(self):
        for st in self.streams.values():
            for o in self.streams.values():
                if o is not st and o.count > st.waited.get(o.key, 0):
                    st.waited[o.key] = o.count
                    st.eng.wait_ge(o.sem, o.count)
            for d in self.dsems:
                if d.count > st.waited.get(d.key, 0):
                    st.waited[d.key] = d.count
                    st.eng.wait_ge(d.handle, d.count)


class _Stop(Exception):
    pass


def build_program(nlayers=DEPTH, dbg=None, stop=None):
    dbg = dbg or {}
    nc = bass.Bass("TRN2", target_bir_lowering=False)
    dr = lambda n, s, k="ExternalInput": nc.dram_tensor(n, s, F32, kind=k).ap()
    x_d = dr("x", [NX, D])
    ctx_d = dr("ctx", [NCX, D])
    c2_d = dr("c2", [128, 16])
    wada_d = dr("w_ada", [DEPTH, D, 6 * D])
    bada_d = dr("b_ada_r", [DEPTH, 128, 48])
    vecs_d = dr("vecs", [128, NV])
    cst_d = dr("consts", [128, NCST])
    win_d = dr("w_in", [DEPTH, D, 3584])
    wout_d = dr("w_out", [DEPTH, D, D])
    rw_d = dr("router_w_r", [128, 8 * NE])
    wg_d = dr("w_gate", [DEPTH, NE, D, D])
    wu_d = dr("w_up", [DEPTH, NE, D, D])
    wd_d = dr("w_down", [DEPTH, NE, D, D])
    y_d = dr("y", [NX, D], "ExternalOutput")
    dbg_d = {k: nc.dram_tensor("dbg_" + k, list(s[0]), BF16 if s[1] == "bf16" else F32, kind="ExternalOutput").ap()
             for k, s in dbg.items()}

    es = ExitStack()
    with suppress(_Stop), es:
        S = Sched(nc, es)
        V, A, G, T = nc.vector, nc.scalar, nc.gpsimd, nc.tensor

        nsb = [0]

        def sbuf(es_, name, shape, dt):
            nsb[0] += 1
            return es_.enter_context(nc.sbuf_tensor(f"sb{nsb[0]}_{name}", shape, dt))

        xT = sbuf(es, "xT", [128, 8, NT], F32)
        hT = sbuf(es, "hT", [128, 8, NT], BF16)
        cst = sbuf(es, "cst", [128, NCST], F32)
        vecs = sbuf(es, "vecs", [128, NV], F32)
        identb = sbuf(es, "identb", [128, 128], BF16)
        onesb = sbuf(es, "onesb", [128, 128], BF16)
        zerob = sbuf(es, "zerob", [128, 128], BF16)
        epsD = sbuf(es, "epsD", [128, 3], F32)
        mod = sbuf(es, "mod", [128, 48, 2], F32)
        se = sbuf(es, "se", [128, 2, 8, 2], F32)
        lbt = sbuf(es, "lbt", [128, 2, 4, 3], F32)
        rw = sbuf(es, "rw", [128, 8, NE], F32)
        P = [es.enter_context(nc.psum_tensor(f"ps{i}", [128, 512], F32)) for i in range(8)]
        rP = S.grid(8)
        rxT = S.grid(8, 5)
        rhT = S.grid(5)
        rcst, rvecs, rmisc, rmod, rse, rlbt, rrw = (S.res() for _ in range(7))
        ident = cst[:, 0:128]
        maskF = cst[:, 128:256]
        maskB = cst[:, 256:384]
        smask = cst[:, 384:896]
        dconst = S.dsem()
        vv = lambda n, i: vecs[:, VOFF[n] + i: VOFF[n] + i + 1]

        S.dma("sp", cst[:], cst_d[:, :], S.dsem(), writes=[rcst])
        S.dma("sp", vecs[:], vecs_d[:, :], S.dsem(), writes=[rvecs])
        S.dma("sp", rw[:].rearrange("p a b -> p (a b)"), rw_d[:, :], S.dsem(), writes=[rrw])
        S.op("dve", lambda: V.memset(onesb[:], 1.0), writes=[rmisc])
        S.op("dve", lambda: V.memset(zerob[:], 0.0), writes=[rmisc])
        S.op("dve", lambda: V.memset(epsD[:, 0:1], float(D * EPS)), writes=[rmisc])
        S.op("dve", lambda: V.memset(epsD[:, 1:3], float(EPS)), writes=[rmisc])
        S.op("dve", lambda: V.tensor_copy(identb[:], cst[:, 0:128]), reads=[rcst], writes=[rmisc])

        pctr = [0]

        def nextp(lo, hi):
            p = lo + pctr[0] % (hi - lo)
            pctr[0] += 1
            return p

        def dump(name, ap_sb, res_list):
            if name in dbg_d:
                ds = S.dsem()
                S.dma("sp", dbg_d[name], ap_sb, ds, reads=res_list)

        with ExitStack() as ea:
            xin = [sbuf(ea, f"xin{i}", [128, D], F32) for i in range(2)]
            rxin = S.grid(2)
            dxin = [S.dsem(), S.dsem()]
            for tt in range(18):
                b = tt % 2
                src = x_d[tt * 128:(tt + 1) * 128, :] if tt < 16 else ctx_d[(tt - 16) * 128:(tt - 15) * 128, :]
                S.dma("sp", xin[b][:], src, dxin[b], writes=[rxin[b]])
                for half in range(2):
                    p = nextp(0, 4)
                    fns = [(lambda c=c, p=p, b=b: T.transpose(P[p][:, (c % 4) * 128:(c % 4 + 1) * 128],
                                                              xin[b][:, c * 128:(c + 1) * 128], ident))
                           for c in range(half * 4, half * 4 + 4)]
                    S.group("pe", fns, reads=[rxin[b], rcst], writes=[rP[p]])
                    ws = [rxT[c][tt // 4] for c in range(half * 4, half * 4 + 4)]
                    eng = "dve" if half == 0 else "act"
                    dst = xT[:, half * 4:half * 4 + 4, tt * 128:(tt + 1) * 128]
                    srcp = P[p][:].rearrange("p (c t) -> p c t", c=4)
                    if eng == "dve":
                        S.op("dve", lambda dst=dst, srcp=srcp: V.tensor_copy(dst, srcp), reads=[rP[p]], writes=ws)
                    else:
                        S.op("act", lambda dst=dst, srcp=srcp: A.copy(dst, srcp), reads=[rP[p]], writes=ws)
            S.barrier()

        allx = lambda ti: [rxT[c][ti] for c in range(8)]

        def norm_mod(nidx, tiles, h2f=None, rh2f=None, es_=None):
            sq = sbuf(es_, f"sq{nidx}", [128, 8, 512], BF16)
            rstd = sbuf(es_, f"rstd{nidx}", [128, 512], F32)
            tmp = [sbuf(es_, f"nt{nidx}_{i}", [128, 512], F32) for i in range(2)]
            rsq, rrstd = S.res(), S.res()
            rtmp = S.grid(2)
            shb = 0 if nidx == 0 else 24
            for ti, (t0, n) in enumerate(tiles):
                col = 0 if t0 < NX else 1
                for c in range(8):
                    S.op("act", lambda c=c: A.activation(sq[:, c, :n], xT[:, c, t0:t0 + n], AF.Square),
                         reads=[rxT[c][ti]], writes=[rsq])
                p = nextp(0, 4)
                S.group("pe", [(lambda c=c, p=p: T.matmul(P[p][:, :n], onesb[:], sq[:, c, :n], start=(c == 0), stop=(c == 7)))
                               for c in range(8)], reads=[rsq, rmisc], writes=[rP[p]])
                S.op("act", lambda p=p: A.activation(rstd[:, :n], P[p][:, :n], AF.Ln, bias=epsD[:, 0:1], scale=1.0),
                     reads=[rP[p], rmisc], writes=[rrstd])
                S.op("act", lambda: A.activation(rstd[:, :n], rstd[:, :n], AF.Exp, scale=-0.5), reads=[rrstd], writes=[rrstd])
                for c in range(8):
                    b = c % 2
                    S.op("dve", lambda c=c, b=b: V.tensor_tensor(tmp[b][:, :n], xT[:, c, t0:t0 + n], rstd[:, :n], ALU.mult),
                         reads=[rxT[c][ti], rrstd], writes=[rtmp[b]])
                    sc_ap = se[:, nidx, c, col:col + 1]
                    sh_ap = mod[:, shb + c, col:col + 1]
                    if h2f is None:
                        S.op("act", lambda c=c, b=b, sc_ap=sc_ap, sh_ap=sh_ap: A.activation(
                            hT[:, c, t0:t0 + n], tmp[b][:, :n], AF.Identity, bias=sh_ap, scale=sc_ap),
                            reads=[rtmp[b], rse, rmod], writes=[rhT[ti]])
                    else:
                        S.op("act", lambda c=c, b=b, sc_ap=sc_ap, sh_ap=sh_ap: A.activation(
                            h2f[:, c, t0:t0 + n], tmp[b][:, :n], AF.Identity, bias=sh_ap, scale=sc_ap),
                            reads=[rtmp[b], rse, rmod], writes=[rh2f[ti]])
                        S.op("pool", lambda c=c: G.tensor_copy(hT[:, c, t0:t0 + n], h2f[:, c, t0:t0 + n]),
                             reads=[rh2f[ti]], writes=[rhT[ti]])

        class WChunks:
            def __init__(self, es_, tag, srcs, ceng, nst=3, nbf=3):
                self.srcs, self.ceng = srcs, ceng
                self.st = [sbuf(es_, f"wst{tag}{i}", [128, 8, 128], F32) for i in range(nst)]
                self.bf = [sbuf(es_, f"wbf{tag}{i}", [128, 8, 128], BF16) for i in range(nbf)]
                self.rst, self.rbf = S.grid(nst), S.grid(nbf)
                self.dst = [S.dsem() for _ in range(nst)]
                self.nl = 0
                self.nc_ = 0
                self.look = nbf - 1

            def _load(self):
                if self.nl >= len(self.srcs):
                    return
                i = self.nl % len(self.st)
                S.dma("sp", self.st[i][:], self.srcs[self.nl], self.dst[i], writes=[self.rst[i]])
                self.nl += 1

            def _cast(self):
                if self.nc_ >= len(self.srcs):
                    return
                i = self.nc_ % len(self.st)
                j = self.nc_ % len(self.bf)
                src, dst = self.st[i], self.bf[j]
                if self.ceng == "pool":
                    S.op("pool", lambda: G.tensor_copy(dst[:], src[:]), reads=[self.rst[i]], writes=[self.rbf[j]])
                else:
                    S.op("act", lambda: A.copy(dst[:], src[:]), reads=[self.rst[i]], writes=[self.rbf[j]])
                self.nc_ += 1

            def prefetch(self):
                for _ in range(len(self.st)):
                    self._load()
                for _ in range(self.look):
                    self._cast()
                    self._load()

            def get(self, k):
                while self.nc_ <= k:
                    self._cast()
                    self._load()
                j = k % len(self.bf)
                return self.bf[j], self.rbf[j]

            def after(self, k):
                while self.nc_ <= k + self.look and self.nc_ < len(self.srcs):
                    self._cast()
                    self._load()

        def proj_fm(wt, rwt, tiles, consume, plo=0, phi=4):
            for ti, (t0, n) in enumerate(tiles):
                p = nextp(plo, phi)
                S.group("pe", [(lambda kc=kc, p=p: T.matmul(P[p][:, :n], wt[:, kc, :], hT[:, kc, t0:t0 + n],
                                                            start=(kc == 0), stop=(kc == 7))) for kc in range(8)],
                        reads=[rwt, rhT[ti]], writes=[rP[p]])
                consume(p, ti, t0, n)

        def ckpt(name):
            if stop == name:
                S.barrier()
                raise _Stop()

        for l in ([] if stop == "load" else range(nlayers)):
            last = (l == DEPTH - 1)
            TO = TT[:4] if last else TT
            with ExitStack() as eb:
                c2 = sbuf(eb, "c2", [128, 8, 2], F32)
                sc2 = sbuf(eb, "sc2", [128, 8, 2], F32)
                bada = sbuf(eb, "bada", [128, 48], F32)
                wa = [sbuf(eb, f"wa{i}", [128, 8, 512], F32) for i in range(2)]
                rc2, rsc2, rbada = S.res(), S.res(), S.res()
                rwa = S.grid(2)
                dwa = [S.dsem(), S.dsem()]
                S.dma("sp", c2[:].rearrange("p a b -> p (a b)"), c2_d[:, :], S.dsem(), writes=[rc2])
                S.dma("sp", bada[:], bada_d[l, :, :], S.dsem(), writes=[rbada])
                S.op("act", lambda: A.activation(sc2[:], c2[:], AF.Silu), reads=[rc2], writes=[rsc2])
                pm = 7
                for g in range(12):
                    b = g % 2
                    S.dma("sp", wa[b][:], wada_d[l, :, g * 512:(g + 1) * 512].rearrange("(kc p) f -> p kc f", p=128),
                          dwa[b], writes=[rwa[b]])
                    fns = []
                    for j in range(4):
                        fc = g * 4 + j
                        for kc in range(8):
                            fns.append(lambda j=j, fc=fc, kc=kc, b=b: T.matmul(
                                P[pm][:, fc * 2:fc * 2 + 2], wa[b][:, kc, j * 128:(j + 1) * 128], sc2[:, kc, :],
                                start=(kc == 0), stop=(kc == 7)))
                    S.group("pe", fns, reads=[rwa[b], rsc2], writes=[rP[pm]])
                S.op("dve", lambda: V.tensor_tensor(mod[:], P[pm][:, 0:96].rearrange("p (a b) -> p a b", b=2),
                                                    bada[:].unsqueeze(2).to_broadcast([128, 48, 2]), ALU.add),
                     reads=[rP[pm], rbada], writes=[rmod])
                for nidx, (scb, gname) in enumerate(((8, "n1g"), (32, "n2g"))):
                    S.op("dve", lambda nidx=nidx, scb=scb: V.tensor_scalar(
                        se[:, nidx, :, :], mod[:, scb:scb + 8, :], 1.0, float(np.sqrt(D)), ALU.add, ALU.mult),
                        reads=[rmod], writes=[rse])
                    gap = vecs[:, VOFF[gname] + l * 8: VOFF[gname] + l * 8 + 8].unsqueeze(2).to_broadcast([128, 8, 2])
                    S.op("dve", lambda nidx=nidx, gap=gap: V.tensor_tensor(se[:, nidx, :, :], se[:, nidx, :, :], gap, ALU.mult),
                         reads=[rse, rvecs], writes=[rse])
                for di, nm in enumerate(("lbf", "lbb")):
                    l0 = vecs[:, VOFF[nm]:VOFF[nm] + 4]
                    l1 = vecs[:, VOFF[nm] + 4:VOFF[nm] + 8]
                    if l == 0:
                        S.op("dve", lambda di=di: V.memset(lbt[:, di, :, 0], 0.0), writes=[rlbt])
                    else:
                        S.op("dve", lambda di=di, l0=l0, l1=l1: V.tensor_tensor(lbt[:, di, :, 0], l1, l0, ALU.subtract),
                             reads=[rvecs], writes=[rlbt])
                        S.op("act", lambda di=di: A.activation(lbt[:, di, :, 0], lbt[:, di, :, 0], AF.Sigmoid),
                             reads=[rlbt], writes=[rlbt])
                    S.op("dve", lambda di=di: V.tensor_scalar(lbt[:, di, :, 1], lbt[:, di, :, 0], -1.0, 1.0, ALU.mult, ALU.add),
                         reads=[rlbt], writes=[rlbt])
                    S.op("dve", lambda di=di: V.tensor_scalar(lbt[:, di, :, 2], lbt[:, di, :, 0], 1.0, -1.0, ALU.mult, ALU.add),
                         reads=[rlbt], writes=[rlbt])
                S.barrier()
            dump(f"mod{l}", mod[:].rearrange("p a b -> p (a b)"), [rmod])

            with ExitStack() as em:
                rym = S.grid(8, 5)
                with ExitStack() as en:
                    norm_mod(0, TT, es_=en)
                    S.barrier()
                dump(f"h{l}", hT[:].rearrange("p a b -> p (a b)"), rhT)
                if stop == "norm1":
                    break
                srcs = []
                wsl = lambda grp, sub: win_d[l, :, grp * 512 + sub * 128: grp * 512 + (sub + 1) * 128].rearrange(
                    "(kc p) f -> p kc f", p=128)
                wrow = lambda r0: wout_d[l, r0:r0 + 128, :].rearrange("p (dj f) -> p dj f", f=128)
                for h in range(4):
                    srcs += [wsl(3, h), wsl(4, h), wsl(2, h), wsl(5, h), wsl(2, h), wsl(6, h), wrow(512 + h * 128)]
                for j in range(4):
                    srcs += [wsl(0, j), wsl(1, j)]
                W = WChunks(em, "m", srcs, "pool", nst=2, nbf=3)
                W.prefetch()
                wk = [0]

                def nextw():
                    k = wk[0]
                    wk[0] += 1
                    t_, r_ = W.get(k)
                    return k, t_, r_

                with ExitStack() as eh:
                    o_acc = sbuf(eh, "o_acc", [128, NT], F32)
                    Vh = sbuf(eh, "Vh", [128, 18, 128], BF16)
                    Vx = [sbuf(eh, f"Vx{i}", [128, 4, 128], BF16) for i in range(2)]
                    yh = sbuf(eh, "yh", [128, NT], BF16)
                    QT = sbuf(eh, "QT", [128, NT], BF16)
                    KT = sbuf(eh, "KT", [128, NT], BF16)
                    KHT = sbuf(eh, "KHT", [128, 18, 128], BF16)
                    dS = sbuf(eh, "dS", [128, NCHK, 128], BF16)
                    Dall = sbuf(eh, "Dall", [128, NCHK], F32)
                    Dpos = sbuf(eh, "Dpos", [128, NCHK], F32)
                    Wt = [[sbuf(eh, f"W{i}_{b_}", [128, 512], F32) for i in range(6)] for b_ in range(2)]
                    rWt = [[S.res() for i in range(6)] for b_ in range(2)]
                    W1 = Wt[0][0]
                    KHt = sbuf(eh, "KHt", [128, 512], BF16)
                    attm = [sbuf(eh, f"attm{i}", [128, 128], BF16) for i in range(2)]
                    ro_acc = S.grid(5)
                    rVh, rKHT, rdS, rDall, rDpos = (S.res() for _ in range(5))
                    rdSh = S.grid(2)
                    rQT, rKT, ryh = S.grid(5), S.grid(5), S.grid(5)
                    rKHt = S.res()
                    rW1 = rWt[0][0]
                    Wq, rWq = Wt[0][4], rWt[0][4]
                    sqh, rsqh, ro, rro = KHt, rKHt, W1, rW1
                    rattm, rVx = S.grid(2), S.grid(2)
                    if l == 0:
                        print("[kernel] SBUF bytes free inside HGRN scope:", nc.sbuf_bytes_remaining)
                    NXC = NX // CH
                    NCC = NCX // CH
                    CPT = 128 // CH
                    for h in range(4):
                        k, wt, rwt = nextw()
                        for g4 in range(5):
                            tts = range(g4 * 4, min(g4 * 4 + 4, 18))
                            p = nextp(0, 4)
                            fns = []
                            for tt in tts:
                                for kc in range(8):
                                    fns.append(lambda tt=tt, kc=kc, p=p: T.matmul(
                                        P[p][:, (tt % 4) * 128:(tt % 4 + 1) * 128], hT[:, kc, tt * 128:(tt + 1) * 128],
                                        wt[:, kc, :], start=(kc == 0), stop=(kc == 7)))
                            S.group("pe", fns, reads=[rwt, rhT[g4]], writes=[rP[p]])
                            nt_ = len(tts)
                            S.op("act", lambda p=p, g4=g4, nt_=nt_: A.copy(
                                Vh[:, g4 * 4:g4 * 4 + nt_, :], P[p][:, :nt_ * 128].rearrange("p (a b) -> p a b", b=128)),
                                reads=[rP[p]], writes=[rVh])
                        W.after(k)
                        ckpt("hg_v")
                        for di in range(2):
                            fwd = di == 0
                            kf, wf, rwf = nextw()
                            kq, wq, rwq = nextw()
                            lb_ap = lbt[:, di, h, 0:1]
                            oml_ap = lbt[:, di, h, 1:2]
                            noml_ap = lbt[:, di, h, 2:3]
                            def prep_a(ti):
                                t0, n = TT[ti]
                                nch, c0 = n // CH, t0 // CH
                                W1, W2, W3, W4, Wq, W5 = Wt[ti % 2]
                                rW1, rW2, rW3, rW4, rWq, rW5 = rWt[ti % 2]
                                v3 = lambda ap: ap[:, :n].rearrange("p (c k) -> p c k", k=CH)
                                pf = nextp(0, 4)
                                S.group("pe", [(lambda kc=kc, pf=pf: T.matmul(P[pf][:, :n], wf[:, kc, :], hT[:, kc, t0:t0 + n],
                                                                             start=(kc == 0), stop=(kc == 7))) for kc in range(8)],
                                        reads=[rwf, rhT[ti]], writes=[rP[pf]])
                                pq = nextp(0, 4)
                                S.group("pe", [(lambda kc=kc, pq=pq: T.matmul(P[pq][:, :n], wq[:, kc, :], hT[:, kc, t0:t0 + n],
                                                                             start=(kc == 0), stop=(kc == 7))) for kc in range(8)],
                                        reads=[rwq, rhT[ti]], writes=[rP[pq]])
                                S.op("act", lambda: A.activation(W1[:, :n], P[pf][:, :n], AF.Sigmoid), reads=[rP[pf]], writes=[rW1])
                                S.op("act", lambda: A.activation(Wq[:, :n], P[pq][:, :n], AF.Sigmoid), reads=[rP[pq]], writes=[rWq])
                                yield
                                S.op("act", lambda: A.activation(W2[:, :n], W1[:, :n], AF.Ln, bias=lb_ap, scale=oml_ap),
                                     reads=[rW1, rlbt], writes=[rW2])
                                S.op("dve", lambda: V.tensor_tensor(Wq[:, :n], P[pq][:, :n], Wq[:, :n], ALU.mult), reads=[rP[pq], rWq], writes=[rWq])
                                S.op("dve", lambda: V.tensor_scalar(W5[:, :n], W1[:, :n], noml_ap, oml_ap, ALU.mult, ALU.add),
                                     reads=[rW1, rlbt], writes=[rW5])
                                yield
                                S.op("dve", lambda: V.tensor_tensor_scan(W3[:, :n], smask[:, :n], W2[:, :n], 0.0, ALU.mult, ALU.add),
                                     reads=[rW2, rcst], writes=[rW3])
                                if fwd:
                                    bb, rbb = W3, rW3
                                    bl = v3(W3)[:, :, CH - 1]
                                else:
                                    tc_ = v3(W3)[:, :, CH - 1:CH].to_broadcast([128, nch, CH])
                                    S.op("dve", lambda: V.tensor_tensor(v3(W4), tc_, v3(W3), ALU.subtract), reads=[rW3], writes=[rW4])
                                    S.op("dve", lambda: V.tensor_tensor(W4[:, :n], W4[:, :n], W2[:, :n], ALU.add),
                                         reads=[rW4, rW2], writes=[rW4])
                                    bb, rbb = W4, rW4
                                    bl = v3(W4)[:, :, 0]
                                yield
                                S.op("act", lambda: A.activation(Dall[:, c0:c0 + nch], bl, AF.Exp), reads=[rbb], writes=[rDall])
                                yield
                                res_[ti] = (bb, rbb)

                            def prep_b(ti):
                                bb, rbb = res_[ti]
                                t0, n = TT[ti]
                                nch, c0 = n // CH, t0 // CH
                                W1, W2, W3, W4, Wq, W5 = Wt[ti % 2]
                                rW1, rW2, rW3, rW4, rWq, rW5 = rWt[ti % 2]
                                v3 = lambda ap: ap[:, :n].rearrange("p (c k) -> p c k", k=CH)
                                S.op("act", lambda: A.activation(W2[:, :n], bb[:, :n], AF.Exp), reads=[rbb, rW2], writes=[rW2])
                                S.op("act", lambda: A.activation(W1[:, :n], bb[:, :n], AF.Exp, scale=-1.0), reads=[rbb, rW1], writes=[rW1])
                                yield
                                S.op("dve", lambda: V.tensor_tensor(QT[:, t0:t0 + n], Wq[:, :n], W2[:, :n], ALU.mult),
                                     reads=[rWq, rW2], writes=[rQT[ti]])
                                S.op("dve", lambda: V.tensor_tensor(W5[:, :n], W5[:, :n], W1[:, :n], ALU.mult),
                                     reads=[rW5, rW1], writes=[rW5])
                                yield
                                S.op("act", lambda: A.copy(KT[:, t0:t0 + n], W5[:, :n]), reads=[rW5], writes=[rKT[ti]])
                                dbc = Dall[:, c0:c0 + nch].unsqueeze(2).to_broadcast([128, nch, CH])
                                S.op("dve", lambda: V.tensor_tensor(v3(KHt), v3(W5), dbc, ALU.mult), reads=[rW5, rDall], writes=[rKHt])
                                yield
                                pk = nextp(4, 6)
                                ntile = n // 128
                                pkb = P[pk][:].bitcast(BF16)
                                S.group("pe", [(lambda j=j: T.transpose(pkb[:, j * 128:(j + 1) * 128],
                                                                        KHt[:, j * 128:(j + 1) * 128], identb[:]))
                                               for j in range(ntile)], reads=[rKHt, rmisc], writes=[rP[pk]])
                                yield
                                S.op("act", lambda: A.copy(KHT[:, t0 // 128:t0 // 128 + ntile, :],
                                                           pkb[:, :ntile * 128].rearrange("p (a b) -> p a b", b=128)),
                                     reads=[rP[pk]], writes=[rKHT])
                                yield
                            res_ = {}

                            def run_zip(g1, g2):
                                gens = [g for g in (g1, g2) if g is not None]
                                while gens:
                                    for g in list(gens):
                                        try:
                                            next(g)
                                        except StopIteration:
                                            gens.remove(g)
                            run_zip(prep_a(0), None)
                            for ti in range(len(TT)):
                                run_zip(prep_a(ti + 1) if ti + 1 < len(TT) else None, prep_b(ti))
                            W.after(kq)
                            ckpt("hg_prep")
                            order = (list(range(NXC, NCHK)) + list(range(NXC))) if fwd else list(range(NCHK - 1, -1, -1))
                            pos = {c: i for i, c in enumerate(order)}
                            for tt in range(18):
                                vb = tt % 2
                                S.op("dve", lambda vb=vb, tt=tt: V.tensor_tensor(
                                    Vx[vb][:], Vh[:, tt, :].unsqueeze(1).to_broadcast([128, CPT, 128]),
                                    cst[:, 896:896 + CPT].unsqueeze(2).to_broadcast([128, CPT, 128]), ALU.mult),
                                    reads=[rVh, rcst], writes=[rVx[vb]])
                                p = nextp(6, 8)
                                S.group("pe", [lambda tt=tt, vb=vb, p=p: T.matmul(
                                    P[p][:, :CPT * 128], KHT[:, tt, :], Vx[vb][:].rearrange("p a b -> p (a b)"), start=True, stop=True)],
                                    reads=[rKHT, rVx[vb]], writes=[rP[p]])
                                p0 = pos[tt * CPT]
                                if fwd:
                                    dst = dS[:, p0:p0 + CPT, :]
                                else:
                                    dst = dS[:, p0:p0 - CPT:-1, :] if p0 - CPT >= 0 else dS[:, p0::-1, :]
                                S.op("act", lambda dst=dst, p=p: A.copy(dst, P[p][:, :CPT * 128].rearrange("p (c v) -> p c v", v=128)),
                                     reads=[rP[p]], writes=[rdS, rdSh[0], rdSh[1]])
                            ckpt("hg_ds")
                            if fwd:
                                S.op("dve", lambda: V.tensor_copy(Dpos[:, NCC:NCHK], Dall[:, 0:NXC]), reads=[rDall], writes=[rDpos])
                                S.op("dve", lambda: V.tensor_copy(Dpos[:, 1:NCC], Dall[:, NXC + 1:NCHK]), reads=[rDall], writes=[rDpos])
                            else:
                                S.op("dve", lambda: V.tensor_copy(Dpos[:, 1:NCHK], Dall[:, NCHK - 2::-1]), reads=[rDall], writes=[rDpos])
                            S.op("dve", lambda: V.memset(Dpos[:, 0:1], 0.0), reads=[rDpos], writes=[rDpos])
                            for pp in range(1, NCHK):
                                for hf in range(2):
                                    vs = slice(hf * 64, hf * 64 + 64)
                                    S.op("dve", lambda pp=pp, vs=vs: V.scalar_tensor_tensor(
                                        dS[:, pp, vs], dS[:, pp - 1, vs], Dpos[:, pp:pp + 1], dS[:, pp, vs], ALU.mult, ALU.add),
                                        reads=[rdSh[hf], rDpos], writes=[rdSh[hf]])
                            ckpt("hg_scan")
                            mask = maskF if fwd else maskB
                            for g4 in range(5):
                                tts = list(range(g4 * 4, min(g4 * 4 + 4, 18)))
                                po = nextp(4, 6)
                                for tt in tts:
                                    off = (tt % 4) * 128
                                    fns = []
                                    for c in range(tt * CPT, (tt + 1) * CPT):
                                        pc = pos[c]
                                        lhs = zerob[:] if pc == 0 else dS[:, pc - 1, :]
                                        co = off + (c % CPT) * CH
                                        fns.append(lambda c=c, lhs=lhs, co=co, po=po: T.matmul(
                                            P[po][:, co:co + CH], lhs, QT[:, c * CH:(c + 1) * CH], start=(c % CPT == 0), stop=False,
                                            skip_group_check=True))
                                    S.group("pe", fns, reads=[rdS, rdSh[0], rdSh[1], rQT[g4], rmisc], writes=[rP[po]])
                                    pa = nextp(6, 8)
                                    S.group("pe", [lambda tt=tt, pa=pa: T.matmul(P[pa][:, 0:128], KT[:, tt * 128:(tt + 1) * 128],
                                                                                QT[:, tt * 128:(tt + 1) * 128], start=True, stop=True)],
                                            reads=[rKT[g4], rQT[g4]], writes=[rP[pa]])
                                    ab = tt % 2
                                    S.op("dve", lambda pa=pa, ab=ab, mask=mask: V.tensor_tensor(attm[ab][:], P[pa][:, 0:128], mask, ALU.mult),
                                         reads=[rP[pa], rcst], writes=[rattm[ab]])
                                    S.group("pe", [lambda tt=tt, ab=ab, off=off, po=po: T.matmul(
                                        P[po][:, off:off + 128], Vh[:, tt, :], attm[ab][:], start=False, stop=True, skip_group_check=True)],
                                        reads=[rVh, rattm[ab]], writes=[rP[po]])
                                nn = len(tts) * 128
                                t0 = g4 * 512
                                if fwd:
                                    S.op("act", lambda po=po, nn=nn, t0=t0: A.copy(o_acc[:, t0:t0 + nn], P[po][:, :nn]),
                                         reads=[rP[po]], writes=[ro_acc[g4]])
                                else:
                                    S.op("dve", lambda po=po, nn=nn, t0=t0: V.tensor_tensor(o_acc[:, t0:t0 + nn], o_acc[:, t0:t0 + nn],
                                                                                          P[po][:, :nn], ALU.add),
                                         reads=[rP[po], ro_acc[g4]], writes=[ro_acc[g4]])
                            if stop == "hg_o" + str(di):
                                dump("QT", QT[:], rQT); dump("KT", KT[:], rKT); dump("dS", dS[:].rearrange("p c v -> p (c v)"), [rdS])
                                dump("Dall", Dall[:], [rDall]); dump("Dpos", Dpos[:], [rDpos]); dump("oacc0_0", o_acc[:], ro_acc)
                            ckpt("hg_o" + str(di))
                        if f"oacc{l}_{h}" in dbg_d:
                            dump(f"oacc{l}_{h}", o_acc[:], ro_acc)
                        W1, rW1, Wq, rWq = Wt[0][0], rWt[0][0], Wt[0][4], rWt[0][4]
                        ro, rro = W1, rW1
                        ko, wo_, rwo = nextw()
                        kr, wr_, rwr = nextw()
                        gh = vv("hg", l)
                        for ti, (t0, n) in enumerate(TO):
                            S.op("act", lambda: A.activation(sqh[:, :n], o_acc[:, t0:t0 + n], AF.Square), reads=[ro_acc[ti]], writes=[rsqh])
                            p = nextp(0, 4)
                            S.group("pe", [lambda p=p: T.matmul(P[p][:, :n], onesb[:], sqh[:, :n], start=True, stop=True)],
                                    reads=[rsqh, rmisc], writes=[rP[p]])
                            S.op("act", lambda p=p: A.activation(ro[:, :n], P[p][:, :n], AF.Ln, bias=epsD[:, 1:2], scale=1.0 / 128),
                                 reads=[rP[p], rmisc], writes=[rro])
                            S.op("act", lambda: A.activation(ro[:, :n], ro[:, :n], AF.Exp, scale=-0.5), reads=[rro], writes=[rro])
                            S.op("dve", lambda: V.tensor_tensor(ro[:, :n], ro[:, :n], o_acc[:, t0:t0 + n], ALU.mult),
                                 reads=[rro, ro_acc[ti]], writes=[rro])
                            p2 = nextp(0, 4)
                            S.group("pe", [(lambda kc=kc, p2=p2: T.matmul(P[p2][:, :n], wo_[:, kc, :], hT[:, kc, t0:t0 + n],
                                                                         start=(kc == 0), stop=(kc == 7))) for kc in range(8)],
                                    reads=[rwo, rhT[ti]], writes=[rP[p2]])
                            S.op("act", lambda p2=p2: A.activation(Wq[:, :n], P[p2][:, :n], AF.Silu), reads=[rP[p2]], writes=[rWq])
                            S.op("dve", lambda: V.scalar_tensor_tensor(yh[:, t0:t0 + n], ro[:, :n], gh, Wq[:, :n], ALU.mult, ALU.mult),
                                 reads=[rro, rWq, rvecs], writes=[ryh[ti]])
                            col = 0 if t0 < NX else 1
                            for dj in range(8):
                                p3 = nextp(4, 8)
                                S.group("pe", [lambda p3=p3, dj=dj: T.matmul(P[p3][:, :n], wr_[:, dj, :], yh[:, t0:t0 + n], start=True, stop=True)],
                                        reads=[rwr, ryh[ti]], writes=[rP[p3]])
                                g1 = mod[:, 16 + dj, col:col + 1]
                                S.op("dve", lambda p3=p3, g1=g1, dj=dj: V.scalar_tensor_tensor(
                                    xT[:, dj, t0:t0 + n], P[p3][:, :n], g1, xT[:, dj, t0:t0 + n], ALU.mult, ALU.add),
                                    reads=[rP[p3], rmod, rxT[dj][ti]], writes=[rxT[dj][ti]])
                        if f"yh{l}_{h}" in dbg_d:
                            dump(f"yh{l}_{h}", yh[:], ryh)
                        W.after(kr)
                    S.barrier()
                if stop == "hgrn":
                    break
                ecm = ExitStack()
                em.enter_context(ecm)
                ymc = sbuf(ecm, "ymc", [128, 4, NT], BF16)
                with ExitStack() as ec:
                    rowo = (l % 2 == 0)
                    conv_tiles = TT if not last else TT[:4]
                    UPL = (32 * 79 + 31) if rowo else (62 * 64)
                    Upad = [sbuf(ec, f"Upad{i}", [128, UPL + 286], BF16) for i in range(2)]
                    Dg = [sbuf(ec, f"Dg{i}", [128, 31, 128], BF16) for i in range(2)]
                    sgt = [sbuf(ec, f"sgt{i}", [128, 512], F32) for i in range(2)]
                    cwb = sbuf(ec, "cwb", [128, 4 * 31], BF16)
                    rUp, rDg, rsgt = S.grid(2), S.grid(2), S.grid(2)
                    rcwb = S.res()
                    for i in range(2):
                        S.op("pool", lambda i=i: G.memset(Upad[i][:], 0.0), writes=[rUp[i]])
                    S.op("dve", lambda: V.tensor_copy(cwb[:], vecs[:, VOFF["convw"] + l * 124: VOFF["convw"] + (l + 1) * 124]),
                         reads=[rvecs], writes=[rcwb])

                    def uview(b, ti, k):
                        t0, n = TT[ti]
                        if t0 >= NX:
                            return Upad[b][:, UPL + k: UPL + k + NCX]
                        r0 = t0 // 64
                        if rowo:
                            return Upad[b][:, r0 * 79 + k: r0 * 79 + k + 8 * 79].rearrange("p (r c) -> p r c", c=79)[:, :, 0:64]
                        return Upad[b][:, (r0 + k) * 64: (r0 + k) * 64 + 512]

                    def pview(p, ti):
                        t0, n = TT[ti]
                        if t0 < NX and rowo:
                            return P[p][:, :n].rearrange("p (r c) -> p r c", c=64)
                        return P[p][:, :n]

                    def emit_glu(j):
                        b = j % 2
                        ka, wa_, rwa_ = nextw()
                        kg, wg_, rwg_ = nextw()
                        for ti in range(len(conv_tiles)):
                            t0, n = TT[ti]
                            pa = nextp(0, 4)
                            S.group("pe", [(lambda kc=kc, pa=pa: T.matmul(P[pa][:, :n], wa_[:, kc, :], hT[:, kc, t0:t0 + n],
                                                                         start=(kc == 0), stop=(kc == 7))) for kc in range(8)],
                                    reads=[rwa_, rhT[ti]], writes=[rP[pa]])
                            pg = nextp(0, 4)
                            S.group("pe", [(lambda kc=kc, pg=pg: T.matmul(P[pg][:, :n], wg_[:, kc, :], hT[:, kc, t0:t0 + n],
                                                                         start=(kc == 0), stop=(kc == 7))) for kc in range(8)],
                                    reads=[rwg_, rhT[ti]], writes=[rP[pg]])
                            sb_ = ti % 2
                            S.op("act", lambda pg=pg, sb_=sb_: A.activation(sgt[sb_][:, :n], P[pg][:, :n], AF.Sigmoid),
                                 reads=[rP[pg]], writes=[rsgt[sb_]])
                            sv = sgt[sb_][:, :n]
                            if t0 < NX and rowo:
                                sv = sv.rearrange("p (r c) -> p r c", c=64)
                            S.op("dve", lambda pa=pa, sv=sv, ti=ti, b=b: V.tensor_tensor(uview(b, ti, 15), pview(pa, ti), sv, ALU.mult),
                                 reads=[rP[pa], rsgt[sb_]], writes=[rUp[b]])
                        W.after(kg)
                        S.op("dve", lambda b=b, j=j: V.tensor_tensor(
                            Dg[b][:], identb[:].unsqueeze(1).to_broadcast([128, 31, 128]),
                            cwb[:, j * 31:(j + 1) * 31].unsqueeze(2).to_broadcast([128, 31, 128]), ALU.mult),
                            reads=[rmisc, rcwb], writes=[rDg[b]])

                    def emit_conv(j):
                        b = j % 2
                        cb = vv("convb", l * 4 + j)
                        for ti in range(len(conv_tiles)):
                            t0, n = TT[ti]
                            p = nextp(4, 8)
                            S.group("pe", [(lambda k_=k_, p=p, ti=ti, b=b: T.matmul(pview(p, ti), Dg[b][:, k_, :], uview(b, ti, k_),
                                                                                 start=(k_ == 0), stop=(k_ == 30))) for k_ in range(31)],
                                    reads=[rDg[b], rUp[b]], writes=[rP[p]])
                            S.op("act", lambda p=p, j=j, cb=cb: A.activation(ymc[:, j, t0:t0 + n], P[p][:, :n], AF.Identity, bias=cb, scale=1.0),
                                 reads=[rP[p], rvecs], writes=[rym[j][ti]])
                    emit_glu(0)
                    for j in range(4):
                        if j + 1 < 4:
                            emit_glu(j + 1)
                        emit_conv(j)
                    S.barrier()
                with ExitStack() as eln:
                    sq4 = sbuf(eln, "sq4", [128, 4, 512], BF16)
                    mu = sbuf(eln, "mu", [128, 512], F32)
                    msq = sbuf(eln, "msq", [128, 512], F32)
                    rsd = sbuf(eln, "rsd", [128, 512], F32)
                    lt = [sbuf(eln, f"lt{i}", [128, 512], F32) for i in range(2)]
                    rsq4, rmu, rmsq, rrsd = (S.res() for _ in range(4))
                    rlt = S.grid(2)
                    for ti, (t0, n) in enumerate(TO):
                        for j in range(4):
                            S.op("act", lambda j=j: A.activation(sq4[:, j, :n], ymc[:, j, t0:t0 + n], AF.Square),
                                 reads=[rym[j][ti]], writes=[rsq4])
                        p1 = nextp(0, 4)
                        S.group("pe", [(lambda j=j, p1=p1: T.matmul(P[p1][:, :n], onesb[:], ymc[:, j, t0:t0 + n], start=(j == 0), stop=(j == 3)))
                                       for j in range(4)], reads=[rym[j][ti] for j in range(4)] + [rmisc], writes=[rP[p1]])
                        p2 = nextp(0, 4)
                        S.group("pe", [(lambda j=j, p2=p2: T.matmul(P[p2][:, :n], onesb[:], sq4[:, j, :n], start=(j == 0), stop=(j == 3)))
                                       for j in range(4)], reads=[rsq4, rmisc], writes=[rP[p2]])
                        S.op("act", lambda p1=p1: A.mul(mu[:, :n], P[p1][:, :n], 1.0 / 512), reads=[rP[p1]], writes=[rmu])
                        S.op("dve", lambda: V.tensor_tensor(msq[:, :n], mu[:, :n], mu[:, :n], ALU.mult), reads=[rmu], writes=[rmsq])
                        S.op("dve", lambda p2=p2: V.scalar_tensor_tensor(rsd[:, :n], P[p2][:, :n], 1.0 / 512, msq[:, :n], ALU.mult, ALU.subtract),
                             reads=[rP[p2], rmsq], writes=[rrsd])
                        S.op("act", lambda: A.activation(rsd[:, :n], rsd[:, :n], AF.Ln, bias=epsD[:, 1:2], scale=1.0),
                             reads=[rrsd, rmisc], writes=[rrsd])
                        S.op("act", lambda: A.activation(rsd[:, :n], rsd[:, :n], AF.Exp, scale=-0.5), reads=[rrsd], writes=[rrsd])
                        for j in range(4):
                            b = j % 2
                            S.op("dve", lambda j=j, b=b: V.tensor_tensor(lt[b][:, :n], ymc[:, j, t0:t0 + n], mu[:, :n], ALU.subtract),
                                 reads=[rym[j][ti], rmu], writes=[rlt[b]])
                            S.op("dve", lambda b=b: V.tensor_tensor(lt[b][:, :n], lt[b][:, :n], rsd[:, :n], ALU.mult),
                                 reads=[rlt[b], rrsd], writes=[rlt[b]])
                            S.op("act", lambda j=j, b=b: A.activation(ymc[:, j, t0:t0 + n], lt[b][:, :n], AF.Silu,
                                                                      bias=vv("lnb", l * 4 + j), scale=vv("lng", l * 4 + j)),
                                 reads=[rlt[b], rvecs], writes=[rym[j][ti]])
                    S.barrier()
                dump(f"ymc{l}", ymc[:].rearrange("p a b -> p (a b)"), [r for rr in rym[:4] for r in rr])
                if stop == "conv":
                    break
                W2 = WChunks(ecm, "o", [wrow(j * 128) for j in range(4)], "pool", nst=2, nbf=4)
                W2.prefetch()
                wo4 = [W2.get(j) for j in range(4)]
                for dj in range(8):
                    for ti, (t0, n) in enumerate(TO):
                        col = 0 if t0 < NX else 1
                        p = nextp(0, 4)
                        S.group("pe", [(lambda j=j, p=p, dj=dj: T.matmul(P[p][:, :n], wo4[j][0][:, dj, :], ymc[:, j, t0:t0 + n],
                                                                        start=(j == 0), stop=(j == 3))) for j in range(4)],
                                reads=[w_[1] for w_ in wo4] + [rym[j][ti] for j in range(4)], writes=[rP[p]])
                        g1 = mod[:, 16 + dj, col:col + 1]
                        S.op("dve", lambda p=p, g1=g1, dj=dj: V.scalar_tensor_tensor(
                            xT[:, dj, t0:t0 + n], P[p][:, :n], g1, xT[:, dj, t0:t0 + n], ALU.mult, ALU.add),
                            reads=[rP[p], rmod, rxT[dj][ti]], writes=[rxT[dj][ti]])
                S.barrier()
            dump(f"xmix{l}", xT[:].rearrange("p a b -> p (a b)"), [r for rr in rxT for r in rr])
            if stop == "mix":
                break

            with ExitStack() as eo:
                comb = sbuf(eo, "comb", [128, 18, NE], F32)
                rcomb = S.res()
                with ExitStack() as er:
                    h2f = sbuf(er, "h2f", [128, 8, 512], F32)
                    sq = sbuf(er, "sq2", [128, 8, 512], BF16)
                    rstd = sbuf(er, "rstd2", [128, 512], F32)
                    tmp = [sbuf(er, f"nt2_{i}", [128, 512], F32) for i in range(2)]
                    rsq, rrstd = S.res(), S.res()
                    rtmp = S.grid(2)
                    rh2 = S.res()
                    pl = 7
                    for ti, (t0, n) in enumerate(TO):
                        if True:
                            col = 0 if t0 < NX else 1
                            for c in range(8):
                                S.op("act", lambda c=c: A.activation(sq[:, c, :n], xT[:, c, t0:t0 + n], AF.Square),
                                     reads=[rxT[c][ti]], writes=[rsq])
                            p = nextp(0, 4)
                            S.group("pe", [(lambda c=c, p=p: T.matmul(P[p][:, :n], onesb[:], sq[:, c, :n], start=(c == 0), stop=(c == 7)))
                                           for c in range(8)], reads=[rsq, rmisc], writes=[rP[p]])
                            S.op("act", lambda p=p: A.activation(rstd[:, :n], P[p][:, :n], AF.Ln, bias=epsD[:, 0:1], scale=1.0),
                                 reads=[rP[p], rmisc], writes=[rrstd])
                            S.op("act", lambda: A.activation(rstd[:, :n], rstd[:, :n], AF.Exp, scale=-0.5), reads=[rrstd], writes=[rrstd])
                            for c in range(8):
                                b = c % 2
                                S.op("dve", lambda c=c, b=b: V.tensor_tensor(tmp[b][:, :n], xT[:, c, t0:t0 + n], rstd[:, :n], ALU.mult),
                                     reads=[rxT[c][ti], rrstd], writes=[rtmp[b]])
                                sc_ap = se[:, 1, c, col:col + 1]
                                sh_ap = mod[:, 24 + c, col:col + 1]
                                S.op("act", lambda c=c, b=b, sc_ap=sc_ap, sh_ap=sh_ap: A.activation(
                                    h2f[:, c, :n], tmp[b][:, :n], AF.Identity, bias=sh_ap, scale=sc_ap),
                                    reads=[rtmp[b], rse, rmod], writes=[rh2])
                                if c % 2 == 0:
                                    S.op("pool", lambda c=c: G.tensor_copy(hT[:, c, t0:t0 + n], h2f[:, c, :n]),
                                         reads=[rh2], writes=[rhT[ti]])
                                else:
                                    S.op("act", lambda c=c, b=b, sc_ap=sc_ap, sh_ap=sh_ap: A.activation(
                                        hT[:, c, t0:t0 + n], tmp[b][:, :n], AF.Identity, bias=sh_ap, scale=sc_ap),
                                        reads=[rtmp[b], rse, rmod], writes=[rhT[ti]])
                            fns = []
                            for s_ in range(n // 128):
                                tt = t0 // 128 + s_
                                for kc in range(8):
                                    fns.append(lambda s_=s_, tt=tt, kc=kc: T.matmul(
                                        P[pl][:, tt * NE:(tt + 1) * NE], h2f[:, kc, s_ * 128:(s_ + 1) * 128], rw[:, kc, :],
                                        start=(kc == 0), stop=(kc == 7)))
                            S.group("pe", fns, reads=[rh2, rrw], writes=[rP[pl]])
                    ntl = sum(n for _, n in TO) // 128
                    NG = ntl * 4
                    sg_ = sbuf(er, "r_s", [128, ntl, NE], F32)
                    sbb = sbuf(er, "r_sb", [128, ntl, NE], F32)
                    ps6 = sbuf(er, "r_p6", [128, NG, 6], F32)
                    gs = sbuf(er, "r_gs", [128, NG], F32)
                    gm = sbuf(er, "r_gm", [128, ntl], F32)
                    ing = sbuf(er, "r_ing", [128, NG], F32)
                    sbm = sbuf(er, "r_sbm", [128, ntl, NE], F32)
                    sel = sbuf(er, "r_sel", [128, ntl, NE], F32)
                    m1 = sbuf(er, "r_m1", [128, ntl], F32)
                    rr_ = S.res()
                    R = dict(reads=[rr_], writes=[rr_])
                    S.op("act", lambda: A.activation(sg_[:].rearrange("p a b -> p (a b)"), P[pl][:, :ntl * NE], AF.Sigmoid),
                         reads=[rP[pl]], writes=[rr_])
                    rb_bc = vecs[:, VOFF["rbias"]:VOFF["rbias"] + NE].unsqueeze(1).to_broadcast([128, ntl, NE])
                    S.op("dve", lambda: V.tensor_tensor(sbb[:], sg_[:], rb_bc, ALU.add), reads=[rr_, rvecs], writes=[rr_])
                    g4v = sbb[:].rearrange("p a (g e) -> p (a g) e", e=4)
                    S.op("dve", lambda: V.tensor_tensor(ps6[:, :, 0:2], g4v[:, :, 0:4:2], g4v[:, :, 1:4:2], ALU.add), **R)
                    S.op("dve", lambda: V.tensor_tensor(ps6[:, :, 2:4], g4v[:, :, 0:2], g4v[:, :, 2:4], ALU.add), **R)
                    S.op("dve", lambda: V.tensor_tensor(ps6[:, :, 4:5], g4v[:, :, 0:1], g4v[:, :, 3:4], ALU.add), **R)
                    S.op("dve", lambda: V.tensor_tensor(ps6[:, :, 5:6], g4v[:, :, 1:2], g4v[:, :, 2:3], ALU.add), **R)
                    S.op("dve", lambda: V.tensor_reduce(gs[:], ps6[:], AX.X, ALU.max), **R)
                    S.op("dve", lambda: V.tensor_reduce(gm[:], gs[:].rearrange("p (a g) -> p a g", g=4), AX.X, ALU.max), **R)
                    S.op("dve", lambda: V.tensor_tensor(ing[:].rearrange("p (a g) -> p a g", g=4), gs[:].rearrange("p (a g) -> p a g", g=4),
                                                        gm[:].unsqueeze(2).to_broadcast([128, ntl, 4]), ALU.is_equal), **R)
                    S.op("dve", lambda: V.tensor_scalar(ing[:], ing[:], BIG, -BIG, ALU.mult, ALU.add), **R)
                    S.op("dve", lambda: V.tensor_tensor(sbm[:].rearrange("p a (g e) -> p (a g) e", e=4), g4v,
                                                        ing[:].unsqueeze(2).to_broadcast([128, NG, 4]), ALU.add), **R)
                    S.op("dve", lambda: V.tensor_reduce(m1[:], sbm[:], AX.X, ALU.max), **R)
                    S.op("dve", lambda: V.tensor_tensor(sel[:], sbm[:], m1[:].unsqueeze(2).to_broadcast([128, ntl, NE]), ALU.is_equal), **R)
                    S.op("dve", lambda: V.scalar_tensor_tensor(sbm[:], sel[:], -BIG, sbm[:], ALU.mult, ALU.add), **R)
                    S.op("dve", lambda: V.tensor_reduce(m1[:], sbm[:], AX.X, ALU.max), **R)
                    S.op("dve", lambda: V.tensor_tensor(sbm[:], sbm[:], m1[:].unsqueeze(2).to_broadcast([128, ntl, NE]), ALU.is_ge), **R)
                    S.op("dve", lambda: V.tensor_tensor(sel[:], sel[:], sbm[:], ALU.add), **R)
                    S.op("dve", lambda: V.tensor_tensor(sel[:], sel[:], sg_[:], ALU.mult), **R)
                    S.op("dve", lambda: V.tensor_reduce(m1[:], sel[:], AX.X, ALU.add), **R)
                    S.op("dve", lambda: V.reciprocal(m1[:], m1[:]), **R)
                    S.op("dve", lambda: V.tensor_tensor(comb[:, :ntl, :], sel[:], m1[:].unsqueeze(2).to_broadcast([128, ntl, NE]), ALU.mult),
                         reads=[rr_], writes=[rcomb])
                    S.barrier()
                comb_hi = sbuf(eo, "comb_hi", [128, 18, NE], BF16)
                comb_lo = sbuf(eo, "comb_lo", [128, 18, NE], BF16)
                comb_r = sbuf(eo, "comb_r", [128, 18, NE], F32)
                S.op("dve", lambda: V.tensor_copy(comb_hi[:], comb[:]), reads=[rcomb], writes=[rcomb])
                S.op("dve", lambda: V.tensor_tensor(comb_r[:], comb[:], comb_hi[:], ALU.subtract), reads=[rcomb], writes=[rcomb])
                S.op("dve", lambda: V.tensor_copy(comb_lo[:], comb_r[:]), reads=[rcomb], writes=[rcomb])
                dump(f"comb{l}", comb[:].rearrange("p a b -> p (a b)"), [rcomb])
                dump(f"h2_{l}", hT[:].rearrange("p a b -> p (a b)"), rhT)
                if stop == "router":
                    break
                NU = NE * 4
                NST, NBF = 4, 2
                stg = [sbuf(eo, f"stg{i}", [128, 2048], F32) for i in range(NST)]
                wbf = [sbuf(eo, f"wbf{i}", [128, 3, 2048], BF16) for i in range(NBF)]
                cg = sbuf(eo, "cg", [128, NT], F32)
                actb = [sbuf(eo, f"actb{i}", [128, 2, 512], BF16) for i in range(2)]
                sgm = [sbuf(eo, f"sgm{i}", [128, 512], F32) for i in range(2)]
                t1m = [sbuf(eo, f"t1m{i}", [128, 512], F32) for i in range(2)]
                rstg = S.grid(NST)
                rwbf = S.grid(NBF, 3)
                rcg = S.res()
                ractb, rsgm, rt1m = S.grid(2), S.grid(2), S.grid(2)
                dstg = [S.dsem() for _ in range(NST)]
                pieces = []
                for e in range(NE):
                    for q in range(4):
                        f0 = q * 256
                        pieces.append((wg_d[l, e, :, f0:f0 + 256].rearrange("(kc p) f -> p kc f", p=128), 0))
                        pieces.append((wu_d[l, e, :, f0:f0 + 256].rearrange("(kc p) f -> p kc f", p=128), 1))
                        pieces.append((wd_d[l, e, f0:f0 + 256, :].rearrange("(fc p) d -> p fc d", p=128), 2))
                pl_, pc_ = [0], [0]

                def p_load():
                    if pl_[0] >= len(pieces):
                        return
                    i = pl_[0] % NST
                    src, kind = pieces[pl_[0]]
                    dst = stg[i][:].rearrange("p (a b) -> p a b", a=(8 if kind < 2 else 2))
                    S.dma("sp", dst, src, dstg[i], writes=[rstg[i]])
                    pl_[0] += 1

                def p_cast():
                    if pc_[0] >= len(pieces):
                        return
                    k = pc_[0]
                    i = k % NST
                    u, kind = k // 3, k % 3
                    j = u % NBF
                    S.op("act", lambda i=i, j=j, kind=kind: A.copy(wbf[j][:, kind, :], stg[i][:]),
                         reads=[rstg[i]], writes=[rwbf[j][kind]])
                    pc_[0] += 1
                    p_load()
                for _ in range(NST):
                    p_load()
                for _ in range(3):
                    p_cast()
                def emit_cg(e):
                    for g4 in range((ntl + 3) // 4):
                        tts = list(range(g4 * 4, min(g4 * 4 + 4, ntl)))
                        p = 7
                        fns = []
                        for tt in tts:
                            for hl, cb_ in enumerate((comb_hi, comb_lo)):
                                fns.append(lambda tt=tt, hl=hl, cb_=cb_: T.matmul(
                                    P[p][:, (tt % 4) * 128:(tt % 4 + 1) * 128],
                                    cb_[:, tt, e:e + 1].to_broadcast([128, 128]), identb[:], start=(hl == 0), stop=(hl == 1)))
                        S.group("pe", fns, reads=[rcomb, rmisc], writes=[rP[p]])
                        nn = len(tts) * 128
                        S.op("dve", lambda p=p, g4=g4, nn=nn: V.tensor_copy(cg[:, g4 * 512:g4 * 512 + nn], P[p][:, :nn]),
                             reads=[rP[p]], writes=[rcg])

                def emit_gu(u, ti, ab):
                    e, q = u // 4, u % 4
                    j = u % NBF
                    t0, n = TO[ti]
                    wgt = wbf[j][:, 0, :].rearrange("p (kc f) -> p kc f", kc=8)
                    wut = wbf[j][:, 1, :].rearrange("p (kc f) -> p kc f", kc=8)
                    if q == 0 and ti == 0:
                        emit_cg(e)
                    for fc in range(2):
                        pg = nextp(0, 4)
                        S.group("pe", [(lambda kc=kc, pg=pg, fc=fc: T.matmul(P[pg][:, :n], wgt[:, kc, fc * 128:(fc + 1) * 128],
                                                                           hT[:, kc, t0:t0 + n], start=(kc == 0), stop=(kc == 7)))
                                       for kc in range(8)], reads=[rwbf[j][0], rhT[ti]], writes=[rP[pg]])
                        pu = nextp(0, 4)
                        S.group("pe", [(lambda kc=kc, pu=pu, fc=fc: T.matmul(P[pu][:, :n], wut[:, kc, fc * 128:(fc + 1) * 128],
                                                                           hT[:, kc, t0:t0 + n], start=(kc == 0), stop=(kc == 7)))
                                       for kc in range(8)], reads=[rwbf[j][1], rhT[ti]], writes=[rP[pu]])
                        S.op("act", lambda pg=pg, fc=fc: A.activation(sgm[fc][:, :n], P[pg][:, :n], AF.Silu),
                             reads=[rP[pg]], writes=[rsgm[fc]])
                        S.op("dve", lambda pu=pu, fc=fc: V.tensor_tensor(t1m[fc][:, :n], P[pu][:, :n], sgm[fc][:, :n], ALU.mult),
                             reads=[rP[pu], rsgm[fc]], writes=[rt1m[fc]])
                        S.op("pool", lambda fc=fc, ab=ab: G.tensor_tensor(actb[ab][:, fc, :n], t1m[fc][:, :n], cg[:, t0:t0 + n], ALU.mult),
                             reads=[rt1m[fc], rcg], writes=[ractb[ab]])

                def emit_dn(u, ti, ab):
                    j = u % NBF
                    t0, n = TO[ti]
                    col = 0 if t0 < NX else 1
                    wdt = wbf[j][:, 2, :].rearrange("p (fc d) -> p fc d", fc=2)
                    for dj in range(8):
                        po = nextp(4, 7)
                        S.group("pe", [(lambda fc=fc, po=po, dj=dj, ab=ab: T.matmul(
                            P[po][:, :n], wdt[:, fc, dj * 128:(dj + 1) * 128], actb[ab][:, fc, :n], start=(fc == 0), stop=(fc == 1)))
                            for fc in range(2)], reads=[rwbf[j][2], ractb[ab]], writes=[rP[po]])
                        g2 = mod[:, 40 + dj, col:col + 1]
                        S.op("dve", lambda po=po, g2=g2, dj=dj: V.scalar_tensor_tensor(
                            xT[:, dj, t0:t0 + n], P[po][:, :n], g2, xT[:, dj, t0:t0 + n], ALU.mult, ALU.add),
                            reads=[rP[po], rmod, rxT[dj][ti]], writes=[rxT[dj][ti]])

                items = [(u, ti) for u in range(NU) for ti in range(len(TO))]
                prev = None
                for k_, (u, ti) in enumerate(items):
                    emit_gu(u, ti, k_ % 2)
                    if not MOE_PIPE:
                        emit_dn(u, ti, k_ % 2)
                    elif prev is not None:
                        emit_dn(*prev)
                    if ti == 0:
                        for _ in range(3):
                            p_cast()
                    prev = (u, ti, k_ % 2)
                if MOE_PIPE:
                    emit_dn(*prev)
                S.barrier()
            dump(f"xout{l}", xT[:].rearrange("p a b -> p (a b)"), [r for rr in rxT for r in rr])

        if stop is None:
            with ExitStack() as ef:
                sq = sbuf(ef, "sqf", [128, 8, 512], BF16)
                rstd = sbuf(ef, "rstdf", [128, 512], F32)
                tmp = [sbuf(ef, f"ntf{i}", [128, 512], F32) for i in range(2)]
                yT = sbuf(ef, "yT", [128, 8, 512], F32)
                yo = [sbuf(ef, f"yo{i}", [128, D], F32) for i in range(2)]
                gfs = sbuf(ef, "gfs", [128, 8], F32)
                rsq, rrstd, ryT, rgfs = (S.res() for _ in range(4))
                rtmp, ryo = S.grid(2), S.grid(2)
                dyo = [S.dsem(), S.dsem()]
                S.op("dve", lambda: V.tensor_scalar(gfs[:], vecs[:, VOFF["fing"]:VOFF["fing"] + 8], float(np.sqrt(D)), None, ALU.mult),
                     reads=[rvecs], writes=[rgfs])
                for ti, (t0, n) in enumerate(TT[:4]):
                    for c in range(8):
                        S.op("act", lambda c=c: A.activation(sq[:, c, :], xT[:, c, t0:t0 + n], AF.Square), reads=[rxT[c][ti]], writes=[rsq])
                    p = nextp(0, 4)
                    S.group("pe", [(lambda c=c, p=p: T.matmul(P[p][:], onesb[:], sq[:, c, :], start=(c == 0), stop=(c == 7)))
                                   for c in range(8)], reads=[rsq, rmisc], writes=[rP[p]])
                    S.op("act", lambda p=p: A.activation(rstd[:], P[p][:], AF.Ln, bias=epsD[:, 0:1], scale=1.0),
                         reads=[rP[p], rmisc], writes=[rrstd])
                    S.op("act", lambda: A.activation(rstd[:], rstd[:], AF.Exp, scale=-0.5), reads=[rrstd], writes=[rrstd])
                    for c in range(8):
                        b = c % 2
                        S.op("dve", lambda c=c, b=b: V.tensor_tensor(tmp[b][:], xT[:, c, t0:t0 + n], rstd[:], ALU.mult),
                             reads=[rxT[c][ti], rrstd], writes=[rtmp[b]])
                        S.op("act", lambda c=c, b=b: A.activation(yT[:, c, :], tmp[b][:], AF.Copy, scale=gfs[:, c:c + 1]),
                             reads=[rtmp[b], rgfs], writes=[ryT])
                    for j in range(4):
                        tt = ti * 4 + j
                        b = tt % 2
                        for half in range(2):
                            pp = nextp(4, 8)
                            S.group("pe", [(lambda c=c, pp=pp, j=j: T.transpose(P[pp][:, (c % 4) * 128:(c % 4 + 1) * 128],
                                                                               yT[:, c, j * 128:(j + 1) * 128], ident))
                                           for c in range(half * 4, half * 4 + 4)], reads=[ryT, rcst], writes=[rP[pp]])
                            if half == 0:
                                S.op("act", lambda pp=pp, b=b: A.copy(yo[b][:, 0:512], P[pp][:]), reads=[rP[pp]], writes=[ryo[b]])
                            else:
                                S.op("dve", lambda pp=pp, b=b: V.tensor_copy(yo[b][:, 512:1024], P[pp][:]), reads=[rP[pp]], writes=[ryo[b]])
                        S.dma("sp", y_d[tt * 128:(tt + 1) * 128, :], yo[b][:], dyo[b], reads=[ryo[b]])
                S.barrier()
        S.barrier()
    return nc


def _chunked(v, n):
    return np.ascontiguousarray(np.asarray(v, np.float32).reshape(n, 128).T)


def make_consts():
    c = np.zeros((128, NCST), np.float32)
    c[:, 0:128] = np.eye(128, dtype=np.float32)
    s = np.arange(128)[:, None]
    t = np.arange(128)[None, :]
    same = (s // CH) == (t // CH)
    c[:, 128:256] = (same & (s <= t)).astype(np.float32)
    c[:, 256:384] = (same & (s >= t)).astype(np.float32)
    m = np.ones(512, np.float32)
    m[::CH] = 0.0
    c[:, 384:896] = m[None, :]
    for i in range(4):
        c[32 * i:32 * i + 32, 896 + i] = 1.0
    return c


def make_in_maps(inputs, cores):
    f = lambda k: np.asarray(inputs[k], np.float32)
    vecs = np.zeros((128, NV), np.float32)

    def put(name, arr):
        arr = np.asarray(arr, np.float32)
        vecs[:, VOFF[name]:VOFF[name] + arr.shape[1]] = arr
    put("n1g", np.concatenate([_chunked(f("norm1_g")[l], 8) for l in range(DEPTH)], 1))
    put("n2g", np.concatenate([_chunked(f("norm2_g")[l], 8) for l in range(DEPTH)], 1))
    put("fing", _chunked(f("final_norm_g"), 8))
    put("convb", np.concatenate([_chunked(f("conv_b")[l], 4) for l in range(DEPTH)], 1))
    put("lng", np.concatenate([_chunked(f("conv_ln_g")[l], 4) for l in range(DEPTH)], 1))
    put("lnb", np.concatenate([_chunked(f("conv_ln_b")[l], 4) for l in range(DEPTH)], 1))
    put("hg", np.stack([f("hgrn_norm_g")[l] for l in range(DEPTH)], 1))
    put("lbf", np.concatenate([_chunked(f("lb_fwd")[l], 4) for l in range(DEPTH)], 1))
    put("lbb", np.concatenate([_chunked(f("lb_bwd")[l], 4) for l in range(DEPTH)], 1))
    cw = f("conv_w")
    cwr = cw.reshape(DEPTH, 31, 4, 128).transpose(3, 0, 2, 1).reshape(128, DEPTH * 4 * 31)
    put("convw", cwr)
    put("rbias", np.broadcast_to(f("router_bias")[None, :], (128, NE)))
    bada = np.stack([_chunked(f("b_ada")[l], 48) for l in range(DEPTH)], 0)
    rwr = np.ascontiguousarray(f("router_w").reshape(8, 128, NE).transpose(1, 0, 2).reshape(128, 8 * NE))
    consts = make_consts()
    shared = {"w_ada": f("w_ada"), "b_ada_r": bada, "vecs": vecs, "consts": consts, "w_in": f("w_in"),
              "w_out": f("w_out"), "router_w_r": rwr, "w_gate": f("w_gate"), "w_up": f("w_up"), "w_down": f("w_down")}
    maps = []
    cc = f("c_ctx")
    for b in cores:
        c2 = np.stack([_chunked(f("c")[b], 8), _chunked(cc, 8)], 2).reshape(128, 16)
        m = dict(shared)
        m["x"] = np.ascontiguousarray(f("x")[b])
        m["ctx"] = np.ascontiguousarray(f("ctx")[b])
        m["c2"] = np.ascontiguousarray(c2)
        maps.append(m)
    return maps


_NC_CACHE = {}


def kernel(**inputs):
    if "nc" not in _NC_CACHE:
        _NC_CACHE["nc"] = build_program()
    nc = _NC_CACHE["nc"]
    maps = make_in_maps(inputs, list(range(8)))
    res = run_bass_kernel_spmd(nc, maps, core_ids=list(range(8)))
    return np.stack([np.asarray(r["y"], np.float32) for r in res.results], 0)
```

```python
import numpy as np
from contextlib import ExitStack, suppress
import concourse.bass as bass
import concourse.mybir as mybir
from concourse.bass_utils import run_bass_kernel_spmd

F32, BF16 = mybir.dt.float32, mybir.dt.bfloat16
AF = mybir.ActivationFunctionType
ALU = mybir.AluOpType
AX = mybir.AxisListType

D = 1024
NX = 2048
NCX = 256
NT = NX + NCX
DEPTH = 2
NE = 16
EPS = 1e-6
CH = 32
NCHK = NT // CH
TT = [(0, 512), (512, 512), (1024, 512), (1536, 512), (2048, 256)]
BIG = 1.0e4
MOE_PIPE = True

VOFF = {}
_o = 0
for _n, _w in (("n1g", 16), ("n2g", 16), ("fing", 8), ("convb", 8), ("lng", 8), ("lnb", 8),
               ("hg", 2), ("lbf", 8), ("lbb", 8), ("convw", 2 * 4 * 31), ("rbias", 16)):
    VOFF[_n] = _o
    _o += _w
NV = _o
COFF = {"ident": 0, "maskF": 128, "maskB": 256, "smask": 384}
NCST = 384 + 512 + 4


class Res:
    __slots__ = ("w", "r")

    def __init__(self):
        self.w = None
        self.r = {}


class Stream:
    def __init__(self, eng, sem, key):
        self.eng, self.sem, self.key = eng, sem, key
        self.count = 0
        self.waited = {}


class DSem:
    def __init__(self, handle, key):
        self.handle, self.key, self.count = handle, key, 0


class Sched:
    def __init__(self, nc, es):
        self.nc, self.es = nc, es
        self.semh = {}
        self.streams = {}
        for name, eng in (("pe", nc.tensor), ("act", nc.scalar), ("dve", nc.vector),
                          ("pool", nc.gpsimd), ("sp", nc.sync)):
            h = es.enter_context(nc.semaphore("s_" + name))
            self.semh[name] = h
            self.streams[name] = Stream(eng, h, name)
        self.dsems = []

    def res(self):
        return Res()

    def grid(self, *dims):
        if len(dims) == 1:
            return [Res() for _ in range(dims[0])]
        return [self.grid(*dims[1:]) for _ in range(dims[0])]

    def dsem(self):
        key = f"d{len(self.dsems)}"
        h = self.es.enter_context(self.nc.semaphore("s_" + key))
        self.semh[key] = h
        d = DSem(h, key)
        self.dsems.append(d)
        return d

    def _deps(self, st, reads, writes):
        deps = {}

        def add(tok, same_ok):
            if tok is None:
                return
            k, v = tok
            if k == st.key and not same_ok:
                return
            if deps.get(k, 0) < v:
                deps[k] = v
        for r in reads:
            add(r.w, True)
        for w in writes:
            add(w.w, False)
            for k, v in w.r.items():
                add((k, v), False)
        for k, v in deps.items():
            if st.waited.get(k, 0) < v:
                st.waited[k] = v
                st.eng.wait_ge(self.semh[k], v)

    def _mark(self, key, val, reads, writes):
        for r in reads:
            r.r[key] = val
        for w in writes:
            w.w = (key, val)
            w.r = {}

    def op(self, sname, fn, reads=(), writes=()):
        st = self.streams[sname]
        self._deps(st, reads, writes)
        st.count += 1
        fn().then_inc(st.sem, 1)
        self._mark(st.key, st.count, reads, writes)

    def group(self, sname, fns, reads=(), writes=()):
        st = self.streams[sname]
        self._deps(st, reads, writes)
        for fn in fns[:-1]:
            fn()
        st.count += 1
        fns[-1]().then_inc(st.sem, 1)
        self._mark(st.key, st.count, reads, writes)

    def dma(self, sname, out, in_, ds, reads=(), writes=()):
        st = self.streams[sname]
        self._deps(st, reads, writes)
        ds.count += 16
        st.eng.dma_start(out=out, in_=in_).then_inc(ds.handle, 16)
        self._mark(ds.key, ds.count, reads, writes)

    def barrier(self):
        for st in self.streams.values():
            for o in self.streams.values():
                if o is not st and o.count > st.waited.get(o.key, 0):
                    st.waited[o.key] = o.count
                    st.eng.wait_ge(o.sem, o.count)
            for d in self.dsems:
                if d.count > st.waited.get(d.key, 0):
                    st.waited[d.key] = d.count
                    st.eng.wait_ge(d.handle, d.count)


class _Stop(Exception):
    pass


def build_program(nlayers=DEPTH, dbg=None, stop=None):
    dbg = dbg or {}
    nc = bass.Bass("TRN2", target_bir_lowering=False)
    dr = lambda n, s, k="ExternalInput": nc.dram_tensor(n, s, F32, kind=k).ap()
    x_d = dr("x", [NX, D])
    ctx_d = dr("ctx", [NCX, D])
    c2_d = dr("c2", [128, 16])
    wada_d = dr("w_ada", [DEPTH, D, 6 * D])
    bada_d = dr("b_ada_r", [DEPTH, 128, 48])
    vecs_d = dr("vecs", [128, NV])
    cst_d = dr("consts", [128, NCST])
    win_d = dr("w_in", [DEPTH, D, 3584])
    wout_d = dr("w_out", [DEPTH, D, D])
    rw_d = dr("router_w_r", [128, 8 * NE])
    wg_d = dr("w_gate", [DEPTH, NE, D, D])
    wu_d = dr("w_up", [DEPTH, NE, D, D])
    wd_d = dr("w_down", [DEPTH, NE, D, D])
    y_d = dr("y", [NX, D], "ExternalOutput")
    dbg_d = {k: nc.dram_tensor("dbg_" + k, list(s[0]), BF16 if s[1] == "bf16" else F32, kind="ExternalOutput").ap()
             for k, s in dbg.items()}

    es = ExitStack()
    with suppress(_Stop), es:
        S = Sched(nc, es)
        V, A, G, T = nc.vector, nc.scalar, nc.gpsimd, nc.tensor

        nsb = [0]

        def sbuf(es_, name, shape, dt):
            nsb[0] += 1
            return es_.enter_context(nc.sbuf_tensor(f"sb{nsb[0]}_{name}", shape, dt))

        xT = sbuf(es, "xT", [128, 8, NT], F32)
        hT = sbuf(es, "hT", [128, 8, NT], BF16)
        cst = sbuf(es, "cst", [128, NCST], F32)
        vecs = sbuf(es, "vecs", [128, NV], F32)
        identb = sbuf(es, "identb", [128, 128], BF16)
        onesb = sbuf(es, "onesb", [128, 128], BF16)
        zerob = sbuf(es, "zerob", [128, 128], BF16)
        epsD = sbuf(es, "epsD", [128, 3], F32)
        mod = sbuf(es, "mod", [128, 48, 2], F32)
        se = sbuf(es, "se", [128, 2, 8, 2], F32)
        lbt = sbuf(es, "lbt", [128, 2, 4, 3], F32)
        rw = sbuf(es, "rw", [128, 8, NE], F32)
        P = [es.enter_context(nc.psum_tensor(f"ps{i}", [128, 512], F32)) for i in range(8)]
        rP = S.grid(8)
        rxT = S.grid(8, 5)
        rhT = S.grid(5)
        rcst, rvecs, rmisc, rmod, rse, rlbt, rrw = (S.res() for _ in range(7))
        ident = cst[:, 0:128]
        maskF = cst[:, 128:256]
        maskB = cst[:, 256:384]
        smask = cst[:, 384:896]
        dconst = S.dsem()
        vv = lambda n, i: vecs[:, VOFF[n] + i: VOFF[n] + i + 1]

        S.dma("sp", cst[:], cst_d[:, :], S.dsem(), writes=[rcst])
        S.dma("sp", vecs[:], vecs_d[:, :], S.dsem(), writes=[rvecs])
        S.dma("sp", rw[:].rearrange("p a b -> p (a b)"), rw_d[:, :], S.dsem(), writes=[rrw])
        S.op("dve", lambda: V.memset(onesb[:], 1.0), writes=[rmisc])
        S.op("dve", lambda: V.memset(zerob[:], 0.0), writes=[rmisc])
        S.op("dve", lambda: V.memset(epsD[:, 0:1], float(D * EPS)), writes=[rmisc])
        S.op("dve", lambda: V.memset(epsD[:, 1:3], float(EPS)), writes=[rmisc])
        S.op("dve", lambda: V.tensor_copy(identb[:], cst[:, 0:128]), reads=[rcst], writes=[rmisc])

        pctr = [0]

        def nextp(lo, hi):
            p = lo + pctr[0] % (hi - lo)
            pctr[0] += 1
            return p

        def dump(name, ap_sb, res_list):
            if name in dbg_d:
                ds = S.dsem()
                S.dma("sp", dbg_d[name], ap_sb, ds, reads=res_list)

        with ExitStack() as ea:
            xin = [sbuf(ea, f"xin{i}", [128, D], F32) for i in range(2)]
            rxin = S.grid(2)
            dxin = [S.dsem(), S.dsem()]
            for tt in range(18):
                b = tt % 2
                src = x_d[tt * 128:(tt + 1) * 128, :] if tt < 16 else ctx_d[(tt - 16) * 128:(tt - 15) * 128, :]
                S.dma("sp", xin[b][:], src, dxin[b], writes=[rxin[b]])
                for half in range(2):
                    p = nextp(0, 4)
                    fns = [(lambda c=c, p=p, b=b: T.transpose(P[p][:, (c % 4) * 128:(c % 4 + 1) * 128],
                                                              xin[b][:, c * 128:(c + 1) * 128], ident))
                           for c in range(half * 4, half * 4 + 4)]
                    S.group("pe", fns, reads=[rxin[b], rcst], writes=[rP[p]])
                    ws = [rxT[c][tt // 4] for c in range(half * 4, half * 4 + 4)]
                    eng = "dve" if half == 0 else "act"
                    dst = xT[:, half * 4:half * 4 + 4, tt * 128:(tt + 1) * 128]
                    srcp = P[p][:].rearrange("p (c t) -> p c t", c=4)
                    if eng == "dve":
                        S.op("dve", lambda dst=dst, srcp=srcp: V.tensor_copy(dst, srcp), reads=[rP[p]], writes=ws)
                    else:
                        S.op("act", lambda dst=dst, srcp=srcp: A.copy(dst, srcp), reads=[rP[p]], writes=ws)
            S.barrier()

        allx = lambda ti: [rxT[c][ti] for c in range(8)]

        def norm_mod(nidx, tiles, h2f=None, rh2f=None, es_=None):
            sq = sbuf(es_, f"sq{nidx}", [128, 8, 512], BF16)
            rstd = sbuf(es_, f"rstd{nidx}", [128, 512], F32)
            tmp = [sbuf(es_, f"nt{nidx}_{i}", [128, 512], F32) for i in range(2)]
            rsq, rrstd = S.res(), S.res()
            rtmp = S.grid(2)
            shb = 0 if nidx == 0 else 24
            for ti, (t0, n) in enumerate(tiles):
                col = 0 if t0 < NX else 1
                for c in range(8):
                    S.op("act", lambda c=c: A.activation(sq[:, c, :n], xT[:, c, t0:t0 + n], AF.Square),
                         reads=[rxT[c][ti]], writes=[rsq])
                p = nextp(0, 4)
                S.group("pe", [(lambda c=c, p=p: T.matmul(P[p][:, :n], onesb[:], sq[:, c, :n], start=(c == 0), stop=(c == 7)))
                               for c in range(8)], reads=[rsq, rmisc], writes=[rP[p]])
                S.op("act", lambda p=p: A.activation(rstd[:, :n], P[p][:, :n], AF.Ln, bias=epsD[:, 0:1], scale=1.0),
                     reads=[rP[p], rmisc], writes=[rrstd])
                S.op("act", lambda: A.activation(rstd[:, :n], rstd[:, :n], AF.Exp, scale=-0.5), reads=[rrstd], writes=[rrstd])
                for c in range(8):
                    b = c % 2
                    S.op("dve", lambda c=c, b=b: V.tensor_tensor(tmp[b][:, :n], xT[:, c, t0:t0 + n], rstd[:, :n], ALU.mult),
                         reads=[rxT[c][ti], rrstd], writes=[rtmp[b]])
                    sc_ap = se[:, nidx, c, col:col + 1]
                    sh_ap = mod[:, shb + c, col:col + 1]
                    if h2f is None:
                        S.op("act", lambda c=c, b=b, sc_ap=sc_ap, sh_ap=sh_ap: A.activation(
                            hT[:, c, t0:t0 + n], tmp[b][:, :n], AF.Identity, bias=sh_ap, scale=sc_ap),
                            reads=[rtmp[b], rse, rmod], writes=[rhT[ti]])
                    else:
                        S.op("act", lambda c=c, b=b, sc_ap=sc_ap, sh_ap=sh_ap: A.activation(
                            h2f[:, c, t0:t0 + n], tmp[b][:, :n], AF.Identity, bias=sh_ap, scale=sc_ap),
                            reads=[rtmp[b], rse, rmod], writes=[rh2f[ti]])
                        S.op("pool", lambda c=c: G.tensor_copy(hT[:, c, t0:t0 + n], h2f[:, c, t0:t0 + n]),
                             reads=[rh2f[ti]], writes=[rhT[ti]])

        class WChunks:
            def __init__(self, es_, tag, srcs, ceng, nst=3, nbf=3):
                self.srcs, self.ceng = srcs, ceng
                self.st = [sbuf(es_, f"wst{tag}{i}", [128, 8, 128], F32) for i in range(nst)]
                self.bf = [sbuf(es_, f"wbf{tag}{i}", [128, 8, 128], BF16) for i in range(nbf)]
                self.rst, self.rbf = S.grid(nst), S.grid(nbf)
                self.dst = [S.dsem() for _ in range(nst)]
                self.nl = 0
                self.nc_ = 0
                self.look = nbf - 1

            def _load(self):
                if self.nl >= len(self.srcs):
                    return
                i = self.nl % len(self.st)
                S.dma("sp", self.st[i][:], self.srcs[self.nl], self.dst[i], writes=[self.rst[i]])
                self.nl += 1

            def _cast(self):
                if self.nc_ >= len(self.srcs):
                    return
                i = self.nc_ % len(self.st)
                j = self.nc_ % len(self.bf)
                src, dst = self.st[i], self.bf[j]
                if self.ceng == "pool":
                    S.op("pool", lambda: G.tensor_copy(dst[:], src[:]), reads=[self.rst[i]], writes=[self.rbf[j]])
                else:
                    S.op("act", lambda: A.copy(dst[:], src[:]), reads=[self.rst[i]], writes=[self.rbf[j]])
                self.nc_ += 1

            def prefetch(self):
                for _ in range(len(self.st)):
                    self._load()
                for _ in range(self.look):
                    self._cast()
                    self._load()

            def get(self, k):
                while self.nc_ <= k:
                    self._cast()
                    self._load()
                j = k % len(self.bf)
                return self.bf[j], self.rbf[j]

            def after(self, k):
                while self.nc_ <= k + self.look and self.nc_ < len(self.srcs):
                    self._cast()
                    self._load()

        def proj_fm(wt, rwt, tiles, consume, plo=0, phi=4):
            for ti, (t0, n) in enumerate(tiles):
                p = nextp(plo, phi)
                S.group("pe", [(lambda kc=kc, p=p: T.matmul(P[p][:, :n], wt[:, kc, :], hT[:, kc, t0:t0 + n],
                                                            start=(kc == 0), stop=(kc == 7))) for kc in range(8)],
                        reads=[rwt, rhT[ti]], writes=[rP[p]])
                consume(p, ti, t0, n)

        def ckpt(name):
            if stop == name:
                S.barrier()
                raise _Stop()

        for l in ([] if stop == "load" else range(nlayers)):
            last = (l == DEPTH - 1)
            TO = TT[:4] if last else TT
            with ExitStack() as eb:
                c2 = sbuf(eb, "c2", [128, 8, 2], F32)
                sc2 = sbuf(eb, "sc2", [128, 8, 2], F32)
                bada = sbuf(eb, "bada", [128, 48], F32)
                wa = [sbuf(eb, f"wa{i}", [128, 8, 512], F32) for i in range(2)]
                rc2, rsc2, rbada = S.res(), S.res(), S.res()
                rwa = S.grid(2)
                dwa = [S.dsem(), S.dsem()]
                S.dma("sp", c2[:].rearrange("p a b -> p (a b)"), c2_d[:, :], S.dsem(), writes=[rc2])
                S.dma("sp", bada[:], bada_d[l, :, :], S.dsem(), writes=[rbada])
                S.op("act", lambda: A.activation(sc2[:], c2[:], AF.Silu), reads=[rc2], writes=[rsc2])
                pm = 7
                for g in range(12):
                    b = g % 2
                    S.dma("sp", wa[b][:], wada_d[l, :, g * 512:(g + 1) * 512].rearrange("(kc p) f -> p kc f", p=128),
                          dwa[b], writes=[rwa[b]])
                    fns = []
                    for j in range(4):
                        fc = g * 4 + j
                        for kc in range(8):
                            fns.append(lambda j=j, fc=fc, kc=kc, b=b: T.matmul(
                                P[pm][:, fc * 2:fc * 2 + 2], wa[b][:, kc, j * 128:(j + 1) * 128], sc2[:, kc, :],
                                start=(kc == 0), stop=(kc == 7)))
                    S.group("pe", fns, reads=[rwa[b], rsc2], writes=[rP[pm]])
                S.op("dve", lambda: V.tensor_tensor(mod[:], P[pm][:, 0:96].rearrange("p (a b) -> p a b", b=2),
                                                    bada[:].unsqueeze(2).to_broadcast([128, 48, 2]), ALU.add),
                     reads=[rP[pm], rbada], writes=[rmod])
                for nidx, (scb, gname) in enumerate(((8, "n1g"), (32, "n2g"))):
                    S.op("dve", lambda nidx=nidx, scb=scb: V.tensor_scalar(
                        se[:, nidx, :, :], mod[:, scb:scb + 8, :], 1.0, float(np.sqrt(D)), ALU.add, ALU.mult),
                        reads=[rmod], writes=[rse])
                    gap = vecs[:, VOFF[gname] + l * 8: VOFF[gname] + l * 8 + 8].unsqueeze(2).to_broadcast([128, 8, 2])
                    S.op("dve", lambda nidx=nidx, gap=gap: V.tensor_tensor(se[:, nidx, :, :], se[:, nidx, :, :], gap, ALU.mult),
                         reads=[rse, rvecs], writes=[rse])
                for di, nm in enumerate(("lbf", "lbb")):
                    l0 = vecs[:, VOFF[nm]:VOFF[nm] + 4]
                    l1 = vecs[:, VOFF[nm] + 4:VOFF[nm] + 8]
                    if l == 0:
                        S.op("dve", lambda di=di: V.memset(lbt[:, di, :, 0], 0.0), writes=[rlbt])
                    else:
                        S.op("dve", lambda di=di, l0=l0, l1=l1: V.tensor_tensor(lbt[:, di, :, 0], l1, l0, ALU.subtract),
                             reads=[rvecs], writes=[rlbt])
                        S.op("act", lambda di=di: A.activation(lbt[:, di, :, 0], lbt[:, di, :, 0], AF.Sigmoid),
                             reads=[rlbt], writes=[rlbt])
                    S.op("dve", lambda di=di: V.tensor_scalar(lbt[:, di, :, 1], lbt[:, di, :, 0], -1.0, 1.0, ALU.mult, ALU.add),
                         reads=[rlbt], writes=[rlbt])
                    S.op("dve", lambda di=di: V.tensor_scalar(lbt[:, di, :, 2], lbt[:, di, :, 0], 1.0, -1.0, ALU.mult, ALU.add),
                         reads=[rlbt], writes=[rlbt])
                S.barrier()
            dump(f"mod{l}", mod[:].rearrange("p a b -> p (a b)"), [rmod])

            with ExitStack() as em:
                rym = S.grid(8, 5)
                with ExitStack() as en:
                    norm_mod(0, TT, es_=en)
                    S.barrier()
                dump(f"h{l}", hT[:].rearrange("p a b -> p (a b)"), rhT)
                if stop == "norm1":
                    break
                srcs = []
                wsl = lambda grp, sub: win_d[l, :, grp * 512 + sub * 128: grp * 512 + (sub + 1) * 128].rearrange(
                    "(kc p) f -> p kc f", p=128)
                wrow = lambda r0: wout_d[l, r0:r0 + 128, :].rearrange("p (dj f) -> p dj f", f=128)
                for h in range(4):
                    srcs += [wsl(3, h), wsl(4, h), wsl(2, h), wsl(5, h), wsl(2, h), wsl(6, h), wrow(512 + h * 128)]
                for j in range(4):
                    srcs += [wsl(0, j), wsl(1, j)]
                W = WChunks(em, "m", srcs, "act", nst=2, nbf=3)
                W.prefetch()
                wk = [0]

                def nextw():
                    k = wk[0]
                    wk[0] += 1
                    t_, r_ = W.get(k)
                    return k, t_, r_

                with ExitStack() as eh:
                    o_acc = sbuf(eh, "o_acc", [128, NT], F32)
                    Vh = sbuf(eh, "Vh", [128, 18, 128], BF16)
                    Vx = [sbuf(eh, f"Vx{i}", [128, 4, 128], BF16) for i in range(2)]
                    yh = sbuf(eh, "yh", [128, NT], BF16)
                    QT = sbuf(eh, "QT", [128, NT], BF16)
                    KT = sbuf(eh, "KT", [128, NT], BF16)
                    KHT = sbuf(eh, "KHT", [128, 18, 128], BF16)
                    dS = sbuf(eh, "dS", [128, NCHK, 128], BF16)
                    Dall = sbuf(eh, "Dall", [128, NCHK], F32)
                    Dpos = sbuf(eh, "Dpos", [128, NCHK], F32)
                    Wt = [[sbuf(eh, f"W{i}_{b_}", [128, 512], F32) for i in range(6)] for b_ in range(2)]
                    rWt = [[S.res() for i in range(6)] for b_ in range(2)]
                    W1 = Wt[0][0]
                    KHt = sbuf(eh, "KHt", [128, 512], BF16)
                    attm = [sbuf(eh, f"attm{i}", [128, 128], BF16) for i in range(2)]
                    ro_acc = S.grid(5)
                    rVh, rKHT, rdS, rDall, rDpos = (S.res() for _ in range(5))
                    rdSh = S.grid(2)
                    rQT, rKT, ryh = S.grid(5), S.grid(5), S.grid(5)
                    rKHt = S.res()
                    rW1 = rWt[0][0]
                    Wq, rWq = Wt[0][4], rWt[0][4]
                    sqh, rsqh, ro, rro = KHt, rKHt, W1, rW1
                    rattm, rVx = S.grid(2), S.grid(2)
                    if l == 0:
                        print("[kernel] SBUF bytes free inside HGRN scope:", nc.sbuf_bytes_remaining)
                    NXC = NX // CH
                    NCC = NCX // CH
                    CPT = 128 // CH
                    for h in range(4):
                        k, wt, rwt = nextw()
                        for g4 in range(5):
                            tts = range(g4 * 4, min(g4 * 4 + 4, 18))
                            p = nextp(0, 4)
                            fns = []
                            for tt in tts:
                                for kc in range(8):
                                    fns.append(lambda tt=tt, kc=kc, p=p: T.matmul(
                                        P[p][:, (tt % 4) * 128:(tt % 4 + 1) * 128], hT[:, kc, tt * 128:(tt + 1) * 128],
                                        wt[:, kc, :], start=(kc == 0), stop=(kc == 7)))
                            S.group("pe", fns, reads=[rwt, rhT[g4]], writes=[rP[p]])
                            nt_ = len(tts)
                            S.op("act", lambda p=p, g4=g4, nt_=nt_: A.copy(
                                Vh[:, g4 * 4:g4 * 4 + nt_, :], P[p][:, :nt_ * 128].rearrange("p (a b) -> p a b", b=128)),
                                reads=[rP[p]], writes=[rVh])
                        W.after(k)
                        ckpt("hg_v")
                        for di in range(2):
                            fwd = di == 0
                            kf, wf, rwf = nextw()
                            kq, wq, rwq = nextw()
                            lb_ap = lbt[:, di, h, 0:1]
                            oml_ap = lbt[:, di, h, 1:2]
                            noml_ap = lbt[:, di, h, 2:3]
                            def prep_a(ti):
                                t0, n = TT[ti]
                                nch, c0 = n // CH, t0 // CH
                                W1, W2, W3, W4, Wq, W5 = Wt[ti % 2]
                                rW1, rW2, rW3, rW4, rWq, rW5 = rWt[ti % 2]
                                v3 = lambda ap: ap[:, :n].rearrange("p (c k) -> p c k", k=CH)
                                pf = nextp(0, 4)
                                S.group("pe", [(lambda kc=kc, pf=pf: T.matmul(P[pf][:, :n], wf[:, kc, :], hT[:, kc, t0:t0 + n],
                                                                             start=(kc == 0), stop=(kc == 7))) for kc in range(8)],
                                        reads=[rwf, rhT[ti]], writes=[rP[pf]])
                                pq = nextp(0, 4)
                                S.group("pe", [(lambda kc=kc, pq=pq: T.matmul(P[pq][:, :n], wq[:, kc, :], hT[:, kc, t0:t0 + n],
                                                                             start=(kc == 0), stop=(kc == 7))) for kc in range(8)],
                                        reads=[rwq, rhT[ti]], writes=[rP[pq]])
                                S.op("act", lambda: A.activation(W1[:, :n], P[pf][:, :n], AF.Sigmoid), reads=[rP[pf]], writes=[rW1])
                                S.op("act", lambda: A.activation(Wq[:, :n], P[pq][:, :n], AF.Sigmoid), reads=[rP[pq]], writes=[rWq])
                                yield
                                S.op("act", lambda: A.activation(W2[:, :n], W1[:, :n], AF.Ln, bias=lb_ap, scale=oml_ap),
                                     reads=[rW1, rlbt], writes=[rW2])
                                S.op("dve", lambda: V.tensor_tensor(Wq[:, :n], P[pq][:, :n], Wq[:, :n], ALU.mult), reads=[rP[pq], rWq], writes=[rWq])
                                S.op("dve", lambda: V.tensor_scalar(W5[:, :n], W1[:, :n], noml_ap, oml_ap, ALU.mult, ALU.add),
                                     reads=[rW1, rlbt], writes=[rW5])
                                yield
                                S.op("dve", lambda: V.tensor_tensor_scan(W3[:, :n], smask[:, :n], W2[:, :n], 0.0, ALU.mult, ALU.add),
                                     reads=[rW2, rcst], writes=[rW3])
                                if fwd:
                                    bb, rbb = W3, rW3
                                    bl = v3(W3)[:, :, CH - 1]
                                else:
                                    tc_ = v3(W3)[:, :, CH - 1:CH].to_broadcast([128, nch, CH])
                                    S.op("dve", lambda: V.tensor_tensor(v3(W4), tc_, v3(W3), ALU.subtract), reads=[rW3], writes=[rW4])
                                    S.op("dve", lambda: V.tensor_tensor(W4[:, :n], W4[:, :n], W2[:, :n], ALU.add),
                                         reads=[rW4, rW2], writes=[rW4])
                                    bb, rbb = W4, rW4
                                    bl = v3(W4)[:, :, 0]
                                yield
                                S.op("act", lambda: A.activation(Dall[:, c0:c0 + nch], bl, AF.Exp), reads=[rbb], writes=[rDall])
                                yield
                                res_[ti] = (bb, rbb)

                            def prep_b(ti):
                                bb, rbb = res_[ti]
                                t0, n = TT[ti]
                                nch, c0 = n // CH, t0 // CH
                                W1, W2, W3, W4, Wq, W5 = Wt[ti % 2]
                                rW1, rW2, rW3, rW4, rWq, rW5 = rWt[ti % 2]
                                v3 = lambda ap: ap[:, :n].rearrange("p (c k) -> p c k", k=CH)
                                S.op("act", lambda: A.activation(W2[:, :n], bb[:, :n], AF.Exp), reads=[rbb, rW2], writes=[rW2])
                                S.op("act", lambda: A.activation(W1[:, :n], bb[:, :n], AF.Exp, scale=-1.0), reads=[rbb, rW1], writes=[rW1])
                                yield
                                S.op("dve", lambda: V.tensor_tensor(QT[:, t0:t0 + n], Wq[:, :n], W2[:, :n], ALU.mult),
                                     reads=[rWq, rW2], writes=[rQT[ti]])
                                S.op("dve", lambda: V.tensor_tensor(W5[:, :n], W5[:, :n], W1[:, :n], ALU.mult),
                                     reads=[rW5, rW1], writes=[rW5])
                                yield
                                S.op("act", lambda: A.copy(KT[:, t0:t0 + n], W5[:, :n]), reads=[rW5], writes=[rKT[ti]])
                                dbc = Dall[:, c0:c0 + nch].unsqueeze(2).to_broadcast([128, nch, CH])
                                S.op("dve", lambda: V.tensor_tensor(v3(KHt), v3(W5), dbc, ALU.mult), reads=[rW5, rDall], writes=[rKHt])
                                yield
                                pk = nextp(4, 6)
                                ntile = n // 128
                                pkb = P[pk][:].bitcast(BF16)
                                S.group("pe", [(lambda j=j: T.transpose(pkb[:, j * 128:(j + 1) * 128],
                                                                        KHt[:, j * 128:(j + 1) * 128], identb[:]))
                                               for j in range(ntile)], reads=[rKHt, rmisc], writes=[rP[pk]])
                                yield
                                S.op("act", lambda: A.copy(KHT[:, t0 // 128:t0 // 128 + ntile, :],
                                                           pkb[:, :ntile * 128].rearrange("p (a b) -> p a b", b=128)),
                                     reads=[rP[pk]], writes=[rKHT])
                                yield
                            res_ = {}

                            def run_zip(g1, g2):
                                gens = [g for g in (g1, g2) if g is not None]
                                while gens:
                                    for g in list(gens):
                                        try:
                                            next(g)
                                        except StopIteration:
                                            gens.remove(g)
                            run_zip(prep_a(0), None)
                            for ti in range(len(TT)):
                                run_zip(prep_a(ti + 1) if ti + 1 < len(TT) else None, prep_b(ti))
                            W.after(kq)
                            ckpt("hg_prep")
                            order = (list(range(NXC, NCHK)) + list(range(NXC))) if fwd else list(range(NCHK - 1, -1, -1))
                            pos = {c: i for i, c in enumerate(order)}
                            for tt in range(18):
                                vb = tt % 2
                                S.op("dve", lambda vb=vb, tt=tt: V.tensor_tensor(
                                    Vx[vb][:], Vh[:, tt, :].unsqueeze(1).to_broadcast([128, CPT, 128]),
                                    cst[:, 896:896 + CPT].unsqueeze(2).to_broadcast([128, CPT, 128]), ALU.mult),
                                    reads=[rVh, rcst], writes=[rVx[vb]])
                                p = nextp(6, 8)
                                S.group("pe", [lambda tt=tt, vb=vb, p=p: T.matmul(
                                    P[p][:, :CPT * 128], KHT[:, tt, :], Vx[vb][:].rearrange("p a b -> p (a b)"), start=True, stop=True)],
                                    reads=[rKHT, rVx[vb]], writes=[rP[p]])
                                p0 = pos[tt * CPT]
                                if fwd:
                                    dst = dS[:, p0:p0 + CPT, :]
                                else:
                                    dst = dS[:, p0:p0 - CPT:-1, :] if p0 - CPT >= 0 else dS[:, p0::-1, :]
                                S.op("act", lambda dst=dst, p=p: A.copy(dst, P[p][:, :CPT * 128].rearrange("p (c v) -> p c v", v=128)),
                                     reads=[rP[p]], writes=[rdS, rdSh[0], rdSh[1]])
                            ckpt("hg_ds")
                            if fwd:
                                S.op("dve", lambda: V.tensor_copy(Dpos[:, NCC:NCHK], Dall[:, 0:NXC]), reads=[rDall], writes=[rDpos])
                                S.op("dve", lambda: V.tensor_copy(Dpos[:, 1:NCC], Dall[:, NXC + 1:NCHK]), reads=[rDall], writes=[rDpos])
                            else:
                                S.op("dve", lambda: V.tensor_copy(Dpos[:, 1:NCHK], Dall[:, NCHK - 2::-1]), reads=[rDall], writes=[rDpos])
                            S.op("dve", lambda: V.memset(Dpos[:, 0:1], 0.0), reads=[rDpos], writes=[rDpos])
                            for pp in range(1, NCHK):
                                for hf in range(2):
                                    vs = slice(hf * 64, hf * 64 + 64)
                                    S.op("dve", lambda pp=pp, vs=vs: V.scalar_tensor_tensor(
                                        dS[:, pp, vs], dS[:, pp - 1, vs], Dpos[:, pp:pp + 1], dS[:, pp, vs], ALU.mult, ALU.add),
                                        reads=[rdSh[hf], rDpos], writes=[rdSh[hf]])
                            ckpt("hg_scan")
                            mask = maskF if fwd else maskB
                            for g4 in range(5):
                                tts = list(range(g4 * 4, min(g4 * 4 + 4, 18)))
                                po = nextp(4, 6)
                                for tt in tts:
                                    off = (tt % 4) * 128
                                    fns = []
                                    for c in range(tt * CPT, (tt + 1) * CPT):
                                        pc = pos[c]
                                        lhs = zerob[:] if pc == 0 else dS[:, pc - 1, :]
                                        co = off + (c % CPT) * CH
                                        fns.append(lambda c=c, lhs=lhs, co=co, po=po: T.matmul(
                                            P[po][:, co:co + CH], lhs, QT[:, c * CH:(c + 1) * CH], start=(c % CPT == 0), stop=False,
                                            skip_group_check=True))
                                    S.group("pe", fns, reads=[rdS, rdSh[0], rdSh[1], rQT[g4], rmisc], writes=[rP[po]])
                                    pa = nextp(6, 8)
                                    S.group("pe", [lambda tt=tt, pa=pa: T.matmul(P[pa][:, 0:128], KT[:, tt * 128:(tt + 1) * 128],
                                                                                QT[:, tt * 128:(tt + 1) * 128], start=True, stop=True)],
                                            reads=[rKT[g4], rQT[g4]], writes=[rP[pa]])
                                    ab = tt % 2
                                    S.op("dve", lambda pa=pa, ab=ab, mask=mask: V.tensor_tensor(attm[ab][:], P[pa][:, 0:128], mask, ALU.mult),
                                         reads=[rP[pa], rcst], writes=[rattm[ab]])
                                    S.group("pe", [lambda tt=tt, ab=ab, off=off, po=po: T.matmul(
                                        P[po][:, off:off + 128], Vh[:, tt, :], attm[ab][:], start=False, stop=True, skip_group_check=True)],
                                        reads=[rVh, rattm[ab]], writes=[rP[po]])
                                nn = len(tts) * 128
                                t0 = g4 * 512
                                if fwd:
                                    S.op("act", lambda po=po, nn=nn, t0=t0: A.copy(o_acc[:, t0:t0 + nn], P[po][:, :nn]),
                                         reads=[rP[po]], writes=[ro_acc[g4]])
                                else:
                                    S.op("dve", lambda po=po, nn=nn, t0=t0: V.tensor_tensor(o_acc[:, t0:t0 + nn], o_acc[:, t0:t0 + nn],
                                                                                          P[po][:, :nn], ALU.add),
                                         reads=[rP[po], ro_acc[g4]], writes=[ro_acc[g4]])
                            if stop == "hg_o" + str(di):
                                dump("QT", QT[:], rQT); dump("KT", KT[:], rKT); dump("dS", dS[:].rearrange("p c v -> p (c v)"), [rdS])
                                dump("Dall", Dall[:], [rDall]); dump("Dpos", Dpos[:], [rDpos]); dump("oacc0_0", o_acc[:], ro_acc)
                            ckpt("hg_o" + str(di))
                        if f"oacc{l}_{h}" in dbg_d:
                            dump(f"oacc{l}_{h}", o_acc[:], ro_acc)
                        W1, rW1, Wq, rWq = Wt[0][0], rWt[0][0], Wt[0][4], rWt[0][4]
                        ro, rro = W1, rW1
                        ko, wo_, rwo = nextw()
                        kr, wr_, rwr = nextw()
                        gh = vv("hg", l)
                        for ti, (t0, n) in enumerate(TO):
                            S.op("act", lambda: A.activation(sqh[:, :n], o_acc[:, t0:t0 + n], AF.Square), reads=[ro_acc[ti]], writes=[rsqh])
                            p = nextp(0, 4)
                            S.group("pe", [lambda p=p: T.matmul(P[p][:, :n], onesb[:], sqh[:, :n], start=True, stop=True)],
                                    reads=[rsqh, rmisc], writes=[rP[p]])
                            S.op("act", lambda p=p: A.activation(ro[:, :n], P[p][:, :n], AF.Ln, bias=epsD[:, 1:2], scale=1.0 / 128),
                                 reads=[rP[p], rmisc], writes=[rro])
                            S.op("act", lambda: A.activation(ro[:, :n], ro[:, :n], AF.Exp, scale=-0.5), reads=[rro], writes=[rro])
                            S.op("dve", lambda: V.tensor_tensor(ro[:, :n], ro[:, :n], o_acc[:, t0:t0 + n], ALU.mult),
                                 reads=[rro, ro_acc[ti]], writes=[rro])
                            p2 = nextp(0, 4)
                            S.group("pe", [(lambda kc=kc, p2=p2: T.matmul(P[p2][:, :n], wo_[:, kc, :], hT[:, kc, t0:t0 + n],
                                                                         start=(kc == 0), stop=(kc == 7))) for kc in range(8)],
                                    reads=[rwo, rhT[ti]], writes=[rP[p2]])
                            S.op("act", lambda p2=p2: A.activation(Wq[:, :n], P[p2][:, :n], AF.Silu), reads=[rP[p2]], writes=[rWq])
                            S.op("dve", lambda: V.scalar_tensor_tensor(yh[:, t0:t0 + n], ro[:, :n], gh, Wq[:, :n], ALU.mult, ALU.mult),
                                 reads=[rro, rWq, rvecs], writes=[ryh[ti]])
                            col = 0 if t0 < NX else 1
                            for dj in range(8):
                                p3 = nextp(4, 8)
                                S.group("pe", [lambda p3=p3, dj=dj: T.matmul(P[p3][:, :n], wr_[:, dj, :], yh[:, t0:t0 + n], start=True, stop=True)],
                                        reads=[rwr, ryh[ti]], writes=[rP[p3]])
                                g1 = mod[:, 16 + dj, col:col + 1]
                                S.op("dve", lambda p3=p3, g1=g1, dj=dj: V.scalar_tensor_tensor(
                                    xT[:, dj, t0:t0 + n], P[p3][:, :n], g1, xT[:, dj, t0:t0 + n], ALU.mult, ALU.add),
                                    reads=[rP[p3], rmod, rxT[dj][ti]], writes=[rxT[dj][ti]])
                        if f"yh{l}_{h}" in dbg_d:
                            dump(f"yh{l}_{h}", yh[:], ryh)
                        W.after(kr)
                    S.barrier()
                if stop == "hgrn":
                    break
                ecm = ExitStack()
                em.enter_context(ecm)
                ymc = sbuf(ecm, "ymc", [128, 4, NT], BF16)
                with ExitStack() as ec:
                    rowo = (l % 2 == 0)
                    conv_tiles = TT if not last else TT[:4]
                    UPL = (32 * 79 + 31) if rowo else (62 * 64)
                    Upad = [sbuf(ec, f"Upad{i}", [128, UPL + 286], BF16) for i in range(2)]
                    Dg = [sbuf(ec, f"Dg{i}", [128, 31, 128], BF16) for i in range(2)]
                    sgt = [sbuf(ec, f"sgt{i}", [128, 512], F32) for i in range(2)]
                    cwb = sbuf(ec, "cwb", [128, 4 * 31], BF16)
                    rUp, rDg, rsgt = S.grid(2), S.grid(2), S.grid(2)
                    rcwb = S.res()
                    for i in range(2):
                        S.op("pool", lambda i=i: G.memset(Upad[i][:], 0.0), writes=[rUp[i]])
                    S.op("dve", lambda: V.tensor_copy(cwb[:], vecs[:, VOFF["convw"] + l * 124: VOFF["convw"] + (l + 1) * 124]),
                         reads=[rvecs], writes=[rcwb])

                    def uview(b, ti, k):
                        t0, n = TT[ti]
                        if t0 >= NX:
                            return Upad[b][:, UPL + k: UPL + k + NCX]
                        r0 = t0 // 64
                        if rowo:
                            return Upad[b][:, r0 * 79 + k: r0 * 79 + k + 8 * 79].rearrange("p (r c) -> p r c", c=79)[:, :, 0:64]
                        return Upad[b][:, (r0 + k) * 64: (r0 + k) * 64 + 512]

                    def pview(p, ti):
                        t0, n = TT[ti]
                        if t0 < NX and rowo:
                            return P[p][:, :n].rearrange("p (r c) -> p r c", c=64)
                        return P[p][:, :n]

                    def emit_glu(j):
                        b = j % 2
                        ka, wa_, rwa_ = nextw()
                        kg, wg_, rwg_ = nextw()
                        for ti in range(len(conv_tiles)):
                            t0, n = TT[ti]
                            pa = nextp(0, 4)
                            S.group("pe", [(lambda kc=kc, pa=pa: T.matmul(P[pa][:, :n], wa_[:, kc, :], hT[:, kc, t0:t0 + n],
                                                                         start=(kc == 0), stop=(kc == 7))) for kc in range(8)],
                                    reads=[rwa_, rhT[ti]], writes=[rP[pa]])
                            pg = nextp(0, 4)
                            S.group("pe", [(lambda kc=kc, pg=pg: T.matmul(P[pg][:, :n], wg_[:, kc, :], hT[:, kc, t0:t0 + n],
                                                                         start=(kc == 0), stop=(kc == 7))) for kc in range(8)],
                                    reads=[rwg_, rhT[ti]], writes=[rP[pg]])
                            sb_ = ti % 2
                            S.op("act", lambda pg=pg, sb_=sb_: A.activation(sgt[sb_][:, :n], P[pg][:, :n], AF.Sigmoid),
                                 reads=[rP[pg]], writes=[rsgt[sb_]])
                            sv = sgt[sb_][:, :n]
                            if t0 < NX and rowo:
                                sv = sv.rearrange("p (r c) -> p r c", c=64)
                            S.op("dve", lambda pa=pa, sv=sv, ti=ti, b=b: V.tensor_tensor(uview(b, ti, 15), pview(pa, ti), sv, ALU.mult),
                                 reads=[rP[pa], rsgt[sb_]], writes=[rUp[b]])
                        W.after(kg)
                        S.op("dve", lambda b=b, j=j: V.tensor_tensor(
                            Dg[b][:], identb[:].unsqueeze(1).to_broadcast([128, 31, 128]),
                            cwb[:, j * 31:(j + 1) * 31].unsqueeze(2).to_broadcast([128, 31, 128]), ALU.mult),
                            reads=[rmisc, rcwb], writes=[rDg[b]])

                    def emit_conv(j):
                        b = j % 2
                        cb = vv("convb", l * 4 + j)
                        for ti in range(len(conv_tiles)):
                            t0, n = TT[ti]
                            p = nextp(4, 8)
                            S.group("pe", [(lambda k_=k_, p=p, ti=ti, b=b: T.matmul(pview(p, ti), Dg[b][:, k_, :], uview(b, ti, k_),
                                                                                 start=(k_ == 0), stop=(k_ == 30))) for k_ in range(31)],
                                    reads=[rDg[b], rUp[b]], writes=[rP[p]])
                            S.op("act", lambda p=p, j=j, cb=cb: A.activation(ymc[:, j, t0:t0 + n], P[p][:, :n], AF.Identity, bias=cb, scale=1.0),
                                 reads=[rP[p], rvecs], writes=[rym[j][ti]])
                    emit_glu(0)
                    for j in range(4):
                        if j + 1 < 4:
                            emit_glu(j + 1)
                        emit_conv(j)
                    S.barrier()
                with ExitStack() as eln:
                    sq4 = sbuf(eln, "sq4", [128, 4, 512], BF16)
                    mu = sbuf(eln, "mu", [128, 512], F32)
                    msq = sbuf(eln, "msq", [128, 512], F32)
                    rsd = sbuf(eln, "rsd", [128, 512], F32)
                    lt = [sbuf(eln, f"lt{i}", [128, 512], F32) for i in range(2)]
                    rsq4, rmu, rmsq, rrsd = (S.res() for _ in range(4))
                    rlt = S.grid(2)
                    for ti, (t0, n) in enumerate(TO):
                        for j in range(4):
                            S.op("act", lambda j=j: A.activation(sq4[:, j, :n], ymc[:, j, t0:t0 + n], AF.Square),
                                 reads=[rym[j][ti]], writes=[rsq4])
                        p1 = nextp(0, 4)
                        S.group("pe", [(lambda j=j, p1=p1: T.matmul(P[p1][:, :n], onesb[:], ymc[:, j, t0:t0 + n], start=(j == 0), stop=(j == 3)))
                                       for j in range(4)], reads=[rym[j][ti] for j in range(4)] + [rmisc], writes=[rP[p1]])
                        p2 = nextp(0, 4)
                        S.group("pe", [(lambda j=j, p2=p2: T.matmul(P[p2][:, :n], onesb[:], sq4[:, j, :n], start=(j == 0), stop=(j == 3)))
                                       for j in range(4)], reads=[rsq4, rmisc], writes=[rP[p2]])
                        S.op("act", lambda p1=p1: A.mul(mu[:, :n], P[p1][:, :n], 1.0 / 512), reads=[rP[p1]], writes=[rmu])
                        S.op("dve", lambda: V.tensor_tensor(msq[:, :n], mu[:, :n], mu[:, :n], ALU.mult), reads=[rmu], writes=[rmsq])
                        S.op("dve", lambda p2=p2: V.scalar_tensor_tensor(rsd[:, :n], P[p2][:, :n], 1.0 / 512, msq[:, :n], ALU.mult, ALU.subtract),
                             reads=[rP[p2], rmsq], writes=[rrsd])
                        S.op("act", lambda: A.activation(rsd[:, :n], rsd[:, :n], AF.Ln, bias=epsD[:, 1:2], scale=1.0),
                             reads=[rrsd, rmisc], writes=[rrsd])
                        S.op("act", lambda: A.activation(rsd[:, :n], rsd[:, :n], AF.Exp, scale=-0.5), reads=[rrsd], writes=[rrsd])
                        for j in range(4):
                            b = j % 2
                            S.op("dve", lambda j=j, b=b: V.tensor_tensor(lt[b][:, :n], ymc[:, j, t0:t0 + n], mu[:, :n], ALU.subtract),
                                 reads=[rym[j][ti], rmu], writes=[rlt[b]])
                            S.op("dve", lambda b=b: V.tensor_tensor(lt[b][:, :n], lt[b][:, :n], rsd[:, :n], ALU.mult),
                                 reads=[rlt[b], rrsd], writes=[rlt[b]])
                            S.op("act", lambda j=j, b=b: A.activation(ymc[:, j, t0:t0 + n], lt[b][:, :n], AF.Silu,
                                                                      bias=vv("lnb", l * 4 + j), scale=vv("lng", l * 4 + j)),
                                 reads=[rlt[b], rvecs], writes=[rym[j][ti]])
                    S.barrier()
                dump(f"ymc{l}", ymc[:].rearrange("p a b -> p (a b)"), [r for rr in rym[:4] for r in rr])
                if stop == "conv":
                    break
                W2 = WChunks(ecm, "o", [wrow(j * 128) for j in range(4)], "act", nst=2, nbf=4)
                W2.prefetch()
                wo4 = [W2.get(j) for j in range(4)]
                for dj in range(8):
                    for ti, (t0, n) in enumerate(TO):
                        col = 0 if t0 < NX else 1
                        p = nextp(0, 4)
                        S.group("pe", [(lambda j=j, p=p, dj=dj: T.matmul(P[p][:, :n], wo4[j][0][:, dj, :], ymc[:, j, t0:t0 + n],
                                                                        start=(j == 0), stop=(j == 3))) for j in range(4)],
                                reads=[w_[1] for w_ in wo4] + [rym[j][ti] for j in range(4)], writes=[rP[p]])
                        g1 = mod[:, 16 + dj, col:col + 1]
                        S.op("dve", lambda p=p, g1=g1, dj=dj: V.scalar_tensor_tensor(
                            xT[:, dj, t0:t0 + n], P[p][:, :n], g1, xT[:, dj, t0:t0 + n], ALU.mult, ALU.add),
                            reads=[rP[p], rmod, rxT[dj][ti]], writes=[rxT[dj][ti]])
                S.barrier()
            dump(f"xmix{l}", xT[:].rearrange("p a b -> p (a b)"), [r for rr in rxT for r in rr])
            if stop == "mix":
                break

            with ExitStack() as eo:
                comb = sbuf(eo, "comb", [128, 18, NE], F32)
                rcomb = S.res()
                with ExitStack() as er:
                    h2f = sbuf(er, "h2f", [128, 8, 512], F32)
                    sq = sbuf(er, "sq2", [128, 8, 512], BF16)
                    rstd = sbuf(er, "rstd2", [128, 512], F32)
                    tmp = [sbuf(er, f"nt2_{i}", [128, 512], F32) for i in range(2)]
                    rsq, rrstd = S.res(), S.res()
                    rtmp = S.grid(2)
                    rh2 = S.res()
                    pl = 7
                    for ti, (t0, n) in enumerate(TO):
                        if True:
                            col = 0 if t0 < NX else 1
                            for c in range(8):
                                S.op("act", lambda c=c: A.activation(sq[:, c, :n], xT[:, c, t0:t0 + n], AF.Square),
                                     reads=[rxT[c][ti]], writes=[rsq])
                            p = nextp(0, 4)
                            S.group("pe", [(lambda c=c, p=p: T.matmul(P[p][:, :n], onesb[:], sq[:, c, :n], start=(c == 0), stop=(c == 7)))
                                           for c in range(8)], reads=[rsq, rmisc], writes=[rP[p]])
                            S.op("act", lambda p=p: A.activation(rstd[:, :n], P[p][:, :n], AF.Ln, bias=epsD[:, 0:1], scale=1.0),
                                 reads=[rP[p], rmisc], writes=[rrstd])
                            S.op("act", lambda: A.activation(rstd[:, :n], rstd[:, :n], AF.Exp, scale=-0.5), reads=[rrstd], writes=[rrstd])
                            for c in range(8):
                                b = c % 2
                                S.op("dve", lambda c=c, b=b: V.tensor_tensor(tmp[b][:, :n], xT[:, c, t0:t0 + n], rstd[:, :n], ALU.mult),
                                     reads=[rxT[c][ti], rrstd], writes=[rtmp[b]])
                                sc_ap = se[:, 1, c, col:col + 1]
                                sh_ap = mod[:, 24 + c, col:col + 1]
                                S.op("act", lambda c=c, b=b, sc_ap=sc_ap, sh_ap=sh_ap: A.activation(
                                    h2f[:, c, :n], tmp[b][:, :n], AF.Identity, bias=sh_ap, scale=sc_ap),
                                    reads=[rtmp[b], rse, rmod], writes=[rh2])
                                if False:
                                    S.op("pool", lambda c=c: G.tensor_copy(hT[:, c, t0:t0 + n], h2f[:, c, :n]),
                                         reads=[rh2], writes=[rhT[ti]])
                                else:
                                    S.op("act", lambda c=c, b=b, sc_ap=sc_ap, sh_ap=sh_ap: A.activation(
                                        hT[:, c, t0:t0 + n], tmp[b][:, :n], AF.Identity, bias=sh_ap, scale=sc_ap),
                                        reads=[rtmp[b], rse, rmod], writes=[rhT[ti]])
                            fns = []
                            for s_ in range(n // 128):
                                tt = t0 // 128 + s_
                                for kc in range(8):
                                    fns.append(lambda s_=s_, tt=tt, kc=kc: T.matmul(
                                        P[pl][:, tt * NE:(tt + 1) * NE], h2f[:, kc, s_ * 128:(s_ + 1) * 128], rw[:, kc, :],
                                        start=(kc == 0), stop=(kc == 7)))
                            S.group("pe", fns, reads=[rh2, rrw], writes=[rP[pl]])
                    ntl = sum(n for _, n in TO) // 128
                    NG = ntl * 4
                    sg_ = sbuf(er, "r_s", [128, ntl, NE], F32)
                    sbb = sbuf(er, "r_sb", [128, ntl, NE], F32)
                    ps6 = sbuf(er, "r_p6", [128, NG, 6], F32)
                    gs = sbuf(er, "r_gs", [128, NG], F32)
                    gm = sbuf(er, "r_gm", [128, ntl], F32)
                    ing = sbuf(er, "r_ing", [128, NG], F32)
                    sbm = sbuf(er, "r_sbm", [128, ntl, NE], F32)
                    sel = sbuf(er, "r_sel", [128, ntl, NE], F32)
                    m1 = sbuf(er, "r_m1", [128, ntl], F32)
                    rr_ = S.res()
                    R = dict(reads=[rr_], writes=[rr_])
                    S.op("act", lambda: A.activation(sg_[:].rearrange("p a b -> p (a b)"), P[pl][:, :ntl * NE], AF.Sigmoid),
                         reads=[rP[pl]], writes=[rr_])
                    rb_bc = vecs[:, VOFF["rbias"]:VOFF["rbias"] + NE].unsqueeze(1).to_broadcast([128, ntl, NE])
                    S.op("dve", lambda: V.tensor_tensor(sbb[:], sg_[:], rb_bc, ALU.add), reads=[rr_, rvecs], writes=[rr_])
                    g4v = sbb[:].rearrange("p a (g e) -> p (a g) e", e=4)
                    S.op("dve", lambda: V.tensor_tensor(ps6[:, :, 0:2], g4v[:, :, 0:4:2], g4v[:, :, 1:4:2], ALU.add), **R)
                    S.op("dve", lambda: V.tensor_tensor(ps6[:, :, 2:4], g4v[:, :, 0:2], g4v[:, :, 2:4], ALU.add), **R)
                    S.op("dve", lambda: V.tensor_tensor(ps6[:, :, 4:5], g4v[:, :, 0:1], g4v[:, :, 3:4], ALU.add), **R)
                    S.op("dve", lambda: V.tensor_tensor(ps6[:, :, 5:6], g4v[:, :, 1:2], g4v[:, :, 2:3], ALU.add), **R)
                    S.op("dve", lambda: V.tensor_reduce(gs[:], ps6[:], AX.X, ALU.max), **R)
                    S.op("dve", lambda: V.tensor_reduce(gm[:], gs[:].rearrange("p (a g) -> p a g", g=4), AX.X, ALU.max), **R)
                    S.op("dve", lambda: V.tensor_tensor(ing[:].rearrange("p (a g) -> p a g", g=4), gs[:].rearrange("p (a g) -> p a g", g=4),
                                                        gm[:].unsqueeze(2).to_broadcast([128, ntl, 4]), ALU.is_equal), **R)
                    S.op("dve", lambda: V.tensor_scalar(ing[:], ing[:], BIG, -BIG, ALU.mult, ALU.add), **R)
                    S.op("dve", lambda: V.tensor_tensor(sbm[:].rearrange("p a (g e) -> p (a g) e", e=4), g4v,
                                                        ing[:].unsqueeze(2).to_broadcast([128, NG, 4]), ALU.add), **R)
                    S.op("dve", lambda: V.tensor_reduce(m1[:], sbm[:], AX.X, ALU.max), **R)
                    S.op("dve", lambda: V.tensor_tensor(sel[:], sbm[:], m1[:].unsqueeze(2).to_broadcast([128, ntl, NE]), ALU.is_equal), **R)
                    S.op("dve", lambda: V.scalar_tensor_tensor(sbm[:], sel[:], -BIG, sbm[:], ALU.mult, ALU.add), **R)
                    S.op("dve", lambda: V.tensor_reduce(m1[:], sbm[:], AX.X, ALU.max), **R)
                    S.op("dve", lambda: V.tensor_tensor(sbm[:], sbm[:], m1[:].unsqueeze(2).to_broadcast([128, ntl, NE]), ALU.is_ge), **R)
                    S.op("dve", lambda: V.tensor_tensor(sel[:], sel[:], sbm[:], ALU.add), **R)
                    S.op("dve", lambda: V.tensor_tensor(sel[:], sel[:], sg_[:], ALU.mult), **R)
                    S.op("dve", lambda: V.tensor_reduce(m1[:], sel[:], AX.X, ALU.add), **R)
                    S.op("dve", lambda: V.reciprocal(m1[:], m1[:]), **R)
                    S.op("dve", lambda: V.tensor_tensor(comb[:, :ntl, :], sel[:], m1[:].unsqueeze(2).to_broadcast([128, ntl, NE]), ALU.mult),
                         reads=[rr_], writes=[rcomb])
                    S.barrier()
                comb_hi = sbuf(eo, "comb_hi", [128, 18, NE], BF16)
                comb_lo = sbuf(eo, "comb_lo", [128, 18, NE], BF16)
                comb_r = sbuf(eo, "comb_r", [128, 18, NE], F32)
                S.op("dve", lambda: V.tensor_copy(comb_hi[:], comb[:]), reads=[rcomb], writes=[rcomb])
                S.op("dve", lambda: V.tensor_tensor(comb_r[:], comb[:], comb_hi[:], ALU.subtract), reads=[rcomb], writes=[rcomb])
                S.op("dve", lambda: V.tensor_copy(comb_lo[:], comb_r[:]), reads=[rcomb], writes=[rcomb])
                dump(f"comb{l}", comb[:].rearrange("p a b -> p (a b)"), [rcomb])
                dump(f"h2_{l}", hT[:].rearrange("p a b -> p (a b)"), rhT)
                if stop == "router":
                    break
                NU = NE * 4
                NST, NBF = 4, 2
                stg = [sbuf(eo, f"stg{i}", [128, 2048], F32) for i in range(NST)]
                wbf = [sbuf(eo, f"wbf{i}", [128, 3, 2048], BF16) for i in range(NBF)]
                cg = sbuf(eo, "cg", [128, NT], F32)
                actb = [sbuf(eo, f"actb{i}", [128, 2, 512], BF16) for i in range(2)]
                sgm = [sbuf(eo, f"sgm{i}", [128, 512], F32) for i in range(2)]
                t1m = [sbuf(eo, f"t1m{i}", [128, 512], F32) for i in range(2)]
                rstg = S.grid(NST)
                rwbf = S.grid(NBF, 3)
                rcg = S.res()
                ractb, rsgm, rt1m = S.grid(2), S.grid(2), S.grid(2)
                dstg = [S.dsem() for _ in range(NST)]
                pieces = []
                for e in range(NE):
                    for q in range(4):
                        f0 = q * 256
                        pieces.append((wg_d[l, e, :, f0:f0 + 256].rearrange("(kc p) f -> p kc f", p=128), 0))
                        pieces.append((wu_d[l, e, :, f0:f0 + 256].rearrange("(kc p) f -> p kc f", p=128), 1))
                        pieces.append((wd_d[l, e, f0:f0 + 256, :].rearrange("(fc p) d -> p fc d", p=128), 2))
                pl_, pc_ = [0], [0]

                def p_load():
                    if pl_[0] >= len(pieces):
                        return
                    i = pl_[0] % NST
                    src, kind = pieces[pl_[0]]
                    dst = stg[i][:].rearrange("p (a b) -> p a b", a=(8 if kind < 2 else 2))
                    S.dma("sp", dst, src, dstg[i], writes=[rstg[i]])
                    pl_[0] += 1

                def p_cast():
                    if pc_[0] >= len(pieces):
                        return
                    k = pc_[0]
                    i = k % NST
                    u, kind = k // 3, k % 3
                    j = u % NBF
                    S.op("act", lambda i=i, j=j, kind=kind: A.copy(wbf[j][:, kind, :], stg[i][:]),
                         reads=[rstg[i]], writes=[rwbf[j][kind]])
                    pc_[0] += 1
                    p_load()
                for _ in range(NST):
                    p_load()
                for _ in range(3):
                    p_cast()
                def emit_cg(e):
                    for g4 in range((ntl + 3) // 4):
                        tts = list(range(g4 * 4, min(g4 * 4 + 4, ntl)))
                        p = 7
                        fns = []
                        for tt in tts:
                            for hl, cb_ in enumerate((comb_hi, comb_lo)):
                                fns.append(lambda tt=tt, hl=hl, cb_=cb_: T.matmul(
                                    P[p][:, (tt % 4) * 128:(tt % 4 + 1) * 128],
                                    cb_[:, tt, e:e + 1].to_broadcast([128, 128]), identb[:], start=(hl == 0), stop=(hl == 1)))
                        S.group("pe", fns, reads=[rcomb, rmisc], writes=[rP[p]])
                        nn = len(tts) * 128
                        S.op("dve", lambda p=p, g4=g4, nn=nn: V.tensor_copy(cg[:, g4 * 512:g4 * 512 + nn], P[p][:, :nn]),
                             reads=[rP[p]], writes=[rcg])

                def emit_gu(u, ti, ab):
                    e, q = u // 4, u % 4
                    j = u % NBF
                    t0, n = TO[ti]
                    wgt = wbf[j][:, 0, :].rearrange("p (kc f) -> p kc f", kc=8)
                    wut = wbf[j][:, 1, :].rearrange("p (kc f) -> p kc f", kc=8)
                    if q == 0 and ti == 0:
                        emit_cg(e)
                    for fc in range(2):
                        pg = nextp(0, 4)
                        S.group("pe", [(lambda kc=kc, pg=pg, fc=fc: T.matmul(P[pg][:, :n], wgt[:, kc, fc * 128:(fc + 1) * 128],
                                                                           hT[:, kc, t0:t0 + n], start=(kc == 0), stop=(kc == 7)))
                                       for kc in range(8)], reads=[rwbf[j][0], rhT[ti]], writes=[rP[pg]])
                        pu = nextp(0, 4)
                        S.group("pe", [(lambda kc=kc, pu=pu, fc=fc: T.matmul(P[pu][:, :n], wut[:, kc, fc * 128:(fc + 1) * 128],
                                                                           hT[:, kc, t0:t0 + n], start=(kc == 0), stop=(kc == 7)))
                                       for kc in range(8)], reads=[rwbf[j][1], rhT[ti]], writes=[rP[pu]])
                        S.op("act", lambda pg=pg, fc=fc: A.activation(sgm[fc][:, :n], P[pg][:, :n], AF.Silu),
                             reads=[rP[pg]], writes=[rsgm[fc]])
                        S.op("dve", lambda pu=pu, fc=fc: V.tensor_tensor(t1m[fc][:, :n], P[pu][:, :n], sgm[fc][:, :n], ALU.mult),
                             reads=[rP[pu], rsgm[fc]], writes=[rt1m[fc]])
                        S.op("pool", lambda fc=fc, ab=ab: G.tensor_tensor(actb[ab][:, fc, :n], t1m[fc][:, :n], cg[:, t0:t0 + n], ALU.mult),
                             reads=[rt1m[fc], rcg], writes=[ractb[ab]])

                def emit_dn(u, ti, ab):
                    j = u % NBF
                    t0, n = TO[ti]
                    col = 0 if t0 < NX else 1
                    wdt = wbf[j][:, 2, :].rearrange("p (fc d) -> p fc d", fc=2)
                    for dj in range(8):
                        po = nextp(4, 7)
                        S.group("pe", [(lambda fc=fc, po=po, dj=dj, ab=ab: T.matmul(
                            P[po][:, :n], wdt[:, fc, dj * 128:(dj + 1) * 128], actb[ab][:, fc, :n], start=(fc == 0), stop=(fc == 1)))
                            for fc in range(2)], reads=[rwbf[j][2], ractb[ab]], writes=[rP[po]])
                        g2 = mod[:, 40 + dj, col:col + 1]
                        S.op("dve", lambda po=po, g2=g2, dj=dj: V.scalar_tensor_tensor(
                            xT[:, dj, t0:t0 + n], P[po][:, :n], g2, xT[:, dj, t0:t0 + n], ALU.mult, ALU.add),
                            reads=[rP[po], rmod, rxT[dj][ti]], writes=[rxT[dj][ti]])

                items = [(u, ti) for u in range(NU) for ti in range(len(TO))]
                prev = None
                for k_, (u, ti) in enumerate(items):
                    emit_gu(u, ti, k_ % 2)
                    if not MOE_PIPE:
                        emit_dn(u, ti, k_ % 2)
                    elif prev is not None:
                        emit_dn(*prev)
                    if ti == 0:
                        for _ in range(3):
                            p_cast()
                    prev = (u, ti, k_ % 2)
                if MOE_PIPE:
                    emit_dn(*prev)
                S.barrier()
            dump(f"xout{l}", xT[:].rearrange("p a b -> p (a b)"), [r for rr in rxT for r in rr])

        if stop is None:
            with ExitStack() as ef:
                sq = sbuf(ef, "sqf", [128, 8, 512], BF16)
                rstd = sbuf(ef, "rstdf", [128, 512], F32)
                tmp = [sbuf(ef, f"ntf{i}", [128, 512], F32) for i in range(2)]
                yT = sbuf(ef, "yT", [128, 8, 512], F32)
                yo = [sbuf(ef, f"yo{i}", [128, D], F32) for i in range(2)]
                gfs = sbuf(ef, "gfs", [128, 8], F32)
                rsq, rrstd, ryT, rgfs = (S.res() for _ in range(4))
                rtmp, ryo = S.grid(2), S.grid(2)
                dyo = [S.dsem(), S.dsem()]
                S.op("dve", lambda: V.tensor_scalar(gfs[:], vecs[:, VOFF["fing"]:VOFF["fing"] + 8], float(np.sqrt(D)), None, ALU.mult),
                     reads=[rvecs], writes=[rgfs])
                for ti, (t0, n) in enumerate(TT[:4]):
                    for c in range(8):
                        S.op("act", lambda c=c: A.activation(sq[:, c, :], xT[:, c, t0:t0 + n], AF.Square), reads=[rxT[c][ti]], writes=[rsq])
                    p = nextp(0, 4)
                    S.group("pe", [(lambda c=c, p=p: T.matmul(P[p][:], onesb[:], sq[:, c, :], start=(c == 0), stop=(c == 7)))
                                   for c in range(8)], reads=[rsq, rmisc], writes=[rP[p]])
                    S.op("act", lambda p=p: A.activation(rstd[:], P[p][:], AF.Ln, bias=epsD[:, 0:1], scale=1.0),
                         reads=[rP[p], rmisc], writes=[rrstd])
                    S.op("act", lambda: A.activation(rstd[:], rstd[:], AF.Exp, scale=-0.5), reads=[rrstd], writes=[rrstd])
                    for c in range(8):
                        b = c % 2
                        S.op("dve", lambda c=c, b=b: V.tensor_tensor(tmp[b][:], xT[:, c, t0:t0 + n], rstd[:], ALU.mult),
                             reads=[rxT[c][ti], rrstd], writes=[rtmp[b]])
                        S.op("act", lambda c=c, b=b: A.activation(yT[:, c, :], tmp[b][:], AF.Copy, scale=gfs[:, c:c + 1]),
                             reads=[rtmp[b], rgfs], writes=[ryT])
                    for j in range(4):
                        tt = ti * 4 + j
                        b = tt % 2
                        for half in range(2):
                            pp = nextp(4, 8)
                            S.group("pe", [(lambda c=c, pp=pp, j=j: T.transpose(P[pp][:, (c % 4) * 128:(c % 4 + 1) * 128],
                                                                               yT[:, c, j * 128:(j + 1) * 128], ident))
                                           for c in range(half * 4, half * 4 + 4)], reads=[ryT, rcst], writes=[rP[pp]])
                            if half == 0:
                                S.op("act", lambda pp=pp, b=b: A.copy(yo[b][:, 0:512], P[pp][:]), reads=[rP[pp]], writes=[ryo[b]])
                            else:
                                S.op("dve", lambda pp=pp, b=b: V.tensor_copy(yo[b][:, 512:1024], P[pp][:]), reads=[rP[pp]], writes=[ryo[b]])
                        S.dma("sp", y_d[tt * 128:(tt + 1) * 128, :], yo[b][:], dyo[b], reads=[ryo[b]])
                S.barrier()
        S.barrier()
    return nc


def _chunked(v, n):
    return np.ascontiguousarray(np.asarray(v, np.float32).reshape(n, 128).T)


def make_consts():
    c = np.zeros((128, NCST), np.float32)
    c[:, 0:128] = np.eye(128, dtype=np.float32)
    s = np.arange(128)[:, None]
    t = np.arange(128)[None, :]
    same = (s // CH) == (t // CH)
    c[:, 128:256] = (same & (s <= t)).astype(np.float32)
    c[:, 256:384] = (same & (s >= t)).astype(np.float32)
    m = np.ones(512, np.float32)
    m[::CH] = 0.0
    c[:, 384:896] = m[None, :]
    for i in range(4):
        c[32 * i:32 * i + 32, 896 + i] = 1.0
    return c


def make_in_maps(inputs, cores):
    f = lambda k: np.asarray(inputs[k], np.float32)
    vecs = np.zeros((128, NV), np.float32)

    def put(name, arr):
        arr = np.asarray(arr, np.float32)
        vecs[:, VOFF[name]:VOFF[name] + arr.shape[1]] = arr
    put("n1g", np.concatenate([_chunked(f("norm1_g")[l], 8) for l in range(DEPTH)], 1))
    put("n2g", np.concatenate([_chunked(f("norm2_g")[l], 8) for l in range(DEPTH)], 1))
    put("fing", _chunked(f("final_norm_g"), 8))
    put("convb", np.concatenate([_chunked(f("conv_b")[l], 4) for l in range(DEPTH)], 1))
    put("lng", np.concatenate([_chunked(f("conv_ln_g")[l], 4) for l in range(DEPTH)], 1))
    put("lnb", np.concatenate([_chunked(f("conv_ln_b")[l], 4) for l in range(DEPTH)], 1))
    put("hg", np.stack([f("hgrn_norm_g")[l] for l in range(DEPTH)], 1))
    put("lbf", np.concatenate([_chunked(f("lb_fwd")[l], 4) for l in range(DEPTH)], 1))
    put("lbb", np.concatenate([_chunked(f("lb_bwd")[l], 4) for l in range(DEPTH)], 1))
    cw = f("conv_w")
    cwr = cw.reshape(DEPTH, 31, 4, 128).transpose(3, 0, 2, 1).reshape(128, DEPTH * 4 * 31)
    put("convw", cwr)
    put("rbias", np.broadcast_to(f("router_bias")[None, :], (128, NE)))
    bada = np.stack([_chunked(f("b_ada")[l], 48) for l in range(DEPTH)], 0)
    rwr = np.ascontiguousarray(f("router_w").reshape(8, 128, NE).transpose(1, 0, 2).reshape(128, 8 * NE))
    consts = make_consts()
    shared = {"w_ada": f("w_ada"), "b_ada_r": bada, "vecs": vecs, "consts": consts, "w_in": f("w_in"),
              "w_out": f("w_out"), "router_w_r": rwr, "w_gate": f("w_gate"), "w_up": f("w_up"), "w_down": f("w_down")}
    maps = []
    cc = f("c_ctx")
    for b in cores:
        c2 = np.stack([_chunked(f("c")[b], 8), _chunked(cc, 8)], 2).reshape(128, 16)
        m = dict(shared)
        m["x"] = np.ascontiguousarray(f("x")[b])
        m["ctx"] = np.ascontiguousarray(f("ctx")[b])
        m["c2"] = np.ascontiguousarray(c2)
        maps.append(m)
    return maps


_NC_CACHE = {}


def kernel(**inputs):
    if "nc" not in _NC_CACHE:
        _NC_CACHE["nc"] = build_program()
    nc = _NC_CACHE["nc"]
    maps = make_in_maps(inputs, list(range(8)))
    res = run_bass_kernel_spmd(nc, maps, core_ids=list(range(8)))
    return np.stack([np.asarray(r["y"], np.float32) for r in res.results], 0)
```

```python
import numpy as np
from contextlib import ExitStack, suppress
import concourse.bass as bass
import concourse.mybir as mybir
from concourse.bass_utils import run_bass_kernel_spmd

F32, BF16 = mybir.dt.float32, mybir.dt.bfloat16
AF = mybir.ActivationFunctionType
ALU = mybir.AluOpType
AX = mybir.AxisListType

D = 1024
NX = 2048
NCX = 256
NT = NX + NCX
DEPTH = 2
NE = 16
EPS = 1e-6
CH = 32
NCHK = NT // CH
TT = [(0, 512), (512, 512), (1024, 512), (1536, 512), (2048, 256)]
BIG = 1.0e4
MOE_PIPE = True
FUSE_WAITS = True

VOFF = {}
_o = 0
for _n, _w in (("n1g", 16), ("n2g", 16), ("fing", 8), ("convb", 8), ("lng", 8), ("lnb", 8),
               ("hg", 2), ("lbf", 8), ("lbb", 8), ("convw", 2 * 4 * 31), ("rbias", 16)):
    VOFF[_n] = _o
    _o += _w
NV = _o
COFF = {"ident": 0, "maskF": 128, "maskB": 256, "smask": 384}
NCST = 384 + 512 + 4


class Res:
    __slots__ = ("w", "r")

    def __init__(self):
        self.w = None
        self.r = {}


class Stream:
    def __init__(self, eng, sem, key):
        self.eng, self.sem, self.key = eng, sem, key
        self.count = 0
        self.waited = {}


class DSem:
    def __init__(self, handle, key):
        self.handle, self.key, self.count = handle, key, 0


class Sched:
    def __init__(self, nc, es):
        self.nc, self.es = nc, es
        self.semh = {}
        self.streams = {}
        for name, eng in (("pe", nc.tensor), ("act", nc.scalar), ("dve", nc.vector),
                          ("pool", nc.gpsimd), ("sp", nc.sync)):
            h = es.enter_context(nc.semaphore("s_" + name))
            self.semh[name] = h
            self.streams[name] = Stream(eng, h, name)
        self.dsems = []

    def res(self):
        return Res()

    def grid(self, *dims):
        if len(dims) == 1:
            return [Res() for _ in range(dims[0])]
        return [self.grid(*dims[1:]) for _ in range(dims[0])]

    def dsem(self):
        key = f"d{len(self.dsems)}"
        h = self.es.enter_context(self.nc.semaphore("s_" + key))
        self.semh[key] = h
        d = DSem(h, key)
        self.dsems.append(d)
        return d

    def _deps(self, st, reads, writes, fuse=False):
        deps = {}

        def add(tok, same_ok):
            if tok is None:
                return
            k, v = tok
            if k == st.key and not same_ok:
                return
            if deps.get(k, 0) < v:
                deps[k] = v
        for r in reads:
            add(r.w, True)
        for w in writes:
            add(w.w, False)
            for k, v in w.r.items():
                add((k, v), False)
        need = []
        for k, v in deps.items():
            if st.waited.get(k, 0) < v:
                st.waited[k] = v
                need.append((k, v))
        if fuse and need:
            for k, v in need[:-1]:
                st.eng.wait_ge(self.semh[k], v)
            return need[-1]
        for k, v in need:
            st.eng.wait_ge(self.semh[k], v)
        return None

    def _mark(self, key, val, reads, writes):
        for r in reads:
            r.r[key] = val
        for w in writes:
            w.w = (key, val)
            w.r = {}

    def _attach(self, st, ins, w):
        if w is not None:
            ins._wait_ge(self.semh[w[0]], st.eng.lower_val(w[1]))
        return ins

    def op(self, sname, fn, reads=(), writes=()):
        st = self.streams[sname]
        w = self._deps(st, reads, writes, fuse=FUSE_WAITS)
        st.count += 1
        self._attach(st, fn(), w).then_inc(st.sem, 1)
        self._mark(st.key, st.count, reads, writes)

    def group(self, sname, fns, reads=(), writes=()):
        st = self.streams[sname]
        w = self._deps(st, reads, writes, fuse=FUSE_WAITS)
        st.count += 1
        n = len(fns)
        for i, fn in enumerate(fns):
            ins = fn()
            if i == 0:
                self._attach(st, ins, w)
            if i == n - 1:
                ins.then_inc(st.sem, 1)
        self._mark(st.key, st.count, reads, writes)

    def dma(self, sname, out, in_, ds, reads=(), writes=()):
        st = self.streams[sname]
        self._deps(st, reads, writes)
        ds.count += 16
        st.eng.dma_start(out=out, in_=in_).then_inc(ds.handle, 16)
        self._mark(ds.key, ds.count, reads, writes)

    def barrier(self):
        for st in self.streams.values():
            for o in self.streams.values():
                if o is not st and o.count > st.waited.get(o.key, 0):
                    st.waited[o.key] = o.count
                    st.eng.wait_ge(o.sem, o.count)
            for d in self.dsems:
                if d.count > st.waited.get(d.key, 0):
                    st.waited[d.key] = d.count
                    st.eng.wait_ge(d.handle, d.count)


class _Stop(Exception):
    pass


def build_program(nlayers=DEPTH, dbg=None, stop=None):
    dbg = dbg or {}
    nc = bass.Bass("TRN2", target_bir_lowering=False)
    dr = lambda n, s, k="ExternalInput": nc.dram_tensor(n, s, F32, kind=k).ap()
    x_d = dr("x", [NX, D])
    ctx_d = dr("ctx", [NCX, D])
    c2_d = dr("c2", [128, 16])
    wada_d = dr("w_ada", [DEPTH, D, 6 * D])
    bada_d = dr("b_ada_r", [DEPTH, 128, 48])
    vecs_d = dr("vecs", [128, NV])
    cst_d = dr("consts", [128, NCST])
    win_d = dr("w_in", [DEPTH, D, 3584])
    wout_d = dr("w_out", [DEPTH, D, D])
    rw_d = dr("router_w_r", [128, 8 * NE])
    wg_d = dr("w_gate", [DEPTH, NE, D, D])
    wu_d = dr("w_up", [DEPTH, NE, D, D])
    wd_d = dr("w_down", [DEPTH, NE, D, D])
    y_d = dr("y", [NX, D], "ExternalOutput")
    dbg_d = {k: nc.dram_tensor("dbg_" + k, list(s[0]), BF16 if s[1] == "bf16" else F32, kind="ExternalOutput").ap()
             for k, s in dbg.items()}

    es = ExitStack()
    with suppress(_Stop), es:
        S = Sched(nc, es)
        V, A, G, T = nc.vector, nc.scalar, nc.gpsimd, nc.tensor

        nsb = [0]

        def sbuf(es_, name, shape, dt):
            nsb[0] += 1
            return es_.enter_context(nc.sbuf_tensor(f"sb{nsb[0]}_{name}", shape, dt))

        xT = sbuf(es, "xT", [128, 8, NT], F32)
        hT = sbuf(es, "hT", [128, 8, NT], BF16)
        cst = sbuf(es, "cst", [128, NCST], F32)
        vecs = sbuf(es, "vecs", [128, NV], F32)
        identb = sbuf(es, "identb", [128, 128], BF16)
        onesb = sbuf(es, "onesb", [128, 128], BF16)
        zerob = sbuf(es, "zerob", [128, 128], BF16)
        epsD = sbuf(es, "epsD", [128, 3], F32)
        mod = sbuf(es, "mod", [128, 48, 2], F32)
        se = sbuf(es, "se", [128, 2, 8, 2], F32)
        lbt = sbuf(es, "lbt", [128, 2, 4, 3], F32)
        rw = sbuf(es, "rw", [128, 8, NE], F32)
        P = [es.enter_context(nc.psum_tensor(f"ps{i}", [128, 512], F32)) for i in range(8)]
        rP = S.grid(8)
        rxT = S.grid(8, 5)
        rhT = S.grid(5)
        rcst, rvecs, rmisc, rmod, rse, rlbt, rrw = (S.res() for _ in range(7))
        ident = cst[:, 0:128]
        maskF = cst[:, 128:256]
        maskB = cst[:, 256:384]
        smask = cst[:, 384:896]
        dconst = S.dsem()
        vv = lambda n, i: vecs[:, VOFF[n] + i: VOFF[n] + i + 1]

        S.dma("sp", cst[:], cst_d[:, :], S.dsem(), writes=[rcst])
        S.dma("sp", vecs[:], vecs_d[:, :], S.dsem(), writes=[rvecs])
        S.dma("sp", rw[:].rearrange("p a b -> p (a b)"), rw_d[:, :], S.dsem(), writes=[rrw])
        S.op("dve", lambda: V.memset(onesb[:], 1.0), writes=[rmisc])
        S.op("dve", lambda: V.memset(zerob[:], 0.0), writes=[rmisc])
        S.op("dve", lambda: V.memset(epsD[:, 0:1], float(D * EPS)), writes=[rmisc])
        S.op("dve", lambda: V.memset(epsD[:, 1:3], float(EPS)), writes=[rmisc])
        S.op("dve", lambda: V.tensor_copy(identb[:], cst[:, 0:128]), reads=[rcst], writes=[rmisc])

        pctr = [0]

        def nextp(lo, hi):
            p = lo + pctr[0] % (hi - lo)
            pctr[0] += 1
            return p

        def dump(name, ap_sb, res_list):
            if name in dbg_d:
                ds = S.dsem()
                S.dma("sp", dbg_d[name], ap_sb, ds, reads=res_list)

        with ExitStack() as ea:
            xin = [sbuf(ea, f"xin{i}", [128, D], F32) for i in range(2)]
            rxin = S.grid(2)
            dxin = [S.dsem(), S.dsem()]
            for tt in range(18):
                b = tt % 2
                src = x_d[tt * 128:(tt + 1) * 128, :] if tt < 16 else ctx_d[(tt - 16) * 128:(tt - 15) * 128, :]
                S.dma("sp", xin[b][:], src, dxin[b], writes=[rxin[b]])
                for half in range(2):
                    p = nextp(0, 4)
                    fns = [(lambda c=c, p=p, b=b: T.transpose(P[p][:, (c % 4) * 128:(c % 4 + 1) * 128],
                                                              xin[b][:, c * 128:(c + 1) * 128], ident))
                           for c in range(half * 4, half * 4 + 4)]
                    S.group("pe", fns, reads=[rxin[b], rcst], writes=[rP[p]])
                    ws = [rxT[c][tt // 4] for c in range(half * 4, half * 4 + 4)]
                    eng = "dve" if half == 0 else "act"
                    dst = xT[:, half * 4:half * 4 + 4, tt * 128:(tt + 1) * 128]
                    srcp = P[p][:].rearrange("p (c t) -> p c t", c=4)
                    if eng == "dve":
                        S.op("dve", lambda dst=dst, srcp=srcp: V.tensor_copy(dst, srcp), reads=[rP[p]], writes=ws)
                    else:
                        S.op("act", lambda dst=dst, srcp=srcp: A.copy(dst, srcp), reads=[rP[p]], writes=ws)
            S.barrier()

        allx = lambda ti: [rxT[c][ti] for c in range(8)]

        def norm_mod(nidx, tiles, h2f=None, rh2f=None, es_=None):
            sq = sbuf(es_, f"sq{nidx}", [128, 8, 512], BF16)
            rstd = sbuf(es_, f"rstd{nidx}", [128, 512], F32)
            tmp = [sbuf(es_, f"nt{nidx}_{i}", [128, 512], F32) for i in range(2)]
            rsq, rrstd = S.res(), S.res()
            rtmp = S.grid(2)
            shb = 0 if nidx == 0 else 24
            for ti, (t0, n) in enumerate(tiles):
                col = 0 if t0 < NX else 1
                for c in range(8):
                    S.op("act", lambda c=c: A.activation(sq[:, c, :n], xT[:, c, t0:t0 + n], AF.Square),
                         reads=[rxT[c][ti]], writes=[rsq])
                p = nextp(0, 4)
                S.group("pe", [(lambda c=c, p=p: T.matmul(P[p][:, :n], onesb[:], sq[:, c, :n], start=(c == 0), stop=(c == 7)))
                               for c in range(8)], reads=[rsq, rmisc], writes=[rP[p]])
                S.op("act", lambda p=p: A.activation(rstd[:, :n], P[p][:, :n], AF.Ln, bias=epsD[:, 0:1], scale=1.0),
                     reads=[rP[p], rmisc], writes=[rrstd])
                S.op("act", lambda: A.activation(rstd[:, :n], rstd[:, :n], AF.Exp, scale=-0.5), reads=[rrstd], writes=[rrstd])
                for c in range(8):
                    b = c % 2
                    S.op("dve", lambda c=c, b=b: V.tensor_tensor(tmp[b][:, :n], xT[:, c, t0:t0 + n], rstd[:, :n], ALU.mult),
                         reads=[rxT[c][ti], rrstd], writes=[rtmp[b]])
                    sc_ap = se[:, nidx, c, col:col + 1]
                    sh_ap = mod[:, shb + c, col:col + 1]
                    if h2f is None:
                        S.op("act", lambda c=c, b=b, sc_ap=sc_ap, sh_ap=sh_ap: A.activation(
                            hT[:, c, t0:t0 + n], tmp[b][:, :n], AF.Identity, bias=sh_ap, scale=sc_ap),
                            reads=[rtmp[b], rse, rmod], writes=[rhT[ti]])
                    else:
                        S.op("act", lambda c=c, b=b, sc_ap=sc_ap, sh_ap=sh_ap: A.activation(
                            h2f[:, c, t0:t0 + n], tmp[b][:, :n], AF.Identity, bias=sh_ap, scale=sc_ap),
                            reads=[rtmp[b], rse, rmod], writes=[rh2f[ti]])
                        S.op("pool", lambda c=c: G.tensor_copy(hT[:, c, t0:t0 + n], h2f[:, c, t0:t0 + n]),
                             reads=[rh2f[ti]], writes=[rhT[ti]])

        class WChunks:
            def __init__(self, es_, tag, srcs, ceng, nst=3, nbf=3):
                self.srcs, self.ceng = srcs, ceng
                self.st = [sbuf(es_, f"wst{tag}{i}", [128, 8, 128], F32) for i in range(nst)]
                self.bf = [sbuf(es_, f"wbf{tag}{i}", [128, 8, 128], BF16) for i in range(nbf)]
                self.rst, self.rbf = S.grid(nst), S.grid(nbf)
                self.dst = [S.dsem() for _ in range(nst)]
                self.nl = 0
                self.nc_ = 0
                self.look = nbf - 1

            def _load(self):
                if self.nl >= len(self.srcs):
                    return
                i = self.nl % len(self.st)
                S.dma("sp", self.st[i][:], self.srcs[self.nl], self.dst[i], writes=[self.rst[i]])
                self.nl += 1

            def _cast(self):
                if self.nc_ >= len(self.srcs):
                    return
                i = self.nc_ % len(self.st)
                j = self.nc_ % len(self.bf)
                src, dst = self.st[i], self.bf[j]
                if self.ceng == "pool":
                    S.op("pool", lambda: G.tensor_copy(dst[:], src[:]), reads=[self.rst[i]], writes=[self.rbf[j]])
                else:
                    S.op("act", lambda: A.copy(dst[:], src[:]), reads=[self.rst[i]], writes=[self.rbf[j]])
                self.nc_ += 1

            def prefetch(self):
                for _ in range(len(self.st)):
                    self._load()
                for _ in range(self.look):
                    self._cast()
                    self._load()

            def get(self, k):
                while self.nc_ <= k:
                    self._cast()
                    self._load()
                j = k % len(self.bf)
                return self.bf[j], self.rbf[j]

            def after(self, k):
                while self.nc_ <= k + self.look and self.nc_ < len(self.srcs):
                    self._cast()
                    self._load()

        def proj_fm(wt, rwt, tiles, consume, plo=0, phi=4):
            for ti, (t0, n) in enumerate(tiles):
                p = nextp(plo, phi)
                S.group("pe", [(lambda kc=kc, p=p: T.matmul(P[p][:, :n], wt[:, kc, :], hT[:, kc, t0:t0 + n],
                                                            start=(kc == 0), stop=(kc == 7))) for kc in range(8)],
                        reads=[rwt, rhT[ti]], writes=[rP[p]])
                consume(p, ti, t0, n)

        def ckpt(name):
            if stop == name:
                S.barrier()
                raise _Stop()

        for l in ([] if stop == "load" else range(nlayers)):
            last = (l == DEPTH - 1)
            TO = TT[:4] if last else TT
            with ExitStack() as eb:
                c2 = sbuf(eb, "c2", [128, 8, 2], F32)
                sc2 = sbuf(eb, "sc2", [128, 8, 2], F32)
                bada = sbuf(eb, "bada", [128, 48], F32)
                wa = [sbuf(eb, f"wa{i}", [128, 8, 512], F32) for i in range(2)]
                rc2, rsc2, rbada = S.res(), S.res(), S.res()
                rwa = S.grid(2)
                dwa = [S.dsem(), S.dsem()]
                S.dma("sp", c2[:].rearrange("p a b -> p (a b)"), c2_d[:, :], S.dsem(), writes=[rc2])
                S.dma("sp", bada[:], bada_d[l, :, :], S.dsem(), writes=[rbada])
                S.op("act", lambda: A.activation(sc2[:], c2[:], AF.Silu), reads=[rc2], writes=[rsc2])
                pm = 7
                for g in range(12):
                    b = g % 2
                    S.dma("sp", wa[b][:], wada_d[l, :, g * 512:(g + 1) * 512].rearrange("(kc p) f -> p kc f", p=128),
                          dwa[b], writes=[rwa[b]])
                    fns = []
                    for j in range(4):
                        fc = g * 4 + j
                        for kc in range(8):
                            fns.append(lambda j=j, fc=fc, kc=kc, b=b: T.matmul(
                                P[pm][:, fc * 2:fc * 2 + 2], wa[b][:, kc, j * 128:(j + 1) * 128], sc2[:, kc, :],
                                start=(kc == 0), stop=(kc == 7)))
                    S.group("pe", fns, reads=[rwa[b], rsc2], writes=[rP[pm]])
                S.op("dve", lambda: V.tensor_tensor(mod[:], P[pm][:, 0:96].rearrange("p (a b) -> p a b", b=2),
                                                    bada[:].unsqueeze(2).to_broadcast([128, 48, 2]), ALU.add),
                     reads=[rP[pm], rbada], writes=[rmod])
                for nidx, (scb, gname) in enumerate(((8, "n1g"), (32, "n2g"))):
                    S.op("dve", lambda nidx=nidx, scb=scb: V.tensor_scalar(
                        se[:, nidx, :, :], mod[:, scb:scb + 8, :], 1.0, float(np.sqrt(D)), ALU.add, ALU.mult),
                        reads=[rmod], writes=[rse])
                    gap = vecs[:, VOFF[gname] + l * 8: VOFF[gname] + l * 8 + 8].unsqueeze(2).to_broadcast([128, 8, 2])
                    S.op("dve", lambda nidx=nidx, gap=gap: V.tensor_tensor(se[:, nidx, :, :], se[:, nidx, :, :], gap, ALU.mult),
                         reads=[rse, rvecs], writes=[rse])
                for di, nm in enumerate(("lbf", "lbb")):
                    l0 = vecs[:, VOFF[nm]:VOFF[nm] + 4]
                    l1 = vecs[:, VOFF[nm] + 4:VOFF[nm] + 8]
                    if l == 0:
                        S.op("dve", lambda di=di: V.memset(lbt[:, di, :, 0], 0.0), writes=[rlbt])
                    else:
                        S.op("dve", lambda di=di, l0=l0, l1=l1: V.tensor_tensor(lbt[:, di, :, 0], l1, l0, ALU.subtract),
                             reads=[rvecs], writes=[rlbt])
                        S.op("act", lambda di=di: A.activation(lbt[:, di, :, 0], lbt[:, di, :, 0], AF.Sigmoid),
                             reads=[rlbt], writes=[rlbt])
                    S.op("dve", lambda di=di: V.tensor_scalar(lbt[:, di, :, 1], lbt[:, di, :, 0], -1.0, 1.0, ALU.mult, ALU.add),
                         reads=[rlbt], writes=[rlbt])
                    S.op("dve", lambda di=di: V.tensor_scalar(lbt[:, di, :, 2], lbt[:, di, :, 0], 1.0, -1.0, ALU.mult, ALU.add),
                         reads=[rlbt], writes=[rlbt])
                S.barrier()
            dump(f"mod{l}", mod[:].rearrange("p a b -> p (a b)"), [rmod])

            with ExitStack() as em:
                rym = S.grid(8, 5)
                with ExitStack() as en:
                    norm_mod(0, TT, es_=en)
                    S.barrier()
                dump(f"h{l}", hT[:].rearrange("p a b -> p (a b)"), rhT)
                if stop == "norm1":
                    break
                srcs = []
                wsl = lambda grp, sub: win_d[l, :, grp * 512 + sub * 128: grp * 512 + (sub + 1) * 128].rearrange(
                    "(kc p) f -> p kc f", p=128)
                wrow = lambda r0: wout_d[l, r0:r0 + 128, :].rearrange("p (dj f) -> p dj f", f=128)
                for h in range(4):
                    srcs += [wsl(3, h), wsl(4, h), wsl(2, h), wsl(5, h), wsl(2, h), wsl(6, h), wrow(512 + h * 128)]
                for j in range(4):
                    srcs += [wsl(0, j), wsl(1, j)]
                W = WChunks(em, "m", srcs, "act", nst=2, nbf=3)
                W.prefetch()
                wk = [0]

                def nextw():
                    k = wk[0]
                    wk[0] += 1
                    t_, r_ = W.get(k)
                    return k, t_, r_

                with ExitStack() as eh:
                    o_acc = sbuf(eh, "o_acc", [128, NT], F32)
                    Vh = sbuf(eh, "Vh", [128, 18, 128], BF16)
                    Vx = [sbuf(eh, f"Vx{i}", [128, 4, 128], BF16) for i in range(2)]
                    yh = sbuf(eh, "yh", [128, NT], BF16)
                    QT = sbuf(eh, "QT", [128, NT], BF16)
                    KT = sbuf(eh, "KT", [128, NT], BF16)
                    KHT = sbuf(eh, "KHT", [128, 18, 128], BF16)
                    dS = sbuf(eh, "dS", [128, NCHK, 128], BF16)
                    Dall = sbuf(eh, "Dall", [128, NCHK], F32)
                    Dpos = sbuf(eh, "Dpos", [128, NCHK], F32)
                    Wt = [[sbuf(eh, f"W{i}_{b_}", [128, 512], F32) for i in range(6)] for b_ in range(2)]
                    rWt = [[S.res() for i in range(6)] for b_ in range(2)]
                    W1 = Wt[0][0]
                    KHt = sbuf(eh, "KHt", [128, 512], BF16)
                    attm = [sbuf(eh, f"attm{i}", [128, 128], BF16) for i in range(2)]
                    ro_acc = S.grid(5)
                    rVh, rKHT, rdS, rDall, rDpos = (S.res() for _ in range(5))
                    rdSh = S.grid(2)
                    rQT, rKT, ryh = S.grid(5), S.grid(5), S.grid(5)
                    rKHt = S.res()
                    rW1 = rWt[0][0]
                    Wq, rWq = Wt[0][4], rWt[0][4]
                    sqh, rsqh, ro, rro = KHt, rKHt, W1, rW1
                    rattm, rVx = S.grid(2), S.grid(2)
                    if l == 0:
                        print("[kernel] SBUF bytes free inside HGRN scope:", nc.sbuf_bytes_remaining)
                    NXC = NX // CH
                    NCC = NCX // CH
                    CPT = 128 // CH
                    for h in range(4):
                        k, wt, rwt = nextw()
                        for g4 in range(5):
                            tts = range(g4 * 4, min(g4 * 4 + 4, 18))
                            p = nextp(0, 4)
                            fns = []
                            for tt in tts:
                                for kc in range(8):
                                    fns.append(lambda tt=tt, kc=kc, p=p: T.matmul(
                                        P[p][:, (tt % 4) * 128:(tt % 4 + 1) * 128], hT[:, kc, tt * 128:(tt + 1) * 128],
                                        wt[:, kc, :], start=(kc == 0), stop=(kc == 7)))
                            S.group("pe", fns, reads=[rwt, rhT[g4]], writes=[rP[p]])
                            nt_ = len(tts)
                            S.op("act", lambda p=p, g4=g4, nt_=nt_: A.copy(
                                Vh[:, g4 * 4:g4 * 4 + nt_, :], P[p][:, :nt_ * 128].rearrange("p (a b) -> p a b", b=128)),
                                reads=[rP[p]], writes=[rVh])
                        W.after(k)
                        ckpt("hg_v")
                        for di in range(2):
                            fwd = di == 0
                            kf, wf, rwf = nextw()
                            kq, wq, rwq = nextw()
                            lb_ap = lbt[:, di, h, 0:1]
                            oml_ap = lbt[:, di, h, 1:2]
                            noml_ap = lbt[:, di, h, 2:3]
                            def prep_a(ti):
                                t0, n = TT[ti]
                                nch, c0 = n // CH, t0 // CH
                                W1, W2, W3, W4, Wq, W5 = Wt[ti % 2]
                                rW1, rW2, rW3, rW4, rWq, rW5 = rWt[ti % 2]
                                v3 = lambda ap: ap[:, :n].rearrange("p (c k) -> p c k", k=CH)
                                pf = nextp(0, 4)
                                S.group("pe", [(lambda kc=kc, pf=pf: T.matmul(P[pf][:, :n], wf[:, kc, :], hT[:, kc, t0:t0 + n],
                                                                             start=(kc == 0), stop=(kc == 7))) for kc in range(8)],
                                        reads=[rwf, rhT[ti]], writes=[rP[pf]])
                                pq = nextp(0, 4)
                                S.group("pe", [(lambda kc=kc, pq=pq: T.matmul(P[pq][:, :n], wq[:, kc, :], hT[:, kc, t0:t0 + n],
                                                                             start=(kc == 0), stop=(kc == 7))) for kc in range(8)],
                                        reads=[rwq, rhT[ti]], writes=[rP[pq]])
                                S.op("act", lambda: A.activation(W1[:, :n], P[pf][:, :n], AF.Sigmoid), reads=[rP[pf]], writes=[rW1])
                                S.op("act", lambda: A.activation(Wq[:, :n], P[pq][:, :n], AF.Sigmoid), reads=[rP[pq]], writes=[rWq])
                                yield
                                S.op("act", lambda: A.activation(W2[:, :n], W1[:, :n], AF.Ln, bias=lb_ap, scale=oml_ap),
                                     reads=[rW1, rlbt], writes=[rW2])
                                S.op("dve", lambda: V.tensor_tensor(Wq[:, :n], P[pq][:, :n], Wq[:, :n], ALU.mult), reads=[rP[pq], rWq], writes=[rWq])
                                S.op("dve", lambda: V.tensor_scalar(W5[:, :n], W1[:, :n], noml_ap, oml_ap, ALU.mult, ALU.add),
                                     reads=[rW1, rlbt], writes=[rW5])
                                yield
                                S.op("dve", lambda: V.tensor_tensor_scan(W3[:, :n], smask[:, :n], W2[:, :n], 0.0, ALU.mult, ALU.add),
                                     reads=[rW2, rcst], writes=[rW3])
                                if fwd:
                                    bb, rbb = W3, rW3
                                    bl = v3(W3)[:, :, CH - 1]
                                else:
                                    tc_ = v3(W3)[:, :, CH - 1:CH].to_broadcast([128, nch, CH])
                                    S.op("dve", lambda: V.tensor_tensor(v3(W4), tc_, v3(W3), ALU.subtract), reads=[rW3], writes=[rW4])
                                    S.op("dve", lambda: V.tensor_tensor(W4[:, :n], W4[:, :n], W2[:, :n], ALU.add),
                                         reads=[rW4, rW2], writes=[rW4])
                                    bb, rbb = W4, rW4
                                    bl = v3(W4)[:, :, 0]
                                yield
                                S.op("act", lambda: A.activation(Dall[:, c0:c0 + nch], bl, AF.Exp), reads=[rbb], writes=[rDall])
                                yield
                                res_[ti] = (bb, rbb)

                            def prep_b(ti):
                                bb, rbb = res_[ti]
                                t0, n = TT[ti]
                                nch, c0 = n // CH, t0 // CH
                                W1, W2, W3, W4, Wq, W5 = Wt[ti % 2]
                                rW1, rW2, rW3, rW4, rWq, rW5 = rWt[ti % 2]
                                v3 = lambda ap: ap[:, :n].rearrange("p (c k) -> p c k", k=CH)
                                S.op("act", lambda: A.activation(W2[:, :n], bb[:, :n], AF.Exp), reads=[rbb, rW2], writes=[rW2])
                                S.op("act", lambda: A.activation(W1[:, :n], bb[:, :n], AF.Exp, scale=-1.0), reads=[rbb, rW1], writes=[rW1])
                                yield
                                S.op("dve", lambda: V.tensor_tensor(QT[:, t0:t0 + n], Wq[:, :n], W2[:, :n], ALU.mult),
                                     reads=[rWq, rW2], writes=[rQT[ti]])
                                S.op("dve", lambda: V.tensor_tensor(W5[:, :n], W5[:, :n], W1[:, :n], ALU.mult),
                                     reads=[rW5, rW1], writes=[rW5])
                                yield
                                S.op("act", lambda: A.copy(KT[:, t0:t0 + n], W5[:, :n]), reads=[rW5], writes=[rKT[ti]])
                                dbc = Dall[:, c0:c0 + nch].unsqueeze(2).to_broadcast([128, nch, CH])
                                S.op("dve", lambda: V.tensor_tensor(v3(KHt), v3(W5), dbc, ALU.mult), reads=[rW5, rDall], writes=[rKHt])
                                yield
                                pk = nextp(4, 6)
                                ntile = n // 128
                                pkb = P[pk][:].bitcast(BF16)
                                S.group("pe", [(lambda j=j: T.transpose(pkb[:, j * 128:(j + 1) * 128],
                                                                        KHt[:, j * 128:(j + 1) * 128], identb[:]))
                                               for j in range(ntile)], reads=[rKHt, rmisc], writes=[rP[pk]])
                                yield
                                S.op("act", lambda: A.copy(KHT[:, t0 // 128:t0 // 128 + ntile, :],
                                                           pkb[:, :ntile * 128].rearrange("p (a b) -> p a b", b=128)),
                                     reads=[rP[pk]], writes=[rKHT])
                                yield
                            res_ = {}

                            def run_zip(g1, g2):
                                gens = [g for g in (g1, g2) if g is not None]
                                while gens:
                                    for g in list(gens):
                                        try:
                                            next(g)
                                        except StopIteration:
                                            gens.remove(g)
                            run_zip(prep_a(0), None)
                            for ti in range(len(TT)):
                                run_zip(prep_a(ti + 1) if ti + 1 < len(TT) else None, prep_b(ti))
                            W.after(kq)
                            ckpt("hg_prep")
                            order = (list(range(NXC, NCHK)) + list(range(NXC))) if fwd else list(range(NCHK - 1, -1, -1))
                            pos = {c: i for i, c in enumerate(order)}
                            for tt in range(18):
                                vb = tt % 2
                                S.op("dve", lambda vb=vb, tt=tt: V.tensor_tensor(
                                    Vx[vb][:], Vh[:, tt, :].unsqueeze(1).to_broadcast([128, CPT, 128]),
                                    cst[:, 896:896 + CPT].unsqueeze(2).to_broadcast([128, CPT, 128]), ALU.mult),
                                    reads=[rVh, rcst], writes=[rVx[vb]])
                                p = nextp(6, 8)
                                S.group("pe", [lambda tt=tt, vb=vb, p=p: T.matmul(
                                    P[p][:, :CPT * 128], KHT[:, tt, :], Vx[vb][:].rearrange("p a b -> p (a b)"), start=True, stop=True)],
                                    reads=[rKHT, rVx[vb]], writes=[rP[p]])
                                p0 = pos[tt * CPT]
                                if fwd:
                                    dst = dS[:, p0:p0 + CPT, :]
                                else:
                                    dst = dS[:, p0:p0 - CPT:-1, :] if p0 - CPT >= 0 else dS[:, p0::-1, :]
                                S.op("act", lambda dst=dst, p=p: A.copy(dst, P[p][:, :CPT * 128].rearrange("p (c v) -> p c v", v=128)),
                                     reads=[rP[p]], writes=[rdS, rdSh[0], rdSh[1]])
                            ckpt("hg_ds")
                            if fwd:
                                S.op("dve", lambda: V.tensor_copy(Dpos[:, NCC:NCHK], Dall[:, 0:NXC]), reads=[rDall], writes=[rDpos])
                                S.op("dve", lambda: V.tensor_copy(Dpos[:, 1:NCC], Dall[:, NXC + 1:NCHK]), reads=[rDall], writes=[rDpos])
                            else:
                                S.op("dve", lambda: V.tensor_copy(Dpos[:, 1:NCHK], Dall[:, NCHK - 2::-1]), reads=[rDall], writes=[rDpos])
                            S.op("dve", lambda: V.memset(Dpos[:, 0:1], 0.0), reads=[rDpos], writes=[rDpos])
                            for pp in range(1, NCHK):
                                for hf in range(2):
                                    vs = slice(hf * 64, hf * 64 + 64)
                                    S.op("dve", lambda pp=pp, vs=vs: V.scalar_tensor_tensor(
                                        dS[:, pp, vs], dS[:, pp - 1, vs], Dpos[:, pp:pp + 1], dS[:, pp, vs], ALU.mult, ALU.add),
                                        reads=[rdSh[hf], rDpos], writes=[rdSh[hf]])
                            ckpt("hg_scan")
                            mask = maskF if fwd else maskB
                            for g4 in range(5):
                                tts = list(range(g4 * 4, min(g4 * 4 + 4, 18)))
                                po = nextp(4, 6)
                                for tt in tts:
                                    off = (tt % 4) * 128
                                    fns = []
                                    for c in range(tt * CPT, (tt + 1) * CPT):
                                        pc = pos[c]
                                        lhs = zerob[:] if pc == 0 else dS[:, pc - 1, :]
                                        co = off + (c % CPT) * CH
                                        fns.append(lambda c=c, lhs=lhs, co=co, po=po: T.matmul(
                                            P[po][:, co:co + CH], lhs, QT[:, c * CH:(c + 1) * CH], start=(c % CPT == 0), stop=False,
                                            skip_group_check=True))
                                    S.group("pe", fns, reads=[rdS, rdSh[0], rdSh[1], rQT[g4], rmisc], writes=[rP[po]])
                                    pa = nextp(6, 8)
                                    S.group("pe", [lambda tt=tt, pa=pa: T.matmul(P[pa][:, 0:128], KT[:, tt * 128:(tt + 1) * 128],
                                                                                QT[:, tt * 128:(tt + 1) * 128], start=True, stop=True)],
                                            reads=[rKT[g4], rQT[g4]], writes=[rP[pa]])
                                    ab = tt % 2
                                    S.op("dve", lambda pa=pa, ab=ab, mask=mask: V.tensor_tensor(attm[ab][:], P[pa][:, 0:128], mask, ALU.mult),
                                         reads=[rP[pa], rcst], writes=[rattm[ab]])
                                    S.group("pe", [lambda tt=tt, ab=ab, off=off, po=po: T.matmul(
                                        P[po][:, off:off + 128], Vh[:, tt, :], attm[ab][:], start=False, stop=True, skip_group_check=True)],
                                        reads=[rVh, rattm[ab]], writes=[rP[po]])
                                nn = len(tts) * 128
                                t0 = g4 * 512
                                if fwd:
                                    S.op("act", lambda po=po, nn=nn, t0=t0: A.copy(o_acc[:, t0:t0 + nn], P[po][:, :nn]),
                                         reads=[rP[po]], writes=[ro_acc[g4]])
                                else:
                                    S.op("dve", lambda po=po, nn=nn, t0=t0: V.tensor_tensor(o_acc[:, t0:t0 + nn], o_acc[:, t0:t0 + nn],
                                                                                          P[po][:, :nn], ALU.add),
                                         reads=[rP[po], ro_acc[g4]], writes=[ro_acc[g4]])
                            if stop == "hg_o" + str(di):
                                dump("QT", QT[:], rQT); dump("KT", KT[:], rKT); dump("dS", dS[:].rearrange("p c v -> p (c v)"), [rdS])
                                dump("Dall", Dall[:], [rDall]); dump("Dpos", Dpos[:], [rDpos]); dump("oacc0_0", o_acc[:], ro_acc)
                            ckpt("hg_o" + str(di))
                        if f"oacc{l}_{h}" in dbg_d:
                            dump(f"oacc{l}_{h}", o_acc[:], ro_acc)
                        W1, rW1, Wq, rWq = Wt[0][0], rWt[0][0], Wt[0][4], rWt[0][4]
                        ro, rro = W1, rW1
                        ko, wo_, rwo = nextw()
                        kr, wr_, rwr = nextw()
                        gh = vv("hg", l)
                        for ti, (t0, n) in enumerate(TO):
                            S.op("act", lambda: A.activation(sqh[:, :n], o_acc[:, t0:t0 + n], AF.Square), reads=[ro_acc[ti]], writes=[rsqh])
                            p = nextp(0, 4)
                            S.group("pe", [lambda p=p: T.matmul(P[p][:, :n], onesb[:], sqh[:, :n], start=True, stop=True)],
                                    reads=[rsqh, rmisc], writes=[rP[p]])
                            S.op("act", lambda p=p: A.activation(ro[:, :n], P[p][:, :n], AF.Ln, bias=epsD[:, 1:2], scale=1.0 / 128),
                                 reads=[rP[p], rmisc], writes=[rro])
                            S.op("act", lambda: A.activation(ro[:, :n], ro[:, :n], AF.Exp, scale=-0.5), reads=[rro], writes=[rro])
                            S.op("dve", lambda: V.tensor_tensor(ro[:, :n], ro[:, :n], o_acc[:, t0:t0 + n], ALU.mult),
                                 reads=[rro, ro_acc[ti]], writes=[rro])
                            p2 = nextp(0, 4)
                            S.group("pe", [(lambda kc=kc, p2=p2: T.matmul(P[p2][:, :n], wo_[:, kc, :], hT[:, kc, t0:t0 + n],
                                                                         start=(kc == 0), stop=(kc == 7))) for kc in range(8)],
                                    reads=[rwo, rhT[ti]], writes=[rP[p2]])
                            S.op("act", lambda p2=p2: A.activation(Wq[:, :n], P[p2][:, :n], AF.Silu), reads=[rP[p2]], writes=[rWq])
                            S.op("dve", lambda: V.scalar_tensor_tensor(yh[:, t0:t0 + n], ro[:, :n], gh, Wq[:, :n], ALU.mult, ALU.mult),
                                 reads=[rro, rWq, rvecs], writes=[ryh[ti]])
                            col = 0 if t0 < NX else 1
                            for dj in range(8):
                                p3 = nextp(4, 8)
                                S.group("pe", [lambda p3=p3, dj=dj: T.matmul(P[p3][:, :n], wr_[:, dj, :], yh[:, t0:t0 + n], start=True, stop=True)],
                                        reads=[rwr, ryh[ti]], writes=[rP[p3]])
                                g1 = mod[:, 16 + dj, col:col + 1]
                                S.op("dve", lambda p3=p3, g1=g1, dj=dj: V.scalar_tensor_tensor(
                                    xT[:, dj, t0:t0 + n], P[p3][:, :n], g1, xT[:, dj, t0:t0 + n], ALU.mult, ALU.add),
                                    reads=[rP[p3], rmod, rxT[dj][ti]], writes=[rxT[dj][ti]])
                        if f"yh{l}_{h}" in dbg_d:
                            dump(f"yh{l}_{h}", yh[:], ryh)
                        W.after(kr)
                    S.barrier()
                if stop == "hgrn":
                    break
                ecm = ExitStack()
                em.enter_context(ecm)
                ymc = sbuf(ecm, "ymc", [128, 4, NT], BF16)
                with ExitStack() as ec:
                    rowo = (l % 2 == 0)
                    conv_tiles = TT if not last else TT[:4]
                    UPL = (32 * 79 + 31) if rowo else (62 * 64)
                    Upad = [sbuf(ec, f"Upad{i}", [128, UPL + 286], BF16) for i in range(2)]
                    Dg = [sbuf(ec, f"Dg{i}", [128, 31, 128], BF16) for i in range(2)]
                    sgt = [sbuf(ec, f"sgt{i}", [128, 512], F32) for i in range(2)]
                    cwb = sbuf(ec, "cwb", [128, 4 * 31], BF16)
                    rUp, rDg, rsgt = S.grid(2), S.grid(2), S.grid(2)
                    rcwb = S.res()
                    for i in range(2):
                        S.op("pool", lambda i=i: G.memset(Upad[i][:], 0.0), writes=[rUp[i]])
                    S.op("dve", lambda: V.tensor_copy(cwb[:], vecs[:, VOFF["convw"] + l * 124: VOFF["convw"] + (l + 1) * 124]),
                         reads=[rvecs], writes=[rcwb])

                    def uview(b, ti, k):
                        t0, n = TT[ti]
                        if t0 >= NX:
                            return Upad[b][:, UPL + k: UPL + k + NCX]
                        r0 = t0 // 64
                        if rowo:
                            return Upad[b][:, r0 * 79 + k: r0 * 79 + k + 8 * 79].rearrange("p (r c) -> p r c", c=79)[:, :, 0:64]
                        return Upad[b][:, (r0 + k) * 64: (r0 + k) * 64 + 512]

                    def pview(p, ti):
                        t0, n = TT[ti]
                        if t0 < NX and rowo:
                            return P[p][:, :n].rearrange("p (r c) -> p r c", c=64)
                        return P[p][:, :n]

                    def emit_glu(j):
                        b = j % 2
                        ka, wa_, rwa_ = nextw()
                        kg, wg_, rwg_ = nextw()
                        for ti in range(len(conv_tiles)):
                            t0, n = TT[ti]
                            pa = nextp(0, 4)
                            S.group("pe", [(lambda kc=kc, pa=pa: T.matmul(P[pa][:, :n], wa_[:, kc, :], hT[:, kc, t0:t0 + n],
                                                                         start=(kc == 0), stop=(kc == 7))) for kc in range(8)],
                                    reads=[rwa_, rhT[ti]], writes=[rP[pa]])
                            pg = nextp(0, 4)
                            S.group("pe", [(lambda kc=kc, pg=pg: T.matmul(P[pg][:, :n], wg_[:, kc, :], hT[:, kc, t0:t0 + n],
                                                                         start=(kc == 0), stop=(kc == 7))) for kc in range(8)],
                                    reads=[rwg_, rhT[ti]], writes=[rP[pg]])
                            sb_ = ti % 2
                            S.op("act", lambda pg=pg, sb_=sb_: A.activation(sgt[sb_][:, :n], P[pg][:, :n], AF.Sigmoid),
                                 reads=[rP[pg]], writes=[rsgt[sb_]])
                            sv = sgt[sb_][:, :n]
                            if t0 < NX and rowo:
                                sv = sv.rearrange("p (r c) -> p r c", c=64)
                            S.op("dve", lambda pa=pa, sv=sv, ti=ti, b=b: V.tensor_tensor(uview(b, ti, 15), pview(pa, ti), sv, ALU.mult),
                                 reads=[rP[pa], rsgt[sb_]], writes=[rUp[b]])
                        W.after(kg)
                        S.op("dve", lambda b=b, j=j: V.tensor_tensor(
                            Dg[b][:], identb[:].unsqueeze(1).to_broadcast([128, 31, 128]),
                            cwb[:, j * 31:(j + 1) * 31].unsqueeze(2).to_broadcast([128, 31, 128]), ALU.mult),
                            reads=[rmisc, rcwb], writes=[rDg[b]])

                    def emit_conv(j):
                        b = j % 2
                        cb = vv("convb", l * 4 + j)
                        for ti in range(len(conv_tiles)):
                            t0, n = TT[ti]
                            p = nextp(4, 8)
                            S.group("pe", [(lambda k_=k_, p=p, ti=ti, b=b: T.matmul(pview(p, ti), Dg[b][:, k_, :], uview(b, ti, k_),
                                                                                 start=(k_ == 0), stop=(k_ == 30))) for k_ in range(31)],
                                    reads=[rDg[b], rUp[b]], writes=[rP[p]])
                            S.op("act", lambda p=p, j=j, cb=cb: A.activation(ymc[:, j, t0:t0 + n], P[p][:, :n], AF.Identity, bias=cb, scale=1.0),
                                 reads=[rP[p], rvecs], writes=[rym[j][ti]])
                    emit_glu(0)
                    for j in range(4):
                        if j + 1 < 4:
                            emit_glu(j + 1)
                        emit_conv(j)
                    S.barrier()
                with ExitStack() as eln:
                    sq4 = sbuf(eln, "sq4", [128, 4, 512], BF16)
                    mu = sbuf(eln, "mu", [128, 512], F32)
                    msq = sbuf(eln, "msq", [128, 512], F32)
                    rsd = sbuf(eln, "rsd", [128, 512], F32)
                    lt = [sbuf(eln, f"lt{i}", [128, 512], F32) for i in range(2)]
                    rsq4, rmu, rmsq, rrsd = (S.res() for _ in range(4))
                    rlt = S.grid(2)
                    for ti, (t0, n) in enumerate(TO):
                        for j in range(4):
                            S.op("act", lambda j=j: A.activation(sq4[:, j, :n], ymc[:, j, t0:t0 + n], AF.Square),
                                 reads=[rym[j][ti]], writes=[rsq4])
                        p1 = nextp(0, 4)
                        S.group("pe", [(lambda j=j, p1=p1: T.matmul(P[p1][:, :n], onesb[:], ymc[:, j, t0:t0 + n], start=(j == 0), stop=(j == 3)))
                                       for j in range(4)], reads=[rym[j][ti] for j in range(4)] + [rmisc], writes=[rP[p1]])
                        p2 = nextp(0, 4)
                        S.group("pe", [(lambda j=j, p2=p2: T.matmul(P[p2][:, :n], onesb[:], sq4[:, j, :n], start=(j == 0), stop=(j == 3)))
                                       for j in range(4)], reads=[rsq4, rmisc], writes=[rP[p2]])
                        S.op("act", lambda p1=p1: A.mul(mu[:, :n], P[p1][:, :n], 1.0 / 512), reads=[rP[p1]], writes=[rmu])
                        S.op("dve", lambda: V.tensor_tensor(msq[:, :n], mu[:, :n], mu[:, :n], ALU.mult), reads=[rmu], writes=[rmsq])
                        S.op("dve", lambda p2=p2: V.scalar_tensor_tensor(rsd[:, :n], P[p2][:, :n], 1.0 / 512, msq[:, :n], ALU.mult, ALU.subtract),
                             reads=[rP[p2], rmsq], writes=[rrsd])
                        S.op("act", lambda: A.activation(rsd[:, :n], rsd[:, :n], AF.Ln, bias=epsD[:, 1:2], scale=1.0),
                             reads=[rrsd, rmisc], writes=[rrsd])
                        S.op("act", lambda: A.activation(rsd[:, :n], rsd[:, :n], AF.Exp, scale=-0.5), reads=[rrsd], writes=[rrsd])
                        for j in range(4):
                            b = j % 2
                            S.op("dve", lambda j=j, b=b: V.tensor_tensor(lt[b][:, :n], ymc[:, j, t0:t0 + n], mu[:, :n], ALU.subtract),
                                 reads=[rym[j][ti], rmu], writes=[rlt[b]])
                            S.op("dve", lambda b=b: V.tensor_tensor(lt[b][:, :n], lt[b][:, :n], rsd[:, :n], ALU.mult),
                                 reads=[rlt[b], rrsd], writes=[rlt[b]])
                            S.op("act", lambda j=j, b=b: A.activation(ymc[:, j, t0:t0 + n], lt[b][:, :n], AF.Silu,
                                                                      bias=vv("lnb", l * 4 + j), scale=vv("lng", l * 4 + j)),
                                 reads=[rlt[b], rvecs], writes=[rym[j][ti]])
                    S.barrier()
                dump(f"ymc{l}", ymc[:].rearrange("p a b -> p (a b)"), [r for rr in rym[:4] for r in rr])
                if stop == "conv":
                    break
                W2 = WChunks(ecm, "o", [wrow(j * 128) for j in range(4)], "act", nst=2, nbf=4)
                W2.prefetch()
                wo4 = [W2.get(j) for j in range(4)]
                for dj in range(8):
                    for ti, (t0, n) in enumerate(TO):
                        col = 0 if t0 < NX else 1
                        p = nextp(0, 4)
                        S.group("pe", [(lambda j=j, p=p, dj=dj: T.matmul(P[p][:, :n], wo4[j][0][:, dj, :], ymc[:, j, t0:t0 + n],
                                                                        start=(j == 0), stop=(j == 3))) for j in range(4)],
                                reads=[w_[1] for w_ in wo4] + [rym[j][ti] for j in range(4)], writes=[rP[p]])
                        g1 = mod[:, 16 + dj, col:col + 1]
                        S.op("dve", lambda p=p, g1=g1, dj=dj: V.scalar_tensor_tensor(
                            xT[:, dj, t0:t0 + n], P[p][:, :n], g1, xT[:, dj, t0:t0 + n], ALU.mult, ALU.add),
                            reads=[rP[p], rmod, rxT[dj][ti]], writes=[rxT[dj][ti]])
                S.barrier()
            dump(f"xmix{l}", xT[:].rearrange("p a b -> p (a b)"), [r for rr in rxT for r in rr])
            if stop == "mix":
                break

            with ExitStack() as eo:
                comb = sbuf(eo, "comb", [128, 18, NE], F32)
                rcomb = S.res()
                with ExitStack() as er:
                    h2f = sbuf(er, "h2f", [128, 8, 512], F32)
                    sq = sbuf(er, "sq2", [128, 8, 512], BF16)
                    rstd = sbuf(er, "rstd2", [128, 512], F32)
                    tmp = [sbuf(er, f"nt2_{i}", [128, 512], F32) for i in range(2)]
                    rsq, rrstd = S.res(), S.res()
                    rtmp = S.grid(2)
                    rh2 = S.res()
                    pl = 7
                    for ti, (t0, n) in enumerate(TO):
                        if True:
                            col = 0 if t0 < NX else 1
                            for c in range(8):
                                S.op("act", lambda c=c: A.activation(sq[:, c, :n], xT[:, c, t0:t0 + n], AF.Square),
                                     reads=[rxT[c][ti]], writes=[rsq])
                            p = nextp(0, 4)
                            S.group("pe", [(lambda c=c, p=p: T.matmul(P[p][:, :n], onesb[:], sq[:, c, :n], start=(c == 0), stop=(c == 7)))
                                           for c in range(8)], reads=[rsq, rmisc], writes=[rP[p]])
                            S.op("act", lambda p=p: A.activation(rstd[:, :n], P[p][:, :n], AF.Ln, bias=epsD[:, 0:1], scale=1.0),
                                 reads=[rP[p], rmisc], writes=[rrstd])
                            S.op("act", lambda: A.activation(rstd[:, :n], rstd[:, :n], AF.Exp, scale=-0.5), reads=[rrstd], writes=[rrstd])
                            for c in range(8):
                                b = c % 2
                                S.op("dve", lambda c=c, b=b: V.tensor_tensor(tmp[b][:, :n], xT[:, c, t0:t0 + n], rstd[:, :n], ALU.mult),
                                     reads=[rxT[c][ti], rrstd], writes=[rtmp[b]])
                                sc_ap = se[:, 1, c, col:col + 1]
                                sh_ap = mod[:, 24 + c, col:col + 1]
                                S.op("act", lambda c=c, b=b, sc_ap=sc_ap, sh_ap=sh_ap: A.activation(
                                    h2f[:, c, :n], tmp[b][:, :n], AF.Identity, bias=sh_ap, scale=sc_ap),
                                    reads=[rtmp[b], rse, rmod], writes=[rh2])
                                if False:
                                    S.op("pool", lambda c=c: G.tensor_copy(hT[:, c, t0:t0 + n], h2f[:, c, :n]),
                                         reads=[rh2], writes=[rhT[ti]])
                                else:
                                    S.op("act", lambda c=c, b=b, sc_ap=sc_ap, sh_ap=sh_ap: A.activation(
                                        hT[:, c, t0:t0 + n], tmp[b][:, :n], AF.Identity, bias=sh_ap, scale=sc_ap),
                                        reads=[rtmp[b], rse, rmod], writes=[rhT[ti]])
                            fns = []
                            for s_ in range(n // 128):
                                tt = t0 // 128 + s_
                                for kc in range(8):
                                    fns.append(lambda s_=s_, tt=tt, kc=kc: T.matmul(
                                        P[pl][:, tt * NE:(tt + 1) * NE], h2f[:, kc, s_ * 128:(s_ + 1) * 128], rw[:, kc, :],
                                        start=(kc == 0), stop=(kc == 7)))
                            S.group("pe", fns, reads=[rh2, rrw], writes=[rP[pl]])
                    ntl = sum(n for _, n in TO) // 128
                    NG = ntl * 4
                    sg_ = sbuf(er, "r_s", [128, ntl, NE], F32)
                    sbb = sbuf(er, "r_sb", [128, ntl, NE], F32)
                    ps6 = sbuf(er, "r_p6", [128, NG, 6], F32)
                    gs = sbuf(er, "r_gs", [128, NG], F32)
                    gm = sbuf(er, "r_gm", [128, ntl], F32)
                    ing = sbuf(er, "r_ing", [128, NG], F32)
                    sbm = sbuf(er, "r_sbm", [128, ntl, NE], F32)
                    sel = sbuf(er, "r_sel", [128, ntl, NE], F32)
                    m1 = sbuf(er, "r_m1", [128, ntl], F32)
                    rr_ = S.res()
                    R = dict(reads=[rr_], writes=[rr_])
                    S.op("act", lambda: A.activation(sg_[:].rearrange("p a b -> p (a b)"), P[pl][:, :ntl * NE], AF.Sigmoid),
                         reads=[rP[pl]], writes=[rr_])
                    rb_bc = vecs[:, VOFF["rbias"]:VOFF["rbias"] + NE].unsqueeze(1).to_broadcast([128, ntl, NE])
                    S.op("dve", lambda: V.tensor_tensor(sbb[:], sg_[:], rb_bc, ALU.add), reads=[rr_, rvecs], writes=[rr_])
                    g4v = sbb[:].rearrange("p a (g e) -> p (a g) e", e=4)
                    S.op("dve", lambda: V.tensor_tensor(ps6[:, :, 0:2], g4v[:, :, 0:4:2], g4v[:, :, 1:4:2], ALU.add), **R)
                    S.op("dve", lambda: V.tensor_tensor(ps6[:, :, 2:4], g4v[:, :, 0:2], g4v[:, :, 2:4], ALU.add), **R)
                    S.op("dve", lambda: V.tensor_tensor(ps6[:, :, 4:5], g4v[:, :, 0:1], g4v[:, :, 3:4], ALU.add), **R)
                    S.op("dve", lambda: V.tensor_tensor(ps6[:, :, 5:6], g4v[:, :, 1:2], g4v[:, :, 2:3], ALU.add), **R)
                    S.op("dve", lambda: V.tensor_reduce(gs[:], ps6[:], AX.X, ALU.max), **R)
                    S.op("dve", lambda: V.tensor_reduce(gm[:], gs[:].rearrange("p (a g) -> p a g", g=4), AX.X, ALU.max), **R)
                    S.op("dve", lambda: V.tensor_tensor(ing[:].rearrange("p (a g) -> p a g", g=4), gs[:].rearrange("p (a g) -> p a g", g=4),
                                                        gm[:].unsqueeze(2).to_broadcast([128, ntl, 4]), ALU.is_equal), **R)
                    S.op("dve", lambda: V.tensor_scalar(ing[:], ing[:], BIG, -BIG, ALU.mult, ALU.add), **R)
                    S.op("dve", lambda: V.tensor_tensor(sbm[:].rearrange("p a (g e) -> p (a g) e", e=4), g4v,
                                                        ing[:].unsqueeze(2).to_broadcast([128, NG, 4]), ALU.add), **R)
                    S.op("dve", lambda: V.tensor_reduce(m1[:], sbm[:], AX.X, ALU.max), **R)
                    S.op("dve", lambda: V.tensor_tensor(sel[:], sbm[:], m1[:].unsqueeze(2).to_broadcast([128, ntl, NE]), ALU.is_equal), **R)
                    S.op("dve", lambda: V.scalar_tensor_tensor(sbm[:], sel[:], -BIG, sbm[:], ALU.mult, ALU.add), **R)
                    S.op("dve", lambda: V.tensor_reduce(m1[:], sbm[:], AX.X, ALU.max), **R)
                    S.op("dve", lambda: V.tensor_tensor(sbm[:], sbm[:], m1[:].unsqueeze(2).to_broadcast([128, ntl, NE]), ALU.is_ge), **R)
                    S.op("dve", lambda: V.tensor_tensor(sel[:], sel[:], sbm[:], ALU.add), **R)
                    S.op("dve", lambda: V.tensor_tensor(sel[:], sel[:], sg_[:], ALU.mult), **R)
                    S.op("dve", lambda: V.tensor_reduce(m1[:], sel[:], AX.X, ALU.add), **R)
                    S.op("dve", lambda: V.reciprocal(m1[:], m1[:]), **R)
                    S.op("dve", lambda: V.tensor_tensor(comb[:, :ntl, :], sel[:], m1[:].unsqueeze(2).to_broadcast([128, ntl, NE]), ALU.mult),
                         reads=[rr_], writes=[rcomb])
                    S.barrier()
                comb_hi = sbuf(eo, "comb_hi", [128, 18, NE], BF16)
                comb_lo = sbuf(eo, "comb_lo", [128, 18, NE], BF16)
                comb_r = sbuf(eo, "comb_r", [128, 18, NE], F32)
                S.op("dve", lambda: V.tensor_copy(comb_hi[:], comb[:]), reads=[rcomb], writes=[rcomb])
                S.op("dve", lambda: V.tensor_tensor(comb_r[:], comb[:], comb_hi[:], ALU.subtract), reads=[rcomb], writes=[rcomb])
                S.op("dve", lambda: V.tensor_copy(comb_lo[:], comb_r[:]), reads=[rcomb], writes=[rcomb])
                dump(f"comb{l}", comb[:].rearrange("p a b -> p (a b)"), [rcomb])
                dump(f"h2_{l}", hT[:].rearrange("p a b -> p (a b)"), rhT)
                if stop == "router":
                    break
                NU = NE * 4
                NST, NBF = 4, 2
                stg = [sbuf(eo, f"stg{i}", [128, 2048], F32) for i in range(NST)]
                wbf = [sbuf(eo, f"wbf{i}", [128, 3, 2048], BF16) for i in range(NBF)]
                cg = sbuf(eo, "cg", [128, NT], F32)
                actb = [sbuf(eo, f"actb{i}", [128, 2, 512], BF16) for i in range(2)]
                sgm = [sbuf(eo, f"sgm{i}", [128, 512], F32) for i in range(2)]
                t1m = [sbuf(eo, f"t1m{i}", [128, 512], F32) for i in range(2)]
                rstg = S.grid(NST)
                rwbf = S.grid(NBF, 3)
                rcg = S.res()
                ractb, rsgm, rt1m = S.grid(2), S.grid(2), S.grid(2)
                dstg = [S.dsem() for _ in range(NST)]
                pieces = []
                for e in range(NE):
                    for q in range(4):
                        f0 = q * 256
                        pieces.append((wg_d[l, e, :, f0:f0 + 256].rearrange("(kc p) f -> p kc f", p=128), 0))
                        pieces.append((wu_d[l, e, :, f0:f0 + 256].rearrange("(kc p) f -> p kc f", p=128), 1))
                        pieces.append((wd_d[l, e, f0:f0 + 256, :].rearrange("(fc p) d -> p fc d", p=128), 2))
                pl_, pc_ = [0], [0]

                def p_load():
                    if pl_[0] >= len(pieces):
                        return
                    i = pl_[0] % NST
                    src, kind = pieces[pl_[0]]
                    dst = stg[i][:].rearrange("p (a b) -> p a b", a=(8 if kind < 2 else 2))
                    S.dma("sp", dst, src, dstg[i], writes=[rstg[i]])
                    pl_[0] += 1

                def p_cast():
                    if pc_[0] >= len(pieces):
                        return
                    k = pc_[0]
                    i = k % NST
                    u, kind = k // 3, k % 3
                    j = u % NBF
                    S.op("act", lambda i=i, j=j, kind=kind: A.copy(wbf[j][:, kind, :], stg[i][:]),
                         reads=[rstg[i]], writes=[rwbf[j][kind]])
                    pc_[0] += 1
                    p_load()
                for _ in range(NST):
                    p_load()
                for _ in range(3):
                    p_cast()
                def emit_cg(e):
                    for g4 in range((ntl + 3) // 4):
                        tts = list(range(g4 * 4, min(g4 * 4 + 4, ntl)))
                        p = 7
                        fns = []
                        for tt in tts:
                            for hl, cb_ in enumerate((comb_hi, comb_lo)):
                                fns.append(lambda tt=tt, hl=hl, cb_=cb_: T.matmul(
                                    P[p][:, (tt % 4) * 128:(tt % 4 + 1) * 128],
                                    cb_[:, tt, e:e + 1].to_broadcast([128, 128]), identb[:], start=(hl == 0), stop=(hl == 1)))
                        S.group("pe", fns, reads=[rcomb, rmisc], writes=[rP[p]])
                        nn = len(tts) * 128
                        S.op("dve", lambda p=p, g4=g4, nn=nn: V.tensor_copy(cg[:, g4 * 512:g4 * 512 + nn], P[p][:, :nn]),
                             reads=[rP[p]], writes=[rcg])

                def emit_gu(u, ti, ab):
                    e, q = u // 4, u % 4
                    j = u % NBF
                    t0, n = TO[ti]
                    wgt = wbf[j][:, 0, :].rearrange("p (kc f) -> p kc f", kc=8)
                    wut = wbf[j][:, 1, :].rearrange("p (kc f) -> p kc f", kc=8)
                    if q == 0 and ti == 0:
                        emit_cg(e)
                    for fc in range(2):
                        pg = nextp(0, 4)
                        S.group("pe", [(lambda kc=kc, pg=pg, fc=fc: T.matmul(P[pg][:, :n], wgt[:, kc, fc * 128:(fc + 1) * 128],
                                                                           hT[:, kc, t0:t0 + n], start=(kc == 0), stop=(kc == 7)))
                                       for kc in range(8)], reads=[rwbf[j][0], rhT[ti]], writes=[rP[pg]])
                        pu = nextp(0, 4)
                        S.group("pe", [(lambda kc=kc, pu=pu, fc=fc: T.matmul(P[pu][:, :n], wut[:, kc, fc * 128:(fc + 1) * 128],
                                                                           hT[:, kc, t0:t0 + n], start=(kc == 0), stop=(kc == 7)))
                                       for kc in range(8)], reads=[rwbf[j][1], rhT[ti]], writes=[rP[pu]])
                        S.op("act", lambda pg=pg, fc=fc: A.activation(sgm[fc][:, :n], P[pg][:, :n], AF.Silu),
                             reads=[rP[pg]], writes=[rsgm[fc]])
                        S.op("dve", lambda pu=pu, fc=fc: V.tensor_tensor(t1m[fc][:, :n], P[pu][:, :n], sgm[fc][:, :n], ALU.mult),
                             reads=[rP[pu], rsgm[fc]], writes=[rt1m[fc]])
                        S.op("pool", lambda fc=fc, ab=ab: G.tensor_tensor(actb[ab][:, fc, :n], t1m[fc][:, :n], cg[:, t0:t0 + n], ALU.mult),
                             reads=[rt1m[fc], rcg], writes=[ractb[ab]])

                def emit_dn(u, ti, ab):
                    j = u % NBF
                    t0, n = TO[ti]
                    col = 0 if t0 < NX else 1
                    wdt = wbf[j][:, 2, :].rearrange("p (fc d) -> p fc d", fc=2)
                    for dj in range(8):
                        po = nextp(4, 7)
                        S.group("pe", [(lambda fc=fc, po=po, dj=dj, ab=ab: T.matmul(
                            P[po][:, :n], wdt[:, fc, dj * 128:(dj + 1) * 128], actb[ab][:, fc, :n], start=(fc == 0), stop=(fc == 1)))
                            for fc in range(2)], reads=[rwbf[j][2], ractb[ab]], writes=[rP[po]])
                        g2 = mod[:, 40 + dj, col:col + 1]
                        S.op("dve", lambda po=po, g2=g2, dj=dj: V.scalar_tensor_tensor(
                            xT[:, dj, t0:t0 + n], P[po][:, :n], g2, xT[:, dj, t0:t0 + n], ALU.mult, ALU.add),
                            reads=[rP[po], rmod, rxT[dj][ti]], writes=[rxT[dj][ti]])

                items = [(u, ti) for u in range(NU) for ti in range(len(TO))]
                prev = None
                for k_, (u, ti) in enumerate(items):
                    emit_gu(u, ti, k_ % 2)
                    if not MOE_PIPE:
                        emit_dn(u, ti, k_ % 2)
                    elif prev is not None:
                        emit_dn(*prev)
                    if ti == 0:
                        for _ in range(3):
                            p_cast()
                    prev = (u, ti, k_ % 2)
                if MOE_PIPE:
                    emit_dn(*prev)
                S.barrier()
            dump(f"xout{l}", xT[:].rearrange("p a b -> p (a b)"), [r for rr in rxT for r in rr])

        if stop is None:
            with ExitStack() as ef:
                sq = sbuf(ef, "sqf", [128, 8, 512], BF16)
                rstd = sbuf(ef, "rstdf", [128, 512], F32)
                tmp = [sbuf(ef, f"ntf{i}", [128, 512], F32) for i in range(2)]
                yT = sbuf(ef, "yT", [128, 8, 512], F32)
                yo = [sbuf(ef, f"yo{i}", [128, D], F32) for i in range(2)]
                gfs = sbuf(ef, "gfs", [128, 8], F32)
                rsq, rrstd, ryT, rgfs = (S.res() for _ in range(4))
                rtmp, ryo = S.grid(2), S.grid(2)
                dyo = [S.dsem(), S.dsem()]
                S.op("dve", lambda: V.tensor_scalar(gfs[:], vecs[:, VOFF["fing"]:VOFF["fing"] + 8], float(np.sqrt(D)), None, ALU.mult),
                     reads=[rvecs], writes=[rgfs])
                for ti, (t0, n) in enumerate(TT[:4]):
                    for c in range(8):
                        S.op("act", lambda c=c: A.activation(sq[:, c, :], xT[:, c, t0:t0 + n], AF.Square), reads=[rxT[c][ti]], writes=[rsq])
                    p = nextp(0, 4)
                    S.group("pe", [(lambda c=c, p=p: T.matmul(P[p][:], onesb[:], sq[:, c, :], start=(c == 0), stop=(c == 7)))
                                   for c in range(8)], reads=[rsq, rmisc], writes=[rP[p]])
                    S.op("act", lambda p=p: A.activation(rstd[:], P[p][:], AF.Ln, bias=epsD[:, 0:1], scale=1.0),
                         reads=[rP[p], rmisc], writes=[rrstd])
                    S.op("act", lambda: A.activation(rstd[:], rstd[:], AF.Exp, scale=-0.5), reads=[rrstd], writes=[rrstd])
                    for c in range(8):
                        b = c % 2
                        S.op("dve", lambda c=c, b=b: V.tensor_tensor(tmp[b][:], xT[:, c, t0:t0 + n], rstd[:], ALU.mult),
                             reads=[rxT[c][ti], rrstd], writes=[rtmp[b]])
                        S.op("act", lambda c=c, b=b: A.activation(yT[:, c, :], tmp[b][:], AF.Copy, scale=gfs[:, c:c + 1]),
                             reads=[rtmp[b], rgfs], writes=[ryT])
                    for j in range(4):
                        tt = ti * 4 + j
                        b = tt % 2
                        for half in range(2):
                            pp = nextp(4, 8)
                            S.group("pe", [(lambda c=c, pp=pp, j=j: T.transpose(P[pp][:, (c % 4) * 128:(c % 4 + 1) * 128],
                                                                               yT[:, c, j * 128:(j + 1) * 128], ident))
                                           for c in range(half * 4, half * 4 + 4)], reads=[ryT, rcst], writes=[rP[pp]])
                            if half == 0:
                                S.op("act", lambda pp=pp, b=b: A.copy(yo[b][:, 0:512], P[pp][:]), reads=[rP[pp]], writes=[ryo[b]])
                            else:
                                S.op("dve", lambda pp=pp, b=b: V.tensor_copy(yo[b][:, 512:1024], P[pp][:]), reads=[rP[pp]], writes=[ryo[b]])
                        S.dma("sp", y_d[tt * 128:(tt + 1) * 128, :], yo[b][:], dyo[b], reads=[ryo[b]])
                S.barrier()
        S.barrier()
    return nc


def _chunked(v, n):
    return np.ascontiguousarray(np.asarray(v, np.float32).reshape(n, 128).T)


def make_consts():
    c = np.zeros((128, NCST), np.float32)
    c[:, 0:128] = np.eye(128, dtype=np.float32)
    s = np.arange(128)[:, None]
    t = np.arange(128)[None, :]
    same = (s // CH) == (t // CH)
    c[:, 128:256] = (same & (s <= t)).astype(np.float32)
    c[:, 256:384] = (same & (s >= t)).astype(np.float32)
    m = np.ones(512, np.float32)
    m[::CH] = 0.0
    c[:, 384:896] = m[None, :]
    for i in range(4):
        c[32 * i:32 * i + 32, 896 + i] = 1.0
    return c


def make_in_maps(inputs, cores):
    f = lambda k: np.asarray(inputs[k], np.float32)
    vecs = np.zeros((128, NV), np.float32)

    def put(name, arr):
        arr = np.asarray(arr, np.float32)
        vecs[:, VOFF[name]:VOFF[name] + arr.shape[1]] = arr
    put("n1g", np.concatenate([_chunked(f("norm1_g")[l], 8) for l in range(DEPTH)], 1))
    put("n2g", np.concatenate([_chunked(f("norm2_g")[l], 8) for l in range(DEPTH)], 1))
    put("fing", _chunked(f("final_norm_g"), 8))
    put("convb", np.concatenate([_chunked(f("conv_b")[l], 4) for l in range(DEPTH)], 1))
    put("lng", np.concatenate([_chunked(f("conv_ln_g")[l], 4) for l in range(DEPTH)], 1))
    put("lnb", np.concatenate([_chunked(f("conv_ln_b")[l], 4) for l in range(DEPTH)], 1))
    put("hg", np.stack([f("hgrn_norm_g")[l] for l in range(DEPTH)], 1))
    put("lbf", np.concatenate([_chunked(f("lb_fwd")[l], 4) for l in range(DEPTH)], 1))
    put("lbb", np.concatenate([_chunked(f("lb_bwd")[l], 4) for l in range(DEPTH)], 1))
    cw = f("conv_w")
    cwr = cw.reshape(DEPTH, 31, 4, 128).transpose(3, 0, 2, 1).reshape(128, DEPTH * 4 * 31)
    put("convw", cwr)
    put("rbias", np.broadcast_to(f("router_bias")[None, :], (128, NE)))
    bada = np.stack([_chunked(f("b_ada")[l], 48) for l in range(DEPTH)], 0)
    rwr = np.ascontiguousarray(f("router_w").reshape(8, 128, NE).transpose(1, 0, 2).reshape(128, 8 * NE))
    consts = make_consts()
    shared = {"w_ada": f("w_ada"), "b_ada_r": bada, "vecs": vecs, "consts": consts, "w_in": f("w_in"),
              "w_out": f("w_out"), "router_w_r": rwr, "w_gate": f("w_gate"), "w_up": f("w_up"), "w_down": f("w_down")}
    maps = []
    cc = f("c_ctx")
    for b in cores:
        c2 = np.stack([_chunked(f("c")[b], 8), _chunked(cc, 8)], 2).reshape(128, 16)
        m = dict(shared)
        m["x"] = np.ascontiguousarray(f("x")[b])
        m["ctx"] = np.ascontiguousarray(f("ctx")[b])
        m["c2"] = np.ascontiguousarray(c2)
        maps.append(m)
    return maps


_NC_CACHE = {}


def kernel(**inputs):
    if "nc" not in _NC_CACHE:
        _NC_CACHE["nc"] = build_program()
    nc = _NC_CACHE["nc"]
    maps = make_in_maps(inputs, list(range(8)))
    res = run_bass_kernel_spmd(nc, maps, core_ids=list(range(8)))
    return np.stack([np.asarray(r["y"], np.float32) for r in res.results], 0)
```

```python
import numpy as np
from contextlib import ExitStack, suppress
import concourse.bass as bass
import concourse.mybir as mybir
from concourse.bass_utils import run_bass_kernel_spmd

F32, BF16 = mybir.dt.float32, mybir.dt.bfloat16
AF = mybir.ActivationFunctionType
ALU = mybir.AluOpType
AX = mybir.AxisListType

D = 1024
NX = 2048
NCX = 256
NT = NX + NCX
DEPTH = 2
NE = 16
EPS = 1e-6
CH = 32
NCHK = NT // CH
TT = [(0, 512), (512, 512), (1024, 512), (1536, 512), (2048, 256)]
BIG = 1.0e4
MOE_PIPE = True
FUSE_WAITS = True

VOFF = {}
_o = 0
for _n, _w in (("n1g", 16), ("n2g", 16), ("fing", 8), ("convb", 8), ("lng", 8), ("lnb", 8),
               ("hg", 2), ("lbf", 8), ("lbb", 8), ("convw", 2 * 4 * 31), ("rbias", 16)):
    VOFF[_n] = _o
    _o += _w
NV = _o
COFF = {"ident": 0, "maskF": 128, "maskB": 256, "smask": 384}
NCST = 384 + 512 + 4


class Res:
    __slots__ = ("w", "r")

    def __init__(self):
        self.w = None
        self.r = {}


class Stream:
    def __init__(self, eng, sem, key):
        self.eng, self.sem, self.key = eng, sem, key
        self.count = 0
        self.waited = {}


class DSem:
    def __init__(self, handle, key):
        self.handle, self.key, self.count = handle, key, 0


class Sched:
    def __init__(self, nc, es):
        self.nc, self.es = nc, es
        self.semh = {}
        self.streams = {}
        for name, eng in (("pe", nc.tensor), ("act", nc.scalar), ("dve", nc.vector),
                          ("pool", nc.gpsimd), ("sp", nc.sync)):
            h = es.enter_context(nc.semaphore("s_" + name))
            self.semh[name] = h
            self.streams[name] = Stream(eng, h, name)
        self.dsems = []
        self.hist = {name: {} for name in self.streams}
        self.n_wait = 0
        self.n_fused = 0

    def res(self):
        return Res()

    def grid(self, *dims):
        if len(dims) == 1:
            return [Res() for _ in range(dims[0])]
        return [self.grid(*dims[1:]) for _ in range(dims[0])]

    def dsem(self):
        key = f"d{len(self.dsems)}"
        h = self.es.enter_context(self.nc.semaphore("s_" + key))
        self.semh[key] = h
        d = DSem(h, key)
        self.dsems.append(d)
        return d

    def _deps(self, st, reads, writes, fuse=False):
        deps = {}

        def add(tok, same_ok):
            if tok is None:
                return
            k, v = tok
            if k == st.key and not same_ok:
                return
            if deps.get(k, 0) < v:
                deps[k] = v
        for r in reads:
            add(r.w, True)
        for w in writes:
            add(w.w, False)
            for k, v in w.r.items():
                add((k, v), False)
        need = []
        for k, v in sorted(deps.items(), key=lambda kv: -kv[1]):
            if st.waited.get(k, 0) < v:
                st.waited[k] = v
                need.append((k, v))
                h = self.hist.get(k)
                if h is not None and v in h:
                    for k2, v2 in h[v].items():
                        if k2 != st.key and st.waited.get(k2, 0) < v2:
                            st.waited[k2] = v2
        need = [(k, v) for k, v in need if True]
        self.n_wait += len(need)
        if fuse and need:
            for k, v in need[:-1]:
                st.eng.wait_ge(self.semh[k], v)
            return need[-1]
        for k, v in need:
            st.eng.wait_ge(self.semh[k], v)
        return None

    def _snap(self, st):
        self.hist[st.key][st.count] = dict(st.waited)

    def _mark(self, key, val, reads, writes):
        for r in reads:
            r.r[key] = val
        for w in writes:
            w.w = (key, val)
            w.r = {}

    def _attach(self, st, ins, w):
        if w is not None:
            ins._wait_ge(self.semh[w[0]], st.eng.lower_val(w[1]))
            self.n_fused += 1
        return ins

    def op(self, sname, fn, reads=(), writes=()):
        st = self.streams[sname]
        w = self._deps(st, reads, writes, fuse=FUSE_WAITS)
        st.count += 1
        self._snap(st)
        self._attach(st, fn(), w).then_inc(st.sem, 1)
        self._mark(st.key, st.count, reads, writes)

    def group(self, sname, fns, reads=(), writes=()):
        st = self.streams[sname]
        w = self._deps(st, reads, writes, fuse=FUSE_WAITS)
        st.count += 1
        self._snap(st)
        n = len(fns)
        for i, fn in enumerate(fns):
            ins = fn()
            if i == 0:
                self._attach(st, ins, w)
            if i == n - 1:
                ins.then_inc(st.sem, 1)
        self._mark(st.key, st.count, reads, writes)

    def dma(self, sname, out, in_, ds, reads=(), writes=()):
        st = self.streams[sname]
        self._deps(st, reads, writes)
        ds.count += 16
        st.eng.dma_start(out=out, in_=in_).then_inc(ds.handle, 16)
        self._mark(ds.key, ds.count, reads, writes)

    def barrier(self):
        for st in self.streams.values():
            for o in self.streams.values():
                if o is not st and o.count > st.waited.get(o.key, 0):
                    st.waited[o.key] = o.count
                    st.eng.wait_ge(o.sem, o.count)
            for d in self.dsems:
                if d.count > st.waited.get(d.key, 0):
                    st.waited[d.key] = d.count
                    st.eng.wait_ge(d.handle, d.count)


class _Stop(Exception):
    pass


def build_program(nlayers=DEPTH, dbg=None, stop=None):
    dbg = dbg or {}
    nc = bass.Bass("TRN2", target_bir_lowering=False)
    dr = lambda n, s, k="ExternalInput": nc.dram_tensor(n, s, F32, kind=k).ap()
    x_d = dr("x", [NX, D])
    ctx_d = dr("ctx", [NCX, D])
    c2_d = dr("c2", [128, 16])
    wada_d = dr("w_ada", [DEPTH, D, 6 * D])
    bada_d = dr("b_ada_r", [DEPTH, 128, 48])
    vecs_d = dr("vecs", [128, NV])
    cst_d = dr("consts", [128, NCST])
    win_d = dr("w_in", [DEPTH, D, 3584])
    wout_d = dr("w_out", [DEPTH, D, D])
    rw_d = dr("router_w_r", [128, 8 * NE])
    wg_d = dr("w_gate", [DEPTH, NE, D, D])
    wu_d = dr("w_up", [DEPTH, NE, D, D])
    wd_d = dr("w_down", [DEPTH, NE, D, D])
    y_d = dr("y", [NX, D], "ExternalOutput")
    dbg_d = {k: nc.dram_tensor("dbg_" + k, list(s[0]), BF16 if s[1] == "bf16" else F32, kind="ExternalOutput").ap()
             for k, s in dbg.items()}

    es = ExitStack()
    with suppress(_Stop), es:
        S = Sched(nc, es)
        V, A, G, T = nc.vector, nc.scalar, nc.gpsimd, nc.tensor

        nsb = [0]

        def sbuf(es_, name, shape, dt):
            nsb[0] += 1
            return es_.enter_context(nc.sbuf_tensor(f"sb{nsb[0]}_{name}", shape, dt))

        xT = sbuf(es, "xT", [128, 8, NT], F32)
        hT = sbuf(es, "hT", [128, 8, NT], BF16)
        cst = sbuf(es, "cst", [128, NCST], F32)
        vecs = sbuf(es, "vecs", [128, NV], F32)
        identb = sbuf(es, "identb", [128, 128], BF16)
        onesb = sbuf(es, "onesb", [128, 128], BF16)
        zerob = sbuf(es, "zerob", [128, 128], BF16)
        epsD = sbuf(es, "epsD", [128, 3], F32)
        mod = sbuf(es, "mod", [128, 48, 2], F32)
        se = sbuf(es, "se", [128, 2, 8, 2], F32)
        lbt = sbuf(es, "lbt", [128, 2, 4, 3], F32)
        rw = sbuf(es, "rw", [128, 8, NE], F32)
        P = [es.enter_context(nc.psum_tensor(f"ps{i}", [128, 512], F32)) for i in range(8)]
        rP = S.grid(8)
        rxT = S.grid(8, 5)
        rhT = S.grid(5)
        rcst, rvecs, rmisc, rmod, rse, rlbt, rrw = (S.res() for _ in range(7))
        ident = cst[:, 0:128]
        maskF = cst[:, 128:256]
        maskB = cst[:, 256:384]
        smask = cst[:, 384:896]
        dconst = S.dsem()
        vv = lambda n, i: vecs[:, VOFF[n] + i: VOFF[n] + i + 1]

        S.dma("sp", cst[:], cst_d[:, :], S.dsem(), writes=[rcst])
        S.dma("sp", vecs[:], vecs_d[:, :], S.dsem(), writes=[rvecs])
        S.dma("sp", rw[:].rearrange("p a b -> p (a b)"), rw_d[:, :], S.dsem(), writes=[rrw])
        S.op("dve", lambda: V.memset(onesb[:], 1.0), writes=[rmisc])
        S.op("dve", lambda: V.memset(zerob[:], 0.0), writes=[rmisc])
        S.op("dve", lambda: V.memset(epsD[:, 0:1], float(D * EPS)), writes=[rmisc])
        S.op("dve", lambda: V.memset(epsD[:, 1:3], float(EPS)), writes=[rmisc])
        S.op("dve", lambda: V.tensor_copy(identb[:], cst[:, 0:128]), reads=[rcst], writes=[rmisc])

        pctr = [0]

        def nextp(lo, hi):
            p = lo + pctr[0] % (hi - lo)
            pctr[0] += 1
            return p

        def dump(name, ap_sb, res_list):
            if name in dbg_d:
                ds = S.dsem()
                S.dma("sp", dbg_d[name], ap_sb, ds, reads=res_list)

        with ExitStack() as ea:
            xin = [sbuf(ea, f"xin{i}", [128, D], F32) for i in range(2)]
            rxin = S.grid(2)
            dxin = [S.dsem(), S.dsem()]
            for tt in range(18):
                b = tt % 2
                src = x_d[tt * 128:(tt + 1) * 128, :] if tt < 16 else ctx_d[(tt - 16) * 128:(tt - 15) * 128, :]
                S.dma("sp", xin[b][:], src, dxin[b], writes=[rxin[b]])
                for half in range(2):
                    p = nextp(0, 4)
                    fns = [(lambda c=c, p=p, b=b: T.transpose(P[p][:, (c % 4) * 128:(c % 4 + 1) * 128],
                                                              xin[b][:, c * 128:(c + 1) * 128], ident))
                           for c in range(half * 4, half * 4 + 4)]
                    S.group("pe", fns, reads=[rxin[b], rcst], writes=[rP[p]])
                    ws = [rxT[c][tt // 4] for c in range(half * 4, half * 4 + 4)]
                    eng = "dve" if half == 0 else "act"
                    dst = xT[:, half * 4:half * 4 + 4, tt * 128:(tt + 1) * 128]
                    srcp = P[p][:].rearrange("p (c t) -> p c t", c=4)
                    if eng == "dve":
                        S.op("dve", lambda dst=dst, srcp=srcp: V.tensor_copy(dst, srcp), reads=[rP[p]], writes=ws)
                    else:
                        S.op("act", lambda dst=dst, srcp=srcp: A.copy(dst, srcp), reads=[rP[p]], writes=ws)
            S.barrier()

        allx = lambda ti: [rxT[c][ti] for c in range(8)]

        def norm_mod(nidx, tiles, h2f=None, rh2f=None, es_=None):
            sq = sbuf(es_, f"sq{nidx}", [128, 8, 512], BF16)
            rstd = sbuf(es_, f"rstd{nidx}", [128, 512], F32)
            tmp = [sbuf(es_, f"nt{nidx}_{i}", [128, 512], F32) for i in range(2)]
            rsq, rrstd = S.res(), S.res()
            rtmp = S.grid(2)
            shb = 0 if nidx == 0 else 24
            for ti, (t0, n) in enumerate(tiles):
                col = 0 if t0 < NX else 1
                for c in range(8):
                    S.op("act", lambda c=c: A.activation(sq[:, c, :n], xT[:, c, t0:t0 + n], AF.Square),
                         reads=[rxT[c][ti]], writes=[rsq])
                p = nextp(0, 4)
                S.group("pe", [(lambda c=c, p=p: T.matmul(P[p][:, :n], onesb[:], sq[:, c, :n], start=(c == 0), stop=(c == 7)))
                               for c in range(8)], reads=[rsq, rmisc], writes=[rP[p]])
                S.op("act", lambda p=p: A.activation(rstd[:, :n], P[p][:, :n], AF.Ln, bias=epsD[:, 0:1], scale=1.0),
                     reads=[rP[p], rmisc], writes=[rrstd])
                S.op("act", lambda: A.activation(rstd[:, :n], rstd[:, :n], AF.Exp, scale=-0.5), reads=[rrstd], writes=[rrstd])
                for c in range(8):
                    b = c % 2
                    S.op("dve", lambda c=c, b=b: V.tensor_tensor(tmp[b][:, :n], xT[:, c, t0:t0 + n], rstd[:, :n], ALU.mult),
                         reads=[rxT[c][ti], rrstd], writes=[rtmp[b]])
                    sc_ap = se[:, nidx, c, col:col + 1]
                    sh_ap = mod[:, shb + c, col:col + 1]
                    if h2f is None:
                        S.op("act", lambda c=c, b=b, sc_ap=sc_ap, sh_ap=sh_ap: A.activation(
                            hT[:, c, t0:t0 + n], tmp[b][:, :n], AF.Identity, bias=sh_ap, scale=sc_ap),
                            reads=[rtmp[b], rse, rmod], writes=[rhT[ti]])
                    else:
                        S.op("act", lambda c=c, b=b, sc_ap=sc_ap, sh_ap=sh_ap: A.activation(
                            h2f[:, c, t0:t0 + n], tmp[b][:, :n], AF.Identity, bias=sh_ap, scale=sc_ap),
                            reads=[rtmp[b], rse, rmod], writes=[rh2f[ti]])
                        S.op("pool", lambda c=c: G.tensor_copy(hT[:, c, t0:t0 + n], h2f[:, c, t0:t0 + n]),
                             reads=[rh2f[ti]], writes=[rhT[ti]])

        class WChunks:
            def __init__(self, es_, tag, srcs, ceng, nst=3, nbf=3):
                self.srcs, self.ceng = srcs, ceng
                self.st = [sbuf(es_, f"wst{tag}{i}", [128, 8, 128], F32) for i in range(nst)]
                self.bf = [sbuf(es_, f"wbf{tag}{i}", [128, 8, 128], BF16) for i in range(nbf)]
                self.rst, self.rbf = S.grid(nst), S.grid(nbf)
                self.dst = [S.dsem() for _ in range(nst)]
                self.nl = 0
                self.nc_ = 0
                self.look = nbf - 1

            def _load(self):
                if self.nl >= len(self.srcs):
                    return
                i = self.nl % len(self.st)
                S.dma("sp", self.st[i][:], self.srcs[self.nl], self.dst[i], writes=[self.rst[i]])
                self.nl += 1

            def _cast(self):
                if self.nc_ >= len(self.srcs):
                    return
                i = self.nc_ % len(self.st)
                j = self.nc_ % len(self.bf)
                src, dst = self.st[i], self.bf[j]
                if self.ceng == "pool":
                    S.op("pool", lambda: G.tensor_copy(dst[:], src[:]), reads=[self.rst[i]], writes=[self.rbf[j]])
                else:
                    S.op("act", lambda: A.copy(dst[:], src[:]), reads=[self.rst[i]], writes=[self.rbf[j]])
                self.nc_ += 1

            def prefetch(self):
                for _ in range(len(self.st)):
                    self._load()
                for _ in range(self.look):
                    self._cast()
                    self._load()

            def get(self, k):
                while self.nc_ <= k:
                    self._cast()
                    self._load()
                j = k % len(self.bf)
                return self.bf[j], self.rbf[j]

            def after(self, k):
                while self.nc_ <= k + self.look and self.nc_ < len(self.srcs):
                    self._cast()
                    self._load()

        def proj_fm(wt, rwt, tiles, consume, plo=0, phi=4):
            for ti, (t0, n) in enumerate(tiles):
                p = nextp(plo, phi)
                S.group("pe", [(lambda kc=kc, p=p: T.matmul(P[p][:, :n], wt[:, kc, :], hT[:, kc, t0:t0 + n],
                                                            start=(kc == 0), stop=(kc == 7))) for kc in range(8)],
                        reads=[rwt, rhT[ti]], writes=[rP[p]])
                consume(p, ti, t0, n)

        def ckpt(name):
            if stop == name:
                S.barrier()
                raise _Stop()

        for l in ([] if stop == "load" else range(nlayers)):
            last = (l == DEPTH - 1)
            TO = TT[:4] if last else TT
            with ExitStack() as eb:
                c2 = sbuf(eb, "c2", [128, 8, 2], F32)
                sc2 = sbuf(eb, "sc2", [128, 8, 2], F32)
                bada = sbuf(eb, "bada", [128, 48], F32)
                wa = [sbuf(eb, f"wa{i}", [128, 8, 512], F32) for i in range(2)]
                rc2, rsc2, rbada = S.res(), S.res(), S.res()
                rwa = S.grid(2)
                dwa = [S.dsem(), S.dsem()]
                S.dma("sp", c2[:].rearrange("p a b -> p (a b)"), c2_d[:, :], S.dsem(), writes=[rc2])
                S.dma("sp", bada[:], bada_d[l, :, :], S.dsem(), writes=[rbada])
                S.op("act", lambda: A.activation(sc2[:], c2[:], AF.Silu), reads=[rc2], writes=[rsc2])
                pm = 7
                for g in range(12):
                    b = g % 2
                    S.dma("sp", wa[b][:], wada_d[l, :, g * 512:(g + 1) * 512].rearrange("(kc p) f -> p kc f", p=128),
                          dwa[b], writes=[rwa[b]])
                    fns = []
                    for j in range(4):
                        fc = g * 4 + j
                        for kc in range(8):
                            fns.append(lambda j=j, fc=fc, kc=kc, b=b: T.matmul(
                                P[pm][:, fc * 2:fc * 2 + 2], wa[b][:, kc, j * 128:(j + 1) * 128], sc2[:, kc, :],
                                start=(kc == 0), stop=(kc == 7)))
                    S.group("pe", fns, reads=[rwa[b], rsc2], writes=[rP[pm]])
                S.op("dve", lambda: V.tensor_tensor(mod[:], P[pm][:, 0:96].rearrange("p (a b) -> p a b", b=2),
                                                    bada[:].unsqueeze(2).to_broadcast([128, 48, 2]), ALU.add),
                     reads=[rP[pm], rbada], writes=[rmod])
                for nidx, (scb, gname) in enumerate(((8, "n1g"), (32, "n2g"))):
                    S.op("dve", lambda nidx=nidx, scb=scb: V.tensor_scalar(
                        se[:, nidx, :, :], mod[:, scb:scb + 8, :], 1.0, float(np.sqrt(D)), ALU.add, ALU.mult),
                        reads=[rmod], writes=[rse])
                    gap = vecs[:, VOFF[gname] + l * 8: VOFF[gname] + l * 8 + 8].unsqueeze(2).to_broadcast([128, 8, 2])
                    S.op("dve", lambda nidx=nidx, gap=gap: V.tensor_tensor(se[:, nidx, :, :], se[:, nidx, :, :], gap, ALU.mult),
                         reads=[rse, rvecs], writes=[rse])
                for di, nm in enumerate(("lbf", "lbb")):
                    l0 = vecs[:, VOFF[nm]:VOFF[nm] + 4]
                    l1 = vecs[:, VOFF[nm] + 4:VOFF[nm] + 8]
                    if l == 0:
                        S.op("dve", lambda di=di: V.memset(lbt[:, di, :, 0], 0.0), writes=[rlbt])
                    else:
                        S.op("dve", lambda di=di, l0=l0, l1=l1: V.tensor_tensor(lbt[:, di, :, 0], l1, l0, ALU.subtract),
                             reads=[rvecs], writes=[rlbt])
                        S.op("act", lambda di=di: A.activation(lbt[:, di, :, 0], lbt[:, di, :, 0], AF.Sigmoid),
                             reads=[rlbt], writes=[rlbt])
                    S.op("dve", lambda di=di: V.tensor_scalar(lbt[:, di, :, 1], lbt[:, di, :, 0], -1.0, 1.0, ALU.mult, ALU.add),
                         reads=[rlbt], writes=[rlbt])
                    S.op("dve", lambda di=di: V.tensor_scalar(lbt[:, di, :, 2], lbt[:, di, :, 0], 1.0, -1.0, ALU.mult, ALU.add),
                         reads=[rlbt], writes=[rlbt])
                S.barrier()
            dump(f"mod{l}", mod[:].rearrange("p a b -> p (a b)"), [rmod])

            with ExitStack() as em:
                rym = S.grid(8, 5)
                with ExitStack() as en:
                    norm_mod(0, TT, es_=en)
                    S.barrier()
                dump(f"h{l}", hT[:].rearrange("p a b -> p (a b)"), rhT)
                if stop == "norm1":
                    break
                srcs = []
                wsl = lambda grp, sub: win_d[l, :, grp * 512 + sub * 128: grp * 512 + (sub + 1) * 128].rearrange(
                    "(kc p) f -> p kc f", p=128)
                wrow = lambda r0: wout_d[l, r0:r0 + 128, :].rearrange("p (dj f) -> p dj f", f=128)
                for h in range(4):
                    srcs += [wsl(3, h), wsl(4, h), wsl(2, h), wsl(5, h), wsl(2, h), wsl(6, h), wrow(512 + h * 128)]
                for j in range(4):
                    srcs += [wsl(0, j), wsl(1, j)]
                W = WChunks(em, "m", srcs, "act", nst=2, nbf=3)
                W.prefetch()
                wk = [0]

                def nextw():
                    k = wk[0]
                    wk[0] += 1
                    t_, r_ = W.get(k)
                    return k, t_, r_

                with ExitStack() as eh:
                    o_acc = sbuf(eh, "o_acc", [128, NT], F32)
                    Vh = sbuf(eh, "Vh", [128, 18, 128], BF16)
                    Vx = [sbuf(eh, f"Vx{i}", [128, 4, 128], BF16) for i in range(2)]
                    yh = sbuf(eh, "yh", [128, NT], BF16)
                    QT = sbuf(eh, "QT", [128, NT], BF16)
                    KT = sbuf(eh, "KT", [128, NT], BF16)
                    KHT = sbuf(eh, "KHT", [128, 18, 128], BF16)
                    dS = sbuf(eh, "dS", [128, NCHK, 128], BF16)
                    Dall = sbuf(eh, "Dall", [128, NCHK], F32)
                    Dpos = sbuf(eh, "Dpos", [128, NCHK], F32)
                    Wt = [[sbuf(eh, f"W{i}_{b_}", [128, 512], F32) for i in range(6)] for b_ in range(2)]
                    rWt = [[S.res() for i in range(6)] for b_ in range(2)]
                    W1 = Wt[0][0]
                    KHt = sbuf(eh, "KHt", [128, 512], BF16)
                    attm = [sbuf(eh, f"attm{i}", [128, 128], BF16) for i in range(2)]
                    ro_acc = S.grid(5)
                    rVh, rKHT, rdS, rDall, rDpos = (S.res() for _ in range(5))
                    rdSh = S.grid(2)
                    rQT, rKT, ryh = S.grid(5), S.grid(5), S.grid(5)
                    rKHt = S.res()
                    rW1 = rWt[0][0]
                    Wq, rWq = Wt[0][4], rWt[0][4]
                    sqh, rsqh, ro, rro = KHt, rKHt, W1, rW1
                    rattm, rVx = S.grid(2), S.grid(2)
                    if l == 0:
                        print("[kernel] SBUF bytes free inside HGRN scope:", nc.sbuf_bytes_remaining)
                    NXC = NX // CH
                    NCC = NCX // CH
                    CPT = 128 // CH
                    for h in range(4):
                        k, wt, rwt = nextw()
                        for g4 in range(5):
                            tts = range(g4 * 4, min(g4 * 4 + 4, 18))
                            p = nextp(0, 4)
                            fns = []
                            for tt in tts:
                                for kc in range(8):
                                    fns.append(lambda tt=tt, kc=kc, p=p: T.matmul(
                                        P[p][:, (tt % 4) * 128:(tt % 4 + 1) * 128], hT[:, kc, tt * 128:(tt + 1) * 128],
                                        wt[:, kc, :], start=(kc == 0), stop=(kc == 7)))
                            S.group("pe", fns, reads=[rwt, rhT[g4]], writes=[rP[p]])
                            nt_ = len(tts)
                            S.op("act", lambda p=p, g4=g4, nt_=nt_: A.copy(
                                Vh[:, g4 * 4:g4 * 4 + nt_, :], P[p][:, :nt_ * 128].rearrange("p (a b) -> p a b", b=128)),
                                reads=[rP[p]], writes=[rVh])
                        W.after(k)
                        ckpt("hg_v")
                        for di in range(2):
                            fwd = di == 0
                            kf, wf, rwf = nextw()
                            kq, wq, rwq = nextw()
                            lb_ap = lbt[:, di, h, 0:1]
                            oml_ap = lbt[:, di, h, 1:2]
                            noml_ap = lbt[:, di, h, 2:3]
                            def prep_a(ti):
                                t0, n = TT[ti]
                                nch, c0 = n // CH, t0 // CH
                                W1, W2, W3, W4, Wq, W5 = Wt[ti % 2]
                                rW1, rW2, rW3, rW4, rWq, rW5 = rWt[ti % 2]
                                v3 = lambda ap: ap[:, :n].rearrange("p (c k) -> p c k", k=CH)
                                pf = nextp(0, 4)
                                S.group("pe", [(lambda kc=kc, pf=pf: T.matmul(P[pf][:, :n], wf[:, kc, :], hT[:, kc, t0:t0 + n],
                                                                             start=(kc == 0), stop=(kc == 7))) for kc in range(8)],
                                        reads=[rwf, rhT[ti]], writes=[rP[pf]])
                                pq = nextp(0, 4)
                                S.group("pe", [(lambda kc=kc, pq=pq: T.matmul(P[pq][:, :n], wq[:, kc, :], hT[:, kc, t0:t0 + n],
                                                                             start=(kc == 0), stop=(kc == 7))) for kc in range(8)],
                                        reads=[rwq, rhT[ti]], writes=[rP[pq]])
                                S.op("act", lambda: A.activation(W1[:, :n], P[pf][:, :n], AF.Sigmoid), reads=[rP[pf]], writes=[rW1])
                                S.op("act", lambda: A.activation(Wq[:, :n], P[pq][:, :n], AF.Sigmoid), reads=[rP[pq]], writes=[rWq])
                                yield
                                S.op("act", lambda: A.activation(W2[:, :n], W1[:, :n], AF.Ln, bias=lb_ap, scale=oml_ap),
                                     reads=[rW1, rlbt], writes=[rW2])
                                S.op("dve", lambda: V.tensor_tensor(Wq[:, :n], P[pq][:, :n], Wq[:, :n], ALU.mult), reads=[rP[pq], rWq], writes=[rWq])
                                S.op("dve", lambda: V.tensor_scalar(W5[:, :n], W1[:, :n], noml_ap, oml_ap, ALU.mult, ALU.add),
                                     reads=[rW1, rlbt], writes=[rW5])
                                yield
                                S.op("dve", lambda: V.tensor_tensor_scan(W3[:, :n], smask[:, :n], W2[:, :n], 0.0, ALU.mult, ALU.add),
                                     reads=[rW2, rcst], writes=[rW3])
                                if fwd:
                                    bb, rbb = W3, rW3
                                    bl = v3(W3)[:, :, CH - 1]
                                else:
                                    tc_ = v3(W3)[:, :, CH - 1:CH].to_broadcast([128, nch, CH])
                                    S.op("dve", lambda: V.tensor_tensor(v3(W4), tc_, v3(W3), ALU.subtract), reads=[rW3], writes=[rW4])
                                    S.op("dve", lambda: V.tensor_tensor(W4[:, :n], W4[:, :n], W2[:, :n], ALU.add),
                                         reads=[rW4, rW2], writes=[rW4])
                                    bb, rbb = W4, rW4
                                    bl = v3(W4)[:, :, 0]
                                yield
                                S.op("act", lambda: A.activation(Dall[:, c0:c0 + nch], bl, AF.Exp), reads=[rbb], writes=[rDall])
                                yield
                                res_[ti] = (bb, rbb)

                            def prep_b(ti):
                                bb, rbb = res_[ti]
                                t0, n = TT[ti]
                                nch, c0 = n // CH, t0 // CH
                                W1, W2, W3, W4, Wq, W5 = Wt[ti % 2]
                                rW1, rW2, rW3, rW4, rWq, rW5 = rWt[ti % 2]
                                v3 = lambda ap: ap[:, :n].rearrange("p (c k) -> p c k", k=CH)
                                S.op("act", lambda: A.activation(W2[:, :n], bb[:, :n], AF.Exp), reads=[rbb, rW2], writes=[rW2])
                                S.op("act", lambda: A.activation(W1[:, :n], bb[:, :n], AF.Exp, scale=-1.0), reads=[rbb, rW1], writes=[rW1])
                                yield
                                S.op("dve", lambda: V.tensor_tensor(QT[:, t0:t0 + n], Wq[:, :n], W2[:, :n], ALU.mult),
                                     reads=[rWq, rW2], writes=[rQT[ti]])
                                S.op("dve", lambda: V.tensor_tensor(W5[:, :n], W5[:, :n], W1[:, :n], ALU.mult),
                                     reads=[rW5, rW1], writes=[rW5])
                                yield
                                S.op("act", lambda: A.copy(KT[:, t0:t0 + n], W5[:, :n]), reads=[rW5], writes=[rKT[ti]])
                                dbc = Dall[:, c0:c0 + nch].unsqueeze(2).to_broadcast([128, nch, CH])
                                S.op("dve", lambda: V.tensor_tensor(v3(KHt), v3(W5), dbc, ALU.mult), reads=[rW5, rDall], writes=[rKHt])
                                yield
                                pk = nextp(4, 6)
                                ntile = n // 128
                                pkb = P[pk][:].bitcast(BF16)
                                S.group("pe", [(lambda j=j: T.transpose(pkb[:, j * 128:(j + 1) * 128],
                                                                        KHt[:, j * 128:(j + 1) * 128], identb[:]))
                                               for j in range(ntile)], reads=[rKHt, rmisc], writes=[rP[pk]])
                                yield
                                S.op("act", lambda: A.copy(KHT[:, t0 // 128:t0 // 128 + ntile, :],
                                                           pkb[:, :ntile * 128].rearrange("p (a b) -> p a b", b=128)),
                                     reads=[rP[pk]], writes=[rKHT])
                                yield
                            res_ = {}

                            def run_zip(g1, g2):
                                gens = [g for g in (g1, g2) if g is not None]
                                while gens:
                                    for g in list(gens):
                                        try:
                                            next(g)
                                        except StopIteration:
                                            gens.remove(g)
                            run_zip(prep_a(0), None)
                            for ti in range(len(TT)):
                                run_zip(prep_a(ti + 1) if ti + 1 < len(TT) else None, prep_b(ti))
                            W.after(kq)
                            ckpt("hg_prep")
                            order = (list(range(NXC, NCHK)) + list(range(NXC))) if fwd else list(range(NCHK - 1, -1, -1))
                            pos = {c: i for i, c in enumerate(order)}
                            for tt in range(18):
                                vb = tt % 2
                                S.op("dve", lambda vb=vb, tt=tt: V.tensor_tensor(
                                    Vx[vb][:], Vh[:, tt, :].unsqueeze(1).to_broadcast([128, CPT, 128]),
                                    cst[:, 896:896 + CPT].unsqueeze(2).to_broadcast([128, CPT, 128]), ALU.mult),
                                    reads=[rVh, rcst], writes=[rVx[vb]])
                                p = nextp(6, 8)
                                S.group("pe", [lambda tt=tt, vb=vb, p=p: T.matmul(
                                    P[p][:, :CPT * 128], KHT[:, tt, :], Vx[vb][:].rearrange("p a b -> p (a b)"), start=True, stop=True)],
                                    reads=[rKHT, rVx[vb]], writes=[rP[p]])
                                p0 = pos[tt * CPT]
                                if fwd:
                                    dst = dS[:, p0:p0 + CPT, :]
                                else:
                                    dst = dS[:, p0:p0 - CPT:-1, :] if p0 - CPT >= 0 else dS[:, p0::-1, :]
                                S.op("act", lambda dst=dst, p=p: A.copy(dst, P[p][:, :CPT * 128].rearrange("p (c v) -> p c v", v=128)),
                                     reads=[rP[p]], writes=[rdS, rdSh[0], rdSh[1]])
                            ckpt("hg_ds")
                            if fwd:
                                S.op("dve", lambda: V.tensor_copy(Dpos[:, NCC:NCHK], Dall[:, 0:NXC]), reads=[rDall], writes=[rDpos])
                                S.op("dve", lambda: V.tensor_copy(Dpos[:, 1:NCC], Dall[:, NXC + 1:NCHK]), reads=[rDall], writes=[rDpos])
                            else:
                                S.op("dve", lambda: V.tensor_copy(Dpos[:, 1:NCHK], Dall[:, NCHK - 2::-1]), reads=[rDall], writes=[rDpos])
                            S.op("dve", lambda: V.memset(Dpos[:, 0:1], 0.0), reads=[rDpos], writes=[rDpos])
                            for pp in range(1, NCHK):
                                for hf in range(2):
                                    vs = slice(hf * 64, hf * 64 + 64)
                                    S.op("dve", lambda pp=pp, vs=vs: V.scalar_tensor_tensor(
                                        dS[:, pp, vs], dS[:, pp - 1, vs], Dpos[:, pp:pp + 1], dS[:, pp, vs], ALU.mult, ALU.add),
                                        reads=[rdSh[hf], rDpos], writes=[rdSh[hf]])
                            ckpt("hg_scan")
                            mask = maskF if fwd else maskB
                            for g4 in range(5):
                                tts = list(range(g4 * 4, min(g4 * 4 + 4, 18)))
                                po = nextp(4, 6)
                                for tt in tts:
                                    off = (tt % 4) * 128
                                    fns = []
                                    for c in range(tt * CPT, (tt + 1) * CPT):
                                        pc = pos[c]
                                        lhs = zerob[:] if pc == 0 else dS[:, pc - 1, :]
                                        co = off + (c % CPT) * CH
                                        fns.append(lambda c=c, lhs=lhs, co=co, po=po: T.matmul(
                                            P[po][:, co:co + CH], lhs, QT[:, c * CH:(c + 1) * CH], start=(c % CPT == 0), stop=False,
                                            skip_group_check=True))
                                    S.group("pe", fns, reads=[rdS, rdSh[0], rdSh[1], rQT[g4], rmisc], writes=[rP[po]])
                                    pa = nextp(6, 8)
                                    S.group("pe", [lambda tt=tt, pa=pa: T.matmul(P[pa][:, 0:128], KT[:, tt * 128:(tt + 1) * 128],
                                                                                QT[:, tt * 128:(tt + 1) * 128], start=True, stop=True)],
                                            reads=[rKT[g4], rQT[g4]], writes=[rP[pa]])
                                    ab = tt % 2
                                    S.op("dve", lambda pa=pa, ab=ab, mask=mask: V.tensor_tensor(attm[ab][:], P[pa][:, 0:128], mask, ALU.mult),
                                         reads=[rP[pa], rcst], writes=[rattm[ab]])
                                    S.group("pe", [lambda tt=tt, ab=ab, off=off, po=po: T.matmul(
                                        P[po][:, off:off + 128], Vh[:, tt, :], attm[ab][:], start=False, stop=True, skip_group_check=True)],
                                        reads=[rVh, rattm[ab]], writes=[rP[po]])
                                nn = len(tts) * 128
                                t0 = g4 * 512
                                if fwd:
                                    S.op("act", lambda po=po, nn=nn, t0=t0: A.copy(o_acc[:, t0:t0 + nn], P[po][:, :nn]),
                                         reads=[rP[po]], writes=[ro_acc[g4]])
                                else:
                                    S.op("dve", lambda po=po, nn=nn, t0=t0: V.tensor_tensor(o_acc[:, t0:t0 + nn], o_acc[:, t0:t0 + nn],
                                                                                          P[po][:, :nn], ALU.add),
                                         reads=[rP[po], ro_acc[g4]], writes=[ro_acc[g4]])
                            if stop == "hg_o" + str(di):
                                dump("QT", QT[:], rQT); dump("KT", KT[:], rKT); dump("dS", dS[:].rearrange("p c v -> p (c v)"), [rdS])
                                dump("Dall", Dall[:], [rDall]); dump("Dpos", Dpos[:], [rDpos]); dump("oacc0_0", o_acc[:], ro_acc)
                            ckpt("hg_o" + str(di))
                        if f"oacc{l}_{h}" in dbg_d:
                            dump(f"oacc{l}_{h}", o_acc[:], ro_acc)
                        W1, rW1, Wq, rWq = Wt[0][0], rWt[0][0], Wt[0][4], rWt[0][4]
                        ro, rro = W1, rW1
                        ko, wo_, rwo = nextw()
                        kr, wr_, rwr = nextw()
                        gh = vv("hg", l)
                        for ti, (t0, n) in enumerate(TO):
                            S.op("act", lambda: A.activation(sqh[:, :n], o_acc[:, t0:t0 + n], AF.Square), reads=[ro_acc[ti]], writes=[rsqh])
                            p = nextp(0, 4)
                            S.group("pe", [lambda p=p: T.matmul(P[p][:, :n], onesb[:], sqh[:, :n], start=True, stop=True)],
                                    reads=[rsqh, rmisc], writes=[rP[p]])
                            S.op("act", lambda p=p: A.activation(ro[:, :n], P[p][:, :n], AF.Ln, bias=epsD[:, 1:2], scale=1.0 / 128),
                                 reads=[rP[p], rmisc], writes=[rro])
                            S.op("act", lambda: A.activation(ro[:, :n], ro[:, :n], AF.Exp, scale=-0.5), reads=[rro], writes=[rro])
                            S.op("dve", lambda: V.tensor_tensor(ro[:, :n], ro[:, :n], o_acc[:, t0:t0 + n], ALU.mult),
                                 reads=[rro, ro_acc[ti]], writes=[rro])
                            p2 = nextp(0, 4)
                            S.group("pe", [(lambda kc=kc, p2=p2: T.matmul(P[p2][:, :n], wo_[:, kc, :], hT[:, kc, t0:t0 + n],
                                                                         start=(kc == 0), stop=(kc == 7))) for kc in range(8)],
                                    reads=[rwo, rhT[ti]], writes=[rP[p2]])
                            S.op("act", lambda p2=p2: A.activation(Wq[:, :n], P[p2][:, :n], AF.Silu), reads=[rP[p2]], writes=[rWq])
                            S.op("dve", lambda: V.scalar_tensor_tensor(yh[:, t0:t0 + n], ro[:, :n], gh, Wq[:, :n], ALU.mult, ALU.mult),
                                 reads=[rro, rWq, rvecs], writes=[ryh[ti]])
                            col = 0 if t0 < NX else 1
                            for dj in range(8):
                                p3 = nextp(4, 8)
                                S.group("pe", [lambda p3=p3, dj=dj: T.matmul(P[p3][:, :n], wr_[:, dj, :], yh[:, t0:t0 + n], start=True, stop=True)],
                                        reads=[rwr, ryh[ti]], writes=[rP[p3]])
                                g1 = mod[:, 16 + dj, col:col + 1]
                                S.op("dve", lambda p3=p3, g1=g1, dj=dj: V.scalar_tensor_tensor(
                                    xT[:, dj, t0:t0 + n], P[p3][:, :n], g1, xT[:, dj, t0:t0 + n], ALU.mult, ALU.add),
                                    reads=[rP[p3], rmod, rxT[dj][ti]], writes=[rxT[dj][ti]])
                        if f"yh{l}_{h}" in dbg_d:
                            dump(f"yh{l}_{h}", yh[:], ryh)
                        W.after(kr)
                    S.barrier()
                if stop == "hgrn":
                    break
                ecm = ExitStack()
                em.enter_context(ecm)
                ymc = sbuf(ecm, "ymc", [128, 4, NT], BF16)
                with ExitStack() as ec:
                    rowo = (l % 2 == 0)
                    conv_tiles = TT if not last else TT[:4]
                    UPL = (32 * 79 + 31) if rowo else (62 * 64)
                    Upad = [sbuf(ec, f"Upad{i}", [128, UPL + 286], BF16) for i in range(2)]
                    Dg = [sbuf(ec, f"Dg{i}", [128, 31, 128], BF16) for i in range(2)]
                    sgt = [sbuf(ec, f"sgt{i}", [128, 512], F32) for i in range(2)]
                    cwb = sbuf(ec, "cwb", [128, 4 * 31], BF16)
                    rUp, rDg, rsgt = S.grid(2), S.grid(2), S.grid(2)
                    rcwb = S.res()
                    for i in range(2):
                        S.op("pool", lambda i=i: G.memset(Upad[i][:], 0.0), writes=[rUp[i]])
                    S.op("dve", lambda: V.tensor_copy(cwb[:], vecs[:, VOFF["convw"] + l * 124: VOFF["convw"] + (l + 1) * 124]),
                         reads=[rvecs], writes=[rcwb])

                    def uview(b, ti, k):
                        t0, n = TT[ti]
                        if t0 >= NX:
                            return Upad[b][:, UPL + k: UPL + k + NCX]
                        r0 = t0 // 64
                        if rowo:
                            return Upad[b][:, r0 * 79 + k: r0 * 79 + k + 8 * 79].rearrange("p (r c) -> p r c", c=79)[:, :, 0:64]
                        return Upad[b][:, (r0 + k) * 64: (r0 + k) * 64 + 512]

                    def pview(p, ti):
                        t0, n = TT[ti]
                        if t0 < NX and rowo:
                            return P[p][:, :n].rearrange("p (r c) -> p r c", c=64)
                        return P[p][:, :n]

                    def emit_glu(j):
                        b = j % 2
                        ka, wa_, rwa_ = nextw()
                        kg, wg_, rwg_ = nextw()
                        for ti in range(len(conv_tiles)):
                            t0, n = TT[ti]
                            pa = nextp(0, 4)
                            S.group("pe", [(lambda kc=kc, pa=pa: T.matmul(P[pa][:, :n], wa_[:, kc, :], hT[:, kc, t0:t0 + n],
                                                                         start=(kc == 0), stop=(kc == 7))) for kc in range(8)],
                                    reads=[rwa_, rhT[ti]], writes=[rP[pa]])
                            pg = nextp(0, 4)
                            S.group("pe", [(lambda kc=kc, pg=pg: T.matmul(P[pg][:, :n], wg_[:, kc, :], hT[:, kc, t0:t0 + n],
                                                                         start=(kc == 0), stop=(kc == 7))) for kc in range(8)],
                                    reads=[rwg_, rhT[ti]], writes=[rP[pg]])
                            sb_ = ti % 2
                            S.op("act", lambda pg=pg, sb_=sb_: A.activation(sgt[sb_][:, :n], P[pg][:, :n], AF.Sigmoid),
                                 reads=[rP[pg]], writes=[rsgt[sb_]])
                            sv = sgt[sb_][:, :n]
                            if t0 < NX and rowo:
                                sv = sv.rearrange("p (r c) -> p r c", c=64)
                            S.op("dve", lambda pa=pa, sv=sv, ti=ti, b=b: V.tensor_tensor(uview(b, ti, 15), pview(pa, ti), sv, ALU.mult),
                                 reads=[rP[pa], rsgt[sb_]], writes=[rUp[b]])
                        W.after(kg)
                        S.op("dve", lambda b=b, j=j: V.tensor_tensor(
                            Dg[b][:], identb[:].unsqueeze(1).to_broadcast([128, 31, 128]),
                            cwb[:, j * 31:(j + 1) * 31].unsqueeze(2).to_broadcast([128, 31, 128]), ALU.mult),
                            reads=[rmisc, rcwb], writes=[rDg[b]])

                    def emit_conv(j):
                        b = j % 2
                        cb = vv("convb", l * 4 + j)
                        for ti in range(len(conv_tiles)):
                            t0, n = TT[ti]
                            p = nextp(4, 8)
                            S.group("pe", [(lambda k_=k_, p=p, ti=ti, b=b: T.matmul(pview(p, ti), Dg[b][:, k_, :], uview(b, ti, k_),
                                                                                 start=(k_ == 0), stop=(k_ == 30))) for k_ in range(31)],
                                    reads=[rDg[b], rUp[b]], writes=[rP[p]])
                            S.op("act", lambda p=p, j=j, cb=cb: A.activation(ymc[:, j, t0:t0 + n], P[p][:, :n], AF.Identity, bias=cb, scale=1.0),
                                 reads=[rP[p], rvecs], writes=[rym[j][ti]])
                    emit_glu(0)
                    for j in range(4):
                        if j + 1 < 4:
                            emit_glu(j + 1)
                        emit_conv(j)
                    S.barrier()
                with ExitStack() as eln:
                    sq4 = sbuf(eln, "sq4", [128, 4, 512], BF16)
                    mu = sbuf(eln, "mu", [128, 512], F32)
                    msq = sbuf(eln, "msq", [128, 512], F32)
                    rsd = sbuf(eln, "rsd", [128, 512], F32)
                    lt = [sbuf(eln, f"lt{i}", [128, 512], F32) for i in range(2)]
                    rsq4, rmu, rmsq, rrsd = (S.res() for _ in range(4))
                    rlt = S.grid(2)
                    for ti, (t0, n) in enumerate(TO):
                        for j in range(4):
                            S.op("act", lambda j=j: A.activation(sq4[:, j, :n], ymc[:, j, t0:t0 + n], AF.Square),
                                 reads=[rym[j][ti]], writes=[rsq4])
                        p1 = nextp(0, 4)
                        S.group("pe", [(lambda j=j, p1=p1: T.matmul(P[p1][:, :n], onesb[:], ymc[:, j, t0:t0 + n], start=(j == 0), stop=(j == 3)))
                                       for j in range(4)], reads=[rym[j][ti] for j in range(4)] + [rmisc], writes=[rP[p1]])
                        p2 = nextp(0, 4)
                        S.group("pe", [(lambda j=j, p2=p2: T.matmul(P[p2][:, :n], onesb[:], sq4[:, j, :n], start=(j == 0), stop=(j == 3)))
                                       for j in range(4)], reads=[rsq4, rmisc], writes=[rP[p2]])
                        S.op("act", lambda p1=p1: A.mul(mu[:, :n], P[p1][:, :n], 1.0 / 512), reads=[rP[p1]], writes=[rmu])
                        S.op("dve", lambda: V.tensor_tensor(msq[:, :n], mu[:, :n], mu[:, :n], ALU.mult), reads=[rmu], writes=[rmsq])
                        S.op("dve", lambda p2=p2: V.scalar_tensor_tensor(rsd[:, :n], P[p2][:, :n], 1.0 / 512, msq[:, :n], ALU.mult, ALU.subtract),
                             reads=[rP[p2], rmsq], writes=[rrsd])
                        S.op("act", lambda: A.activation(rsd[:, :n], rsd[:, :n], AF.Ln, bias=epsD[:, 1:2], scale=1.0),
                             reads=[rrsd, rmisc], writes=[rrsd])
                        S.op("act", lambda: A.activation(rsd[:, :n], rsd[:, :n], AF.Exp, scale=-0.5), reads=[rrsd], writes=[rrsd])
                        for j in range(4):
                            b = j % 2
                            S.op("dve", lambda j=j, b=b: V.tensor_tensor(lt[b][:, :n], ymc[:, j, t0:t0 + n], mu[:, :n], ALU.subtract),
                                 reads=[rym[j][ti], rmu], writes=[rlt[b]])
                            S.op("dve", lambda b=b: V.tensor_tensor(lt[b][:, :n], lt[b][:, :n], rsd[:, :n], ALU.mult),
                                 reads=[rlt[b], rrsd], writes=[rlt[b]])
                            S.op("act", lambda j=j, b=b: A.activation(ymc[:, j, t0:t0 + n], lt[b][:, :n], AF.Silu,
                                                                      bias=vv("lnb", l * 4 + j), scale=vv("lng", l * 4 + j)),
                                 reads=[rlt[b], rvecs], writes=[rym[j][ti]])
                    S.barrier()
                dump(f"ymc{l}", ymc[:].rearrange("p a b -> p (a b)"), [r for rr in rym[:4] for r in rr])
                if stop == "conv":
                    break
                W2 = WChunks(ecm, "o", [wrow(j * 128) for j in range(4)], "act", nst=2, nbf=4)
                W2.prefetch()
                wo4 = [W2.get(j) for j in range(4)]
                for dj in range(8):
                    for ti, (t0, n) in enumerate(TO):
                        col = 0 if t0 < NX else 1
                        p = nextp(0, 4)
                        S.group("pe", [(lambda j=j, p=p, dj=dj: T.matmul(P[p][:, :n], wo4[j][0][:, dj, :], ymc[:, j, t0:t0 + n],
                                                                        start=(j == 0), stop=(j == 3))) for j in range(4)],
                                reads=[w_[1] for w_ in wo4] + [rym[j][ti] for j in range(4)], writes=[rP[p]])
                        g1 = mod[:, 16 + dj, col:col + 1]
                        S.op("dve", lambda p=p, g1=g1, dj=dj: V.scalar_tensor_tensor(
                            xT[:, dj, t0:t0 + n], P[p][:, :n], g1, xT[:, dj, t0:t0 + n], ALU.mult, ALU.add),
                            reads=[rP[p], rmod, rxT[dj][ti]], writes=[rxT[dj][ti]])
                S.barrier()
            dump(f"xmix{l}", xT[:].rearrange("p a b -> p (a b)"), [r for rr in rxT for r in rr])
            if stop == "mix":
                break

            with ExitStack() as eo:
                comb = sbuf(eo, "comb", [128, 18, NE], F32)
                rcomb = S.res()
                with ExitStack() as er:
                    h2f = sbuf(er, "h2f", [128, 8, 512], F32)
                    sq = sbuf(er, "sq2", [128, 8, 512], BF16)
                    rstd = sbuf(er, "rstd2", [128, 512], F32)
                    tmp = [sbuf(er, f"nt2_{i}", [128, 512], F32) for i in range(2)]
                    rsq, rrstd = S.res(), S.res()
                    rtmp = S.grid(2)
                    rh2 = S.res()
                    pl = 7
                    for ti, (t0, n) in enumerate(TO):
                        if True:
                            col = 0 if t0 < NX else 1
                            for c in range(8):
                                S.op("act", lambda c=c: A.activation(sq[:, c, :n], xT[:, c, t0:t0 + n], AF.Square),
                                     reads=[rxT[c][ti]], writes=[rsq])
                            p = nextp(0, 4)
                            S.group("pe", [(lambda c=c, p=p: T.matmul(P[p][:, :n], onesb[:], sq[:, c, :n], start=(c == 0), stop=(c == 7)))
                                           for c in range(8)], reads=[rsq, rmisc], writes=[rP[p]])
                            S.op("act", lambda p=p: A.activation(rstd[:, :n], P[p][:, :n], AF.Ln, bias=epsD[:, 0:1], scale=1.0),
                                 reads=[rP[p], rmisc], writes=[rrstd])
                            S.op("act", lambda: A.activation(rstd[:, :n], rstd[:, :n], AF.Exp, scale=-0.5), reads=[rrstd], writes=[rrstd])
                            for c in range(8):
                                b = c % 2
                                S.op("dve", lambda c=c, b=b: V.tensor_tensor(tmp[b][:, :n], xT[:, c, t0:t0 + n], rstd[:, :n], ALU.mult),
                                     reads=[rxT[c][ti], rrstd], writes=[rtmp[b]])
                                sc_ap = se[:, 1, c, col:col + 1]
                                sh_ap = mod[:, 24 + c, col:col + 1]
                                S.op("act", lambda c=c, b=b, sc_ap=sc_ap, sh_ap=sh_ap: A.activation(
                                    h2f[:, c, :n], tmp[b][:, :n], AF.Identity, bias=sh_ap, scale=sc_ap),
                                    reads=[rtmp[b], rse, rmod], writes=[rh2])
                                if False:
                                    S.op("pool", lambda c=c: G.tensor_copy(hT[:, c, t0:t0 + n], h2f[:, c, :n]),
                                         reads=[rh2], writes=[rhT[ti]])
                                else:
                                    S.op("act", lambda c=c, b=b, sc_ap=sc_ap, sh_ap=sh_ap: A.activation(
                                        hT[:, c, t0:t0 + n], tmp[b][:, :n], AF.Identity, bias=sh_ap, scale=sc_ap),
                                        reads=[rtmp[b], rse, rmod], writes=[rhT[ti]])
                            fns = []
                            for s_ in range(n // 128):
                                tt = t0 // 128 + s_
                                for kc in range(8):
                                    fns.append(lambda s_=s_, tt=tt, kc=kc: T.matmul(
                                        P[pl][:, tt * NE:(tt + 1) * NE], h2f[:, kc, s_ * 128:(s_ + 1) * 128], rw[:, kc, :],
                                        start=(kc == 0), stop=(kc == 7)))
                            S.group("pe", fns, reads=[rh2, rrw], writes=[rP[pl]])
                    ntl = sum(n for _, n in TO) // 128
                    NG = ntl * 4
                    sg_ = sbuf(er, "r_s", [128, ntl, NE], F32)
                    sbb = sbuf(er, "r_sb", [128, ntl, NE], F32)
                    ps6 = sbuf(er, "r_p6", [128, NG, 6], F32)
                    gs = sbuf(er, "r_gs", [128, NG], F32)
                    gm = sbuf(er, "r_gm", [128, ntl], F32)
                    ing = sbuf(er, "r_ing", [128, NG], F32)
                    sbm = sbuf(er, "r_sbm", [128, ntl, NE], F32)
                    sel = sbuf(er, "r_sel", [128, ntl, NE], F32)
                    m1 = sbuf(er, "r_m1", [128, ntl], F32)
                    rr_ = S.res()
                    R = dict(reads=[rr_], writes=[rr_])
                    S.op("act", lambda: A.activation(sg_[:].rearrange("p a b -> p (a b)"), P[pl][:, :ntl * NE], AF.Sigmoid),
                         reads=[rP[pl]], writes=[rr_])
                    rb_bc = vecs[:, VOFF["rbias"]:VOFF["rbias"] + NE].unsqueeze(1).to_broadcast([128, ntl, NE])
                    S.op("dve", lambda: V.tensor_tensor(sbb[:], sg_[:], rb_bc, ALU.add), reads=[rr_, rvecs], writes=[rr_])
                    g4v = sbb[:].rearrange("p a (g e) -> p (a g) e", e=4)
                    S.op("dve", lambda: V.tensor_tensor(ps6[:, :, 0:2], g4v[:, :, 0:4:2], g4v[:, :, 1:4:2], ALU.add), **R)
                    S.op("dve", lambda: V.tensor_tensor(ps6[:, :, 2:4], g4v[:, :, 0:2], g4v[:, :, 2:4], ALU.add), **R)
                    S.op("dve", lambda: V.tensor_tensor(ps6[:, :, 4:5], g4v[:, :, 0:1], g4v[:, :, 3:4], ALU.add), **R)
                    S.op("dve", lambda: V.tensor_tensor(ps6[:, :, 5:6], g4v[:, :, 1:2], g4v[:, :, 2:3], ALU.add), **R)
                    S.op("dve", lambda: V.tensor_reduce(gs[:], ps6[:], AX.X, ALU.max), **R)
                    S.op("dve", lambda: V.tensor_reduce(gm[:], gs[:].rearrange("p (a g) -> p a g", g=4), AX.X, ALU.max), **R)
                    S.op("dve", lambda: V.tensor_tensor(ing[:].rearrange("p (a g) -> p a g", g=4), gs[:].rearrange("p (a g) -> p a g", g=4),
                                                        gm[:].unsqueeze(2).to_broadcast([128, ntl, 4]), ALU.is_equal), **R)
                    S.op("dve", lambda: V.tensor_scalar(ing[:], ing[:], BIG, -BIG, ALU.mult, ALU.add), **R)
                    S.op("dve", lambda: V.tensor_tensor(sbm[:].rearrange("p a (g e) -> p (a g) e", e=4), g4v,
                                                        ing[:].unsqueeze(2).to_broadcast([128, NG, 4]), ALU.add), **R)
                    S.op("dve", lambda: V.tensor_reduce(m1[:], sbm[:], AX.X, ALU.max), **R)
                    S.op("dve", lambda: V.tensor_tensor(sel[:], sbm[:], m1[:].unsqueeze(2).to_broadcast([128, ntl, NE]), ALU.is_equal), **R)
                    S.op("dve", lambda: V.scalar_tensor_tensor(sbm[:], sel[:], -BIG, sbm[:], ALU.mult, ALU.add), **R)
                    S.op("dve", lambda: V.tensor_reduce(m1[:], sbm[:], AX.X, ALU.max), **R)
                    S.op("dve", lambda: V.tensor_tensor(sbm[:], sbm[:], m1[:].unsqueeze(2).to_broadcast([128, ntl, NE]), ALU.is_ge), **R)
                    S.op("dve", lambda: V.tensor_tensor(sel[:], sel[:], sbm[:], ALU.add), **R)
                    S.op("dve", lambda: V.tensor_tensor(sel[:], sel[:], sg_[:], ALU.mult), **R)
                    S.op("dve", lambda: V.tensor_reduce(m1[:], sel[:], AX.X, ALU.add), **R)
                    S.op("dve", lambda: V.reciprocal(m1[:], m1[:]), **R)
                    S.op("dve", lambda: V.tensor_tensor(comb[:, :ntl, :], sel[:], m1[:].unsqueeze(2).to_broadcast([128, ntl, NE]), ALU.mult),
                         reads=[rr_], writes=[rcomb])
                    S.barrier()
                comb_hi = sbuf(eo, "comb_hi", [128, 18, NE], BF16)
                comb_lo = sbuf(eo, "comb_lo", [128, 18, NE], BF16)
                comb_r = sbuf(eo, "comb_r", [128, 18, NE], F32)
                S.op("dve", lambda: V.tensor_copy(comb_hi[:], comb[:]), reads=[rcomb], writes=[rcomb])
                S.op("dve", lambda: V.tensor_tensor(comb_r[:], comb[:], comb_hi[:], ALU.subtract), reads=[rcomb], writes=[rcomb])
                S.op("dve", lambda: V.tensor_copy(comb_lo[:], comb_r[:]), reads=[rcomb], writes=[rcomb])
                dump(f"comb{l}", comb[:].rearrange("p a b -> p (a b)"), [rcomb])
                dump(f"h2_{l}", hT[:].rearrange("p a b -> p (a b)"), rhT)
                if stop == "router":
                    break
                NU = NE * 4
                NST, NBF = 4, 2
                stg = [sbuf(eo, f"stg{i}", [128, 2048], F32) for i in range(NST)]
                wbf = [sbuf(eo, f"wbf{i}", [128, 3, 2048], BF16) for i in range(NBF)]
                cg = sbuf(eo, "cg", [128, NT], F32)
                actb = [sbuf(eo, f"actb{i}", [128, 2, 512], BF16) for i in range(2)]
                sgm = [sbuf(eo, f"sgm{i}", [128, 512], F32) for i in range(2)]
                t1m = [sbuf(eo, f"t1m{i}", [128, 512], F32) for i in range(2)]
                rstg = S.grid(NST)
                rwbf = S.grid(NBF, 3)
                rcg = S.res()
                ractb, rsgm, rt1m = S.grid(2), S.grid(2), S.grid(2)
                dstg = [S.dsem() for _ in range(NST)]
                pieces = []
                for e in range(NE):
                    for q in range(4):
                        f0 = q * 256
                        pieces.append((wg_d[l, e, :, f0:f0 + 256].rearrange("(kc p) f -> p kc f", p=128), 0))
                        pieces.append((wu_d[l, e, :, f0:f0 + 256].rearrange("(kc p) f -> p kc f", p=128), 1))
                        pieces.append((wd_d[l, e, f0:f0 + 256, :].rearrange("(fc p) d -> p fc d", p=128), 2))
                pl_, pc_ = [0], [0]

                def p_load():
                    if pl_[0] >= len(pieces):
                        return
                    i = pl_[0] % NST
                    src, kind = pieces[pl_[0]]
                    dst = stg[i][:].rearrange("p (a b) -> p a b", a=(8 if kind < 2 else 2))
                    S.dma("sp", dst, src, dstg[i], writes=[rstg[i]])
                    pl_[0] += 1

                def p_cast():
                    if pc_[0] >= len(pieces):
                        return
                    k = pc_[0]
                    i = k % NST
                    u, kind = k // 3, k % 3
                    j = u % NBF
                    S.op("act", lambda i=i, j=j, kind=kind: A.copy(wbf[j][:, kind, :], stg[i][:]),
                         reads=[rstg[i]], writes=[rwbf[j][kind]])
                    pc_[0] += 1
                    p_load()
                for _ in range(NST):
                    p_load()
                for _ in range(3):
                    p_cast()
                def emit_cg(e):
                    for g4 in range((ntl + 3) // 4):
                        tts = list(range(g4 * 4, min(g4 * 4 + 4, ntl)))
                        p = 7
                        fns = []
                        for tt in tts:
                            for hl, cb_ in enumerate((comb_hi, comb_lo)):
                                fns.append(lambda tt=tt, hl=hl, cb_=cb_: T.matmul(
                                    P[p][:, (tt % 4) * 128:(tt % 4 + 1) * 128],
                                    cb_[:, tt, e:e + 1].to_broadcast([128, 128]), identb[:], start=(hl == 0), stop=(hl == 1)))
                        S.group("pe", fns, reads=[rcomb, rmisc], writes=[rP[p]])
                        nn = len(tts) * 128
                        S.op("dve", lambda p=p, g4=g4, nn=nn: V.tensor_copy(cg[:, g4 * 512:g4 * 512 + nn], P[p][:, :nn]),
                             reads=[rP[p]], writes=[rcg])

                def emit_gu(u, ti, ab):
                    e, q = u // 4, u % 4
                    j = u % NBF
                    t0, n = TO[ti]
                    wgt = wbf[j][:, 0, :].rearrange("p (kc f) -> p kc f", kc=8)
                    wut = wbf[j][:, 1, :].rearrange("p (kc f) -> p kc f", kc=8)
                    if q == 0 and ti == 0:
                        emit_cg(e)
                    for fc in range(2):
                        pg = nextp(0, 4)
                        S.group("pe", [(lambda kc=kc, pg=pg, fc=fc: T.matmul(P[pg][:, :n], wgt[:, kc, fc * 128:(fc + 1) * 128],
                                                                           hT[:, kc, t0:t0 + n], start=(kc == 0), stop=(kc == 7)))
                                       for kc in range(8)], reads=[rwbf[j][0], rhT[ti]], writes=[rP[pg]])
                        pu = nextp(0, 4)
                        S.group("pe", [(lambda kc=kc, pu=pu, fc=fc: T.matmul(P[pu][:, :n], wut[:, kc, fc * 128:(fc + 1) * 128],
                                                                           hT[:, kc, t0:t0 + n], start=(kc == 0), stop=(kc == 7)))
                                       for kc in range(8)], reads=[rwbf[j][1], rhT[ti]], writes=[rP[pu]])
                        S.op("act", lambda pg=pg, fc=fc: A.activation(sgm[fc][:, :n], P[pg][:, :n], AF.Silu),
                             reads=[rP[pg]], writes=[rsgm[fc]])
                        S.op("dve", lambda pu=pu, fc=fc: V.tensor_tensor(t1m[fc][:, :n], P[pu][:, :n], sgm[fc][:, :n], ALU.mult),
                             reads=[rP[pu], rsgm[fc]], writes=[rt1m[fc]])
                        S.op("pool", lambda fc=fc, ab=ab: G.tensor_tensor(actb[ab][:, fc, :n], t1m[fc][:, :n], cg[:, t0:t0 + n], ALU.mult),
                             reads=[rt1m[fc], rcg], writes=[ractb[ab]])

                def emit_dn(u, ti, ab):
                    j = u % NBF
                    t0, n = TO[ti]
                    col = 0 if t0 < NX else 1
                    wdt = wbf[j][:, 2, :].rearrange("p (fc d) -> p fc d", fc=2)
                    for dj in range(8):
                        po = nextp(4, 7)
                        S.group("pe", [(lambda fc=fc, po=po, dj=dj, ab=ab: T.matmul(
                            P[po][:, :n], wdt[:, fc, dj * 128:(dj + 1) * 128], actb[ab][:, fc, :n], start=(fc == 0), stop=(fc == 1)))
                            for fc in range(2)], reads=[rwbf[j][2], ractb[ab]], writes=[rP[po]])
                        g2 = mod[:, 40 + dj, col:col + 1]
                        S.op("dve", lambda po=po, g2=g2, dj=dj: V.scalar_tensor_tensor(
                            xT[:, dj, t0:t0 + n], P[po][:, :n], g2, xT[:, dj, t0:t0 + n], ALU.mult, ALU.add),
                            reads=[rP[po], rmod, rxT[dj][ti]], writes=[rxT[dj][ti]])

                items = [(u, ti) for u in range(NU) for ti in range(len(TO))]
                prev = None
                for k_, (u, ti) in enumerate(items):
                    emit_gu(u, ti, k_ % 2)
                    if not MOE_PIPE:
                        emit_dn(u, ti, k_ % 2)
                    elif prev is not None:
                        emit_dn(*prev)
                    if ti == 0:
                        for _ in range(3):
                            p_cast()
                    prev = (u, ti, k_ % 2)
                if MOE_PIPE:
                    emit_dn(*prev)
                S.barrier()
            dump(f"xout{l}", xT[:].rearrange("p a b -> p (a b)"), [r for rr in rxT for r in rr])

        if stop is None:
            with ExitStack() as ef:
                sq = sbuf(ef, "sqf", [128, 8, 512], BF16)
                rstd = sbuf(ef, "rstdf", [128, 512], F32)
                tmp = [sbuf(ef, f"ntf{i}", [128, 512], F32) for i in range(2)]
                yT = sbuf(ef, "yT", [128, 8, 512], F32)
                yo = [sbuf(ef, f"yo{i}", [128, D], F32) for i in range(2)]
                gfs = sbuf(ef, "gfs", [128, 8], F32)
                rsq, rrstd, ryT, rgfs = (S.res() for _ in range(4))
                rtmp, ryo = S.grid(2), S.grid(2)
                dyo = [S.dsem(), S.dsem()]
                S.op("dve", lambda: V.tensor_scalar(gfs[:], vecs[:, VOFF["fing"]:VOFF["fing"] + 8], float(np.sqrt(D)), None, ALU.mult),
                     reads=[rvecs], writes=[rgfs])
                for ti, (t0, n) in enumerate(TT[:4]):
                    for c in range(8):
                        S.op("act", lambda c=c: A.activation(sq[:, c, :], xT[:, c, t0:t0 + n], AF.Square), reads=[rxT[c][ti]], writes=[rsq])
                    p = nextp(0, 4)
                    S.group("pe", [(lambda c=c, p=p: T.matmul(P[p][:], onesb[:], sq[:, c, :], start=(c == 0), stop=(c == 7)))
                                   for c in range(8)], reads=[rsq, rmisc], writes=[rP[p]])
                    S.op("act", lambda p=p: A.activation(rstd[:], P[p][:], AF.Ln, bias=epsD[:, 0:1], scale=1.0),
                         reads=[rP[p], rmisc], writes=[rrstd])
                    S.op("act", lambda: A.activation(rstd[:], rstd[:], AF.Exp, scale=-0.5), reads=[rrstd], writes=[rrstd])
                    for c in range(8):
                        b = c % 2
                        S.op("dve", lambda c=c, b=b: V.tensor_tensor(tmp[b][:], xT[:, c, t0:t0 + n], rstd[:], ALU.mult),
                             reads=[rxT[c][ti], rrstd], writes=[rtmp[b]])
                        S.op("act", lambda c=c, b=b: A.activation(yT[:, c, :], tmp[b][:], AF.Copy, scale=gfs[:, c:c + 1]),
                             reads=[rtmp[b], rgfs], writes=[ryT])
                    for j in range(4):
                        tt = ti * 4 + j
                        b = tt % 2
                        for half in range(2):
                            pp = nextp(4, 8)
                            S.group("pe", [(lambda c=c, pp=pp, j=j: T.transpose(P[pp][:, (c % 4) * 128:(c % 4 + 1) * 128],
                                                                               yT[:, c, j * 128:(j + 1) * 128], ident))
                                           for c in range(half * 4, half * 4 + 4)], reads=[ryT, rcst], writes=[rP[pp]])
                            if half == 0:
                                S.op("act", lambda pp=pp, b=b: A.copy(yo[b][:, 0:512], P[pp][:]), reads=[rP[pp]], writes=[ryo[b]])
                            else:
                                S.op("dve", lambda pp=pp, b=b: V.tensor_copy(yo[b][:, 512:1024], P[pp][:]), reads=[rP[pp]], writes=[ryo[b]])
                        S.dma("sp", y_d[tt * 128:(tt + 1) * 128, :], yo[b][:], dyo[b], reads=[ryo[b]])
                S.barrier()
        S.barrier()
        print("[kernel] semaphore waits:", S.n_wait, "fused into instructions:", S.n_fused)
    return nc


def _chunked(v, n):
    return np.ascontiguousarray(np.asarray(v, np.float32).reshape(n, 128).T)


def make_consts():
    c = np.zeros((128, NCST), np.float32)
    c[:, 0:128] = np.eye(128, dtype=np.float32)
    s = np.arange(128)[:, None]
    t = np.arange(128)[None, :]
    same = (s // CH) == (t // CH)
    c[:, 128:256] = (same & (s <= t)).astype(np.float32)
    c[:, 256:384] = (same & (s >= t)).astype(np.float32)
    m = np.ones(512, np.float32)
    m[::CH] = 0.0
    c[:, 384:896] = m[None, :]
    for i in range(4):
        c[32 * i:32 * i + 32, 896 + i] = 1.0
    return c


def make_in_maps(inputs, cores):
    f = lambda k: np.asarray(inputs[k], np.float32)
    vecs = np.zeros((128, NV), np.float32)

    def put(name, arr):
        arr = np.asarray(arr, np.float32)
        vecs[:, VOFF[name]:VOFF[name] + arr.shape[1]] = arr
    put("n1g", np.concatenate([_chunked(f("norm1_g")[l], 8) for l in range(DEPTH)], 1))
    put("n2g", np.concatenate([_chunked(f("norm2_g")[l], 8) for l in range(DEPTH)], 1))
    put("fing", _chunked(f("final_norm_g"), 8))
    put("convb", np.concatenate([_chunked(f("conv_b")[l], 4) for l in range(DEPTH)], 1))
    put("lng", np.concatenate([_chunked(f("conv_ln_g")[l], 4) for l in range(DEPTH)], 1))
    put("lnb", np.concatenate([_chunked(f("conv_ln_b")[l], 4) for l in range(DEPTH)], 1))
    put("hg", np.stack([f("hgrn_norm_g")[l] for l in range(DEPTH)], 1))
    put("lbf", np.concatenate([_chunked(f("lb_fwd")[l], 4) for l in range(DEPTH)], 1))
    put("lbb", np.concatenate([_chunked(f("lb_bwd")[l], 4) for l in range(DEPTH)], 1))
    cw = f("conv_w")
    cwr = cw.reshape(DEPTH, 31, 4, 128).transpose(3, 0, 2, 1).reshape(128, DEPTH * 4 * 31)
    put("convw", cwr)
    put("rbias", np.broadcast_to(f("router_bias")[None, :], (128, NE)))
    bada = np.stack([_chunked(f("b_ada")[l], 48) for l in range(DEPTH)], 0)
    rwr = np.ascontiguousarray(f("router_w").reshape(8, 128, NE).transpose(1, 0, 2).reshape(128, 8 * NE))
    consts = make_consts()
    shared = {"w_ada": f("w_ada"), "b_ada_r": bada, "vecs": vecs, "consts": consts, "w_in": f("w_in"),
              "w_out": f("w_out"), "router_w_r": rwr, "w_gate": f("w_gate"), "w_up": f("w_up"), "w_down": f("w_down")}
    maps = []
    cc = f("c_ctx")
    for b in cores:
        c2 = np.stack([_chunked(f("c")[b], 8), _chunked(cc, 8)], 2).reshape(128, 16)
        m = dict(shared)
        m["x"] = np.ascontiguousarray(f("x")[b])
        m["ctx"] = np.ascontiguousarray(f("ctx")[b])
        m["c2"] = np.ascontiguousarray(c2)
        maps.append(m)
    return maps


_NC_CACHE = {}


def kernel(**inputs):
    if "nc" not in _NC_CACHE:
        _NC_CACHE["nc"] = build_program()
    nc = _NC_CACHE["nc"]
    maps = make_in_maps(inputs, list(range(8)))
    res = run_bass_kernel_spmd(nc, maps, core_ids=list(range(8)))
    return np.stack([np.asarray(r["y"], np.float32) for r in res.results], 0)
```

```python
import numpy as np
from contextlib import ExitStack, suppress
import concourse.bass as bass
import concourse.mybir as mybir
from concourse.bass_utils import run_bass_kernel_spmd

F32, BF16 = mybir.dt.float32, mybir.dt.bfloat16
AF = mybir.ActivationFunctionType
ALU = mybir.AluOpType
AX = mybir.AxisListType

D = 1024
NX = 2048
NCX = 256
NT = NX + NCX
DEPTH = 2
NE = 16
EPS = 1e-6
CH = 32
NCHK = NT // CH
TT = [(0, 512), (512, 512), (1024, 512), (1536, 512), (2048, 256)]
BIG = 1.0e4
MOE_PIPE = True
FUSE_WAITS = True

VOFF = {}
_o = 0
for _n, _w in (("n1g", 16), ("n2g", 16), ("fing", 8), ("convb", 8), ("lng", 8), ("lnb", 8),
               ("hg", 2), ("lbf", 8), ("lbb", 8), ("convw", 2 * 4 * 31), ("rbias", 16)):
    VOFF[_n] = _o
    _o += _w
NV = _o
COFF = {"ident": 0, "maskF": 128, "maskB": 256, "smask": 384}
NCST = 384 + 512 + 4


class Res:
    __slots__ = ("w", "r")

    def __init__(self):
        self.w = None
        self.r = {}


class Stream:
    def __init__(self, eng, sem, key):
        self.eng, self.sem, self.key = eng, sem, key
        self.count = 0
        self.waited = {}


class DSem:
    def __init__(self, handle, key):
        self.handle, self.key, self.count = handle, key, 0


class Sched:
    def __init__(self, nc, es):
        self.nc, self.es = nc, es
        self.semh = {}
        self.streams = {}
        for name, eng in (("pe", nc.tensor), ("act", nc.scalar), ("dve", nc.vector),
                          ("pool", nc.gpsimd), ("sp", nc.sync)):
            h = es.enter_context(nc.semaphore("s_" + name))
            self.semh[name] = h
            self.streams[name] = Stream(eng, h, name)
        self.dsems = []
        self.hist = {name: {} for name in self.streams}
        self.n_wait = 0
        self.n_fused = 0

    def res(self):
        return Res()

    def grid(self, *dims):
        if len(dims) == 1:
            return [Res() for _ in range(dims[0])]
        return [self.grid(*dims[1:]) for _ in range(dims[0])]

    def dsem(self):
        key = f"d{len(self.dsems)}"
        h = self.es.enter_context(self.nc.semaphore("s_" + key))
        self.semh[key] = h
        d = DSem(h, key)
        self.dsems.append(d)
        return d

    def _deps(self, st, reads, writes, fuse=False):
        deps = {}

        def add(tok, same_ok):
            if tok is None:
                return
            k, v = tok
            if k == st.key and not same_ok:
                return
            if deps.get(k, 0) < v:
                deps[k] = v
        for r in reads:
            add(r.w, True)
        for w in writes:
            add(w.w, False)
            for k, v in w.r.items():
                add((k, v), False)
        need = []
        for k, v in sorted(deps.items(), key=lambda kv: -kv[1]):
            if st.waited.get(k, 0) < v:
                st.waited[k] = v
                need.append((k, v))
                h = self.hist.get(k)
                if h is not None and v in h:
                    for k2, v2 in h[v].items():
                        if k2 != st.key and st.waited.get(k2, 0) < v2:
                            st.waited[k2] = v2
        need = [(k, v) for k, v in need if True]
        self.n_wait += len(need)
        if fuse and need:
            for k, v in need[:-1]:
                st.eng.wait_ge(self.semh[k], v)
            return need[-1]
        for k, v in need:
            st.eng.wait_ge(self.semh[k], v)
        return None

    def _snap(self, st):
        self.hist[st.key][st.count] = dict(st.waited)

    def _mark(self, key, val, reads, writes):
        for r in reads:
            r.r[key] = val
        for w in writes:
            w.w = (key, val)
            w.r = {}

    def _attach(self, st, ins, w):
        if w is not None:
            ins._wait_ge(self.semh[w[0]], st.eng.lower_val(w[1]))
            self.n_fused += 1
        return ins

    def op(self, sname, fn, reads=(), writes=()):
        st = self.streams[sname]
        w = self._deps(st, reads, writes, fuse=FUSE_WAITS)
        st.count += 1
        self._snap(st)
        self._attach(st, fn(), w).then_inc(st.sem, 1)
        self._mark(st.key, st.count, reads, writes)

    def group(self, sname, fns, reads=(), writes=()):
        st = self.streams[sname]
        w = self._deps(st, reads, writes, fuse=FUSE_WAITS)
        st.count += 1
        self._snap(st)
        n = len(fns)
        for i, fn in enumerate(fns):
            ins = fn()
            if i == 0:
                self._attach(st, ins, w)
            if i == n - 1:
                ins.then_inc(st.sem, 1)
        self._mark(st.key, st.count, reads, writes)

    def dma(self, sname, out, in_, ds, reads=(), writes=()):
        st = self.streams[sname]
        self._deps(st, reads, writes)
        ds.count += 16
        st.eng.dma_start(out=out, in_=in_).then_inc(ds.handle, 16)
        self._mark(ds.key, ds.count, reads, writes)

    def barrier(self):
        for st in self.streams.values():
            for o in self.streams.values():
                if o is not st and o.count > st.waited.get(o.key, 0):
                    st.waited[o.key] = o.count
                    st.eng.wait_ge(o.sem, o.count)
            for d in self.dsems:
                if d.count > st.waited.get(d.key, 0):
                    st.waited[d.key] = d.count
                    st.eng.wait_ge(d.handle, d.count)


class _Stop(Exception):
    pass


def build_program(nlayers=DEPTH, dbg=None, stop=None):
    dbg = dbg or {}
    nc = bass.Bass("TRN2", target_bir_lowering=False)
    dr = lambda n, s, k="ExternalInput": nc.dram_tensor(n, s, F32, kind=k).ap()
    x_d = dr("x", [NX, D])
    ctx_d = dr("ctx", [NCX, D])
    c2_d = dr("c2", [128, 16])
    wada_d = dr("w_ada", [DEPTH, D, 6 * D])
    bada_d = dr("b_ada_r", [DEPTH, 128, 48])
    vecs_d = dr("vecs", [128, NV])
    cst_d = dr("consts", [128, NCST])
    win_d = dr("w_in", [DEPTH, D, 3584])
    wout_d = dr("w_out", [DEPTH, D, D])
    rw_d = dr("router_w_r", [128, 8 * NE])
    wg_d = dr("w_gate", [DEPTH, NE, D, D])
    wu_d = dr("w_up", [DEPTH, NE, D, D])
    wd_d = dr("w_down", [DEPTH, NE, D, D])
    y_d = dr("y", [NX, D], "ExternalOutput")
    dbg_d = {k: nc.dram_tensor("dbg_" + k, list(s[0]), BF16 if s[1] == "bf16" else F32, kind="ExternalOutput").ap()
             for k, s in dbg.items()}

    es = ExitStack()
    with suppress(_Stop), es:
        S = Sched(nc, es)
        V, A, G, T = nc.vector, nc.scalar, nc.gpsimd, nc.tensor

        nsb = [0]

        def sbuf(es_, name, shape, dt):
            nsb[0] += 1
            return es_.enter_context(nc.sbuf_tensor(f"sb{nsb[0]}_{name}", shape, dt))

        xT = sbuf(es, "xT", [128, 8, NT], F32)
        hT = sbuf(es, "hT", [128, 8, NT], BF16)
        cst = sbuf(es, "cst", [128, NCST], F32)
        vecs = sbuf(es, "vecs", [128, NV], F32)
        identb = sbuf(es, "identb", [128, 128], BF16)
        onesb = sbuf(es, "onesb", [128, 128], BF16)
        zerob = sbuf(es, "zerob", [128, 128], BF16)
        epsD = sbuf(es, "epsD", [128, 3], F32)
        mod = sbuf(es, "mod", [128, 48, 2], F32)
        se = sbuf(es, "se", [128, 2, 8, 2], F32)
        lbt = sbuf(es, "lbt", [128, 2, 4, 3], F32)
        rw = sbuf(es, "rw", [128, 8, NE], F32)
        P = [es.enter_context(nc.psum_tensor(f"ps{i}", [128, 512], F32)) for i in range(8)]
        rP = S.grid(8)
        rxT = S.grid(8, 5)
        rhT = S.grid(5)
        rcst, rvecs, rmisc, rmod, rse, rlbt, rrw = (S.res() for _ in range(7))
        ident = cst[:, 0:128]
        maskF = cst[:, 128:256]
        maskB = cst[:, 256:384]
        smask = cst[:, 384:896]
        dconst = S.dsem()
        vv = lambda n, i: vecs[:, VOFF[n] + i: VOFF[n] + i + 1]

        S.dma("sp", cst[:], cst_d[:, :], S.dsem(), writes=[rcst])
        S.dma("sp", vecs[:], vecs_d[:, :], S.dsem(), writes=[rvecs])
        S.dma("sp", rw[:].rearrange("p a b -> p (a b)"), rw_d[:, :], S.dsem(), writes=[rrw])
        S.op("dve", lambda: V.memset(onesb[:], 1.0), writes=[rmisc])
        S.op("dve", lambda: V.memset(zerob[:], 0.0), writes=[rmisc])
        S.op("dve", lambda: V.memset(epsD[:, 0:1], float(D * EPS)), writes=[rmisc])
        S.op("dve", lambda: V.memset(epsD[:, 1:3], float(EPS)), writes=[rmisc])
        S.op("dve", lambda: V.tensor_copy(identb[:], cst[:, 0:128]), reads=[rcst], writes=[rmisc])

        pctr = [0]

        def nextp(lo, hi):
            p = lo + pctr[0] % (hi - lo)
            pctr[0] += 1
            return p

        def dump(name, ap_sb, res_list):
            if name in dbg_d:
                ds = S.dsem()
                S.dma("sp", dbg_d[name], ap_sb, ds, reads=res_list)

        with ExitStack() as ea:
            xin = [sbuf(ea, f"xin{i}", [128, D], F32) for i in range(2)]
            rxin = S.grid(2)
            dxin = [S.dsem(), S.dsem()]
            for tt in range(18):
                b = tt % 2
                src = x_d[tt * 128:(tt + 1) * 128, :] if tt < 16 else ctx_d[(tt - 16) * 128:(tt - 15) * 128, :]
                S.dma("sp", xin[b][:], src, dxin[b], writes=[rxin[b]])
                for half in range(2):
                    p = nextp(0, 4)
                    fns = [(lambda c=c, p=p, b=b: T.transpose(P[p][:, (c % 4) * 128:(c % 4 + 1) * 128],
                                                              xin[b][:, c * 128:(c + 1) * 128], ident))
                           for c in range(half * 4, half * 4 + 4)]
                    S.group("pe", fns, reads=[rxin[b], rcst], writes=[rP[p]])
                    ws = [rxT[c][tt // 4] for c in range(half * 4, half * 4 + 4)]
                    eng = "dve" if half == 0 else "act"
                    dst = xT[:, half * 4:half * 4 + 4, tt * 128:(tt + 1) * 128]
                    srcp = P[p][:].rearrange("p (c t) -> p c t", c=4)
                    if eng == "dve":
                        S.op("dve", lambda dst=dst, srcp=srcp: V.tensor_copy(dst, srcp), reads=[rP[p]], writes=ws)
                    else:
                        S.op("act", lambda dst=dst, srcp=srcp: A.copy(dst, srcp), reads=[rP[p]], writes=ws)
            S.barrier()

        allx = lambda ti: [rxT[c][ti] for c in range(8)]

        def norm_mod(nidx, tiles, h2f=None, rh2f=None, es_=None):
            sq = sbuf(es_, f"sq{nidx}", [128, 8, 512], BF16)
            rstd = sbuf(es_, f"rstd{nidx}", [128, 512], F32)
            tmp = [sbuf(es_, f"nt{nidx}_{i}", [128, 512], F32) for i in range(2)]
            rsq, rrstd = S.res(), S.res()
            rtmp = S.grid(2)
            shb = 0 if nidx == 0 else 24
            for ti, (t0, n) in enumerate(tiles):
                col = 0 if t0 < NX else 1
                for c in range(8):
                    S.op("act", lambda c=c: A.activation(sq[:, c, :n], xT[:, c, t0:t0 + n], AF.Square),
                         reads=[rxT[c][ti]], writes=[rsq])
                p = nextp(0, 4)
                S.group("pe", [(lambda c=c, p=p: T.matmul(P[p][:, :n], onesb[:], sq[:, c, :n], start=(c == 0), stop=(c == 7)))
                               for c in range(8)], reads=[rsq, rmisc], writes=[rP[p]])
                S.op("act", lambda p=p: A.activation(rstd[:, :n], P[p][:, :n], AF.Ln, bias=epsD[:, 0:1], scale=1.0),
                     reads=[rP[p], rmisc], writes=[rrstd])
                S.op("act", lambda: A.activation(rstd[:, :n], rstd[:, :n], AF.Exp, scale=-0.5), reads=[rrstd], writes=[rrstd])
                for c in range(8):
                    b = c % 2
                    S.op("dve", lambda c=c, b=b: V.tensor_tensor(tmp[b][:, :n], xT[:, c, t0:t0 + n], rstd[:, :n], ALU.mult),
                         reads=[rxT[c][ti], rrstd], writes=[rtmp[b]])
                    sc_ap = se[:, nidx, c, col:col + 1]
                    sh_ap = mod[:, shb + c, col:col + 1]
                    if h2f is None:
                        S.op("act", lambda c=c, b=b, sc_ap=sc_ap, sh_ap=sh_ap: A.activation(
                            hT[:, c, t0:t0 + n], tmp[b][:, :n], AF.Identity, bias=sh_ap, scale=sc_ap),
                            reads=[rtmp[b], rse, rmod], writes=[rhT[ti]])
                    else:
                        S.op("act", lambda c=c, b=b, sc_ap=sc_ap, sh_ap=sh_ap: A.activation(
                            h2f[:, c, t0:t0 + n], tmp[b][:, :n], AF.Identity, bias=sh_ap, scale=sc_ap),
                            reads=[rtmp[b], rse, rmod], writes=[rh2f[ti]])
                        S.op("pool", lambda c=c: G.tensor_copy(hT[:, c, t0:t0 + n], h2f[:, c, t0:t0 + n]),
                             reads=[rh2f[ti]], writes=[rhT[ti]])

        class WChunks:
            def __init__(self, es_, tag, srcs, ceng, nst=3, nbf=3):
                self.srcs, self.ceng = srcs, ceng
                self.st = [sbuf(es_, f"wst{tag}{i}", [128, 8, 128], F32) for i in range(nst)]
                self.bf = [sbuf(es_, f"wbf{tag}{i}", [128, 8, 128], BF16) for i in range(nbf)]
                self.rst, self.rbf = S.grid(nst), S.grid(nbf)
                self.dst = [S.dsem() for _ in range(nst)]
                self.nl = 0
                self.nc_ = 0
                self.look = nbf - 1

            def _load(self):
                if self.nl >= len(self.srcs):
                    return
                i = self.nl % len(self.st)
                S.dma("sp", self.st[i][:], self.srcs[self.nl], self.dst[i], writes=[self.rst[i]])
                self.nl += 1

            def _cast(self):
                if self.nc_ >= len(self.srcs):
                    return
                i = self.nc_ % len(self.st)
                j = self.nc_ % len(self.bf)
                src, dst = self.st[i], self.bf[j]
                if self.ceng == "pool":
                    S.op("pool", lambda: G.tensor_copy(dst[:], src[:]), reads=[self.rst[i]], writes=[self.rbf[j]])
                else:
                    S.op("act", lambda: A.copy(dst[:], src[:]), reads=[self.rst[i]], writes=[self.rbf[j]])
                self.nc_ += 1

            def prefetch(self):
                for _ in range(len(self.st)):
                    self._load()
                for _ in range(self.look):
                    self._cast()
                    self._load()

            def get(self, k):
                while self.nc_ <= k:
                    self._cast()
                    self._load()
                j = k % len(self.bf)
                return self.bf[j], self.rbf[j]

            def after(self, k):
                while self.nc_ <= k + self.look and self.nc_ < len(self.srcs):
                    self._cast()
                    self._load()

        def proj_fm(wt, rwt, tiles, consume, plo=0, phi=4):
            for ti, (t0, n) in enumerate(tiles):
                p = nextp(plo, phi)
                S.group("pe", [(lambda kc=kc, p=p: T.matmul(P[p][:, :n], wt[:, kc, :], hT[:, kc, t0:t0 + n],
                                                            start=(kc == 0), stop=(kc == 7))) for kc in range(8)],
                        reads=[rwt, rhT[ti]], writes=[rP[p]])
                consume(p, ti, t0, n)

        def ckpt(name):
            if stop == name:
                S.barrier()
                raise _Stop()

        for l in ([] if stop == "load" else range(nlayers)):
            last = (l == DEPTH - 1)
            TO = TT[:4] if last else TT
            with ExitStack() as eb:
                c2 = sbuf(eb, "c2", [128, 8, 2], F32)
                sc2 = sbuf(eb, "sc2", [128, 8, 2], F32)
                bada = sbuf(eb, "bada", [128, 48], F32)
                wa = [sbuf(eb, f"wa{i}", [128, 8, 512], F32) for i in range(2)]
                rc2, rsc2, rbada = S.res(), S.res(), S.res()
                rwa = S.grid(2)
                dwa = [S.dsem(), S.dsem()]
                S.dma("sp", c2[:].rearrange("p a b -> p (a b)"), c2_d[:, :], S.dsem(), writes=[rc2])
                S.dma("sp", bada[:], bada_d[l, :, :], S.dsem(), writes=[rbada])
                S.op("act", lambda: A.activation(sc2[:], c2[:], AF.Silu), reads=[rc2], writes=[rsc2])
                pm = 7
                for g in range(12):
                    b = g % 2
                    S.dma("sp", wa[b][:], wada_d[l, :, g * 512:(g + 1) * 512].rearrange("(kc p) f -> p kc f", p=128),
                          dwa[b], writes=[rwa[b]])
                    fns = []
                    for j in range(4):
                        fc = g * 4 + j
                        for kc in range(8):
                            fns.append(lambda j=j, fc=fc, kc=kc, b=b: T.matmul(
                                P[pm][:, fc * 2:fc * 2 + 2], wa[b][:, kc, j * 128:(j + 1) * 128], sc2[:, kc, :],
                                start=(kc == 0), stop=(kc == 7)))
                    S.group("pe", fns, reads=[rwa[b], rsc2], writes=[rP[pm]])
                S.op("dve", lambda: V.tensor_tensor(mod[:], P[pm][:, 0:96].rearrange("p (a b) -> p a b", b=2),
                                                    bada[:].unsqueeze(2).to_broadcast([128, 48, 2]), ALU.add),
                     reads=[rP[pm], rbada], writes=[rmod])
                for nidx, (scb, gname) in enumerate(((8, "n1g"), (32, "n2g"))):
                    S.op("dve", lambda nidx=nidx, scb=scb: V.tensor_scalar(
                        se[:, nidx, :, :], mod[:, scb:scb + 8, :], 1.0, float(np.sqrt(D)), ALU.add, ALU.mult),
                        reads=[rmod], writes=[rse])
                    gap = vecs[:, VOFF[gname] + l * 8: VOFF[gname] + l * 8 + 8].unsqueeze(2).to_broadcast([128, 8, 2])
                    S.op("dve", lambda nidx=nidx, gap=gap: V.tensor_tensor(se[:, nidx, :, :], se[:, nidx, :, :], gap, ALU.mult),
                         reads=[rse, rvecs], writes=[rse])
                for di, nm in enumerate(("lbf", "lbb")):
                    l0 = vecs[:, VOFF[nm]:VOFF[nm] + 4]
                    l1 = vecs[:, VOFF[nm] + 4:VOFF[nm] + 8]
                    if l == 0:
                        S.op("dve", lambda di=di: V.memset(lbt[:, di, :, 0], 0.0), writes=[rlbt])
                    else:
                        S.op("dve", lambda di=di, l0=l0, l1=l1: V.tensor_tensor(lbt[:, di, :, 0], l1, l0, ALU.subtract),
                             reads=[rvecs], writes=[rlbt])
                        S.op("act", lambda di=di: A.activation(lbt[:, di, :, 0], lbt[:, di, :, 0], AF.Sigmoid),
                             reads=[rlbt], writes=[rlbt])
                    S.op("dve", lambda di=di: V.tensor_scalar(lbt[:, di, :, 1], lbt[:, di, :, 0], -1.0, 1.0, ALU.mult, ALU.add),
                         reads=[rlbt], writes=[rlbt])
                    S.op("dve", lambda di=di: V.tensor_scalar(lbt[:, di, :, 2], lbt[:, di, :, 0], 1.0, -1.0, ALU.mult, ALU.add),
                         reads=[rlbt], writes=[rlbt])
                S.barrier()
            dump(f"mod{l}", mod[:].rearrange("p a b -> p (a b)"), [rmod])

            with ExitStack() as em:
                rym = S.grid(8, 5)
                with ExitStack() as en:
                    norm_mod(0, TT, es_=en)
                    S.barrier()
                dump(f"h{l}", hT[:].rearrange("p a b -> p (a b)"), rhT)
                if stop == "norm1":
                    break
                srcs = []
                wsl = lambda grp, sub: win_d[l, :, grp * 512 + sub * 128: grp * 512 + (sub + 1) * 128].rearrange(
                    "(kc p) f -> p kc f", p=128)
                wrow = lambda r0: wout_d[l, r0:r0 + 128, :].rearrange("p (dj f) -> p dj f", f=128)
                for h in range(4):
                    srcs += [wsl(3, h), wsl(4, h), wsl(2, h), wsl(5, h), wsl(2, h), wsl(6, h), wrow(512 + h * 128)]
                for j in range(4):
                    srcs += [wsl(0, j), wsl(1, j)]
                W = WChunks(em, "m", srcs, "act", nst=2, nbf=3)
                W.prefetch()
                wk = [0]

                def nextw():
                    k = wk[0]
                    wk[0] += 1
                    t_, r_ = W.get(k)
                    return k, t_, r_

                with ExitStack() as eh:
                    o_acc = sbuf(eh, "o_acc", [128, NT], F32)
                    Vh = sbuf(eh, "Vh", [128, 18, 128], BF16)
                    Vx = [sbuf(eh, f"Vx{i}", [128, 4, 128], BF16) for i in range(2)]
                    yh = sbuf(eh, "yh", [128, NT], BF16)
                    QT = sbuf(eh, "QT", [128, NT], BF16)
                    KT = sbuf(eh, "KT", [128, NT], BF16)
                    KHT = sbuf(eh, "KHT", [128, 18, 128], BF16)
                    dS = sbuf(eh, "dS", [128, NCHK, 128], BF16)
                    Dall = sbuf(eh, "Dall", [128, NCHK], F32)
                    Dpos = sbuf(eh, "Dpos", [128, NCHK], F32)
                    Wt = [[sbuf(eh, f"W{i}_{b_}", [128, 512], F32) for i in range(6)] for b_ in range(2)]
                    rWt = [[S.res() for i in range(6)] for b_ in range(2)]
                    W1 = Wt[0][0]
                    KHt = sbuf(eh, "KHt", [128, 512], BF16)
                    attm = [sbuf(eh, f"attm{i}", [128, 128], BF16) for i in range(2)]
                    ro_acc = S.grid(5)
                    rVh, rKHT, rdS, rDall, rDpos = (S.res() for _ in range(5))
                    rdSh = S.grid(2)
                    rQT, rKT, ryh = S.grid(5), S.grid(5), S.grid(5)
                    rKHt = S.res()
                    rW1 = rWt[0][0]
                    Wq, rWq = Wt[0][4], rWt[0][4]
                    sqh, rsqh, ro, rro = KHt, rKHt, W1, rW1
                    rattm, rVx = S.grid(2), S.grid(2)
                    if l == 0:
                        print("[kernel] SBUF bytes free inside HGRN scope:", nc.sbuf_bytes_remaining)
                    NXC = NX // CH
                    NCC = NCX // CH
                    CPT = 128 // CH
                    for h in range(4):
                        k, wt, rwt = nextw()
                        for g4 in range(5):
                            tts = range(g4 * 4, min(g4 * 4 + 4, 18))
                            p = nextp(0, 4)
                            fns = []
                            for tt in tts:
                                for kc in range(8):
                                    fns.append(lambda tt=tt, kc=kc, p=p: T.matmul(
                                        P[p][:, (tt % 4) * 128:(tt % 4 + 1) * 128], hT[:, kc, tt * 128:(tt + 1) * 128],
                                        wt[:, kc, :], start=(kc == 0), stop=(kc == 7)))
                            S.group("pe", fns, reads=[rwt, rhT[g4]], writes=[rP[p]])
                            nt_ = len(tts)
                            S.op("act", lambda p=p, g4=g4, nt_=nt_: A.copy(
                                Vh[:, g4 * 4:g4 * 4 + nt_, :], P[p][:, :nt_ * 128].rearrange("p (a b) -> p a b", b=128)),
                                reads=[rP[p]], writes=[rVh])
                        W.after(k)
                        ckpt("hg_v")
                        for di in range(2):
                            fwd = di == 0
                            kf, wf, rwf = nextw()
                            kq, wq, rwq = nextw()
                            lb_ap = lbt[:, di, h, 0:1]
                            oml_ap = lbt[:, di, h, 1:2]
                            noml_ap = lbt[:, di, h, 2:3]
                            def prep_a(ti):
                                t0, n = TT[ti]
                                nch, c0 = n // CH, t0 // CH
                                W1, W2, W3, W4, Wq, W5 = Wt[ti % 2]
                                rW1, rW2, rW3, rW4, rWq, rW5 = rWt[ti % 2]
                                v3 = lambda ap: ap[:, :n].rearrange("p (c k) -> p c k", k=CH)
                                pf = nextp(0, 4)
                                S.group("pe", [(lambda kc=kc, pf=pf: T.matmul(P[pf][:, :n], wf[:, kc, :], hT[:, kc, t0:t0 + n],
                                                                             start=(kc == 0), stop=(kc == 7))) for kc in range(8)],
                                        reads=[rwf, rhT[ti]], writes=[rP[pf]])
                                pq = nextp(0, 4)
                                S.group("pe", [(lambda kc=kc, pq=pq: T.matmul(P[pq][:, :n], wq[:, kc, :], hT[:, kc, t0:t0 + n],
                                                                             start=(kc == 0), stop=(kc == 7))) for kc in range(8)],
                                        reads=[rwq, rhT[ti]], writes=[rP[pq]])
                                S.op("act", lambda: A.activation(W1[:, :n], P[pf][:, :n], AF.Sigmoid), reads=[rP[pf]], writes=[rW1])
                                S.op("act", lambda: A.activation(Wq[:, :n], P[pq][:, :n], AF.Sigmoid), reads=[rP[pq]], writes=[rWq])
                                yield
                                S.op("act", lambda: A.activation(W2[:, :n], W1[:, :n], AF.Ln, bias=lb_ap, scale=oml_ap),
                                     reads=[rW1, rlbt], writes=[rW2])
                                S.op("dve", lambda: V.tensor_tensor(Wq[:, :n], P[pq][:, :n], Wq[:, :n], ALU.mult), reads=[rP[pq], rWq], writes=[rWq])
                                S.op("dve", lambda: V.tensor_scalar(W5[:, :n], W1[:, :n], noml_ap, oml_ap, ALU.mult, ALU.add),
                                     reads=[rW1, rlbt], writes=[rW5])
                                yield
                                S.op("dve", lambda: V.tensor_tensor_scan(W3[:, :n], smask[:, :n], W2[:, :n], 0.0, ALU.mult, ALU.add),
                                     reads=[rW2, rcst], writes=[rW3])
                                if fwd:
                                    bb, rbb = W3, rW3
                                    bl = v3(W3)[:, :, CH - 1]
                                else:
                                    tc_ = v3(W3)[:, :, CH - 1:CH].to_broadcast([128, nch, CH])
                                    S.op("dve", lambda: V.tensor_tensor(v3(W4), tc_, v3(W3), ALU.subtract), reads=[rW3], writes=[rW4])
                                    S.op("dve", lambda: V.tensor_tensor(W4[:, :n], W4[:, :n], W2[:, :n], ALU.add),
                                         reads=[rW4, rW2], writes=[rW4])
                                    bb, rbb = W4, rW4
                                    bl = v3(W4)[:, :, 0]
                                yield
                                S.op("act", lambda: A.activation(Dall[:, c0:c0 + nch], bl, AF.Exp), reads=[rbb], writes=[rDall])
                                yield
                                res_[ti] = (bb, rbb)

                            def prep_b(ti):
                                bb, rbb = res_[ti]
                                t0, n = TT[ti]
                                nch, c0 = n // CH, t0 // CH
                                W1, W2, W3, W4, Wq, W5 = Wt[ti % 2]
                                rW1, rW2, rW3, rW4, rWq, rW5 = rWt[ti % 2]
                                v3 = lambda ap: ap[:, :n].rearrange("p (c k) -> p c k", k=CH)
                                S.op("act", lambda: A.activation(W2[:, :n], bb[:, :n], AF.Exp), reads=[rbb, rW2], writes=[rW2])
                                S.op("act", lambda: A.activation(W1[:, :n], bb[:, :n], AF.Exp, scale=-1.0), reads=[rbb, rW1], writes=[rW1])
                                yield
                                S.op("dve", lambda: V.tensor_tensor(QT[:, t0:t0 + n], Wq[:, :n], W2[:, :n], ALU.mult),
                                     reads=[rWq, rW2], writes=[rQT[ti]])
                                S.op("dve", lambda: V.tensor_tensor(W5[:, :n], W5[:, :n], W1[:, :n], ALU.mult),
                                     reads=[rW5, rW1], writes=[rW5])
                                yield
                                S.op("act", lambda: A.copy(KT[:, t0:t0 + n], W5[:, :n]), reads=[rW5], writes=[rKT[ti]])
                                dbc = Dall[:, c0:c0 + nch].unsqueeze(2).to_broadcast([128, nch, CH])
                                S.op("dve", lambda: V.tensor_tensor(v3(KHt), v3(W5), dbc, ALU.mult), reads=[rW5, rDall], writes=[rKHt])
                                yield
                                pk = nextp(4, 6)
                                ntile = n // 128
                                pkb = P[pk][:].bitcast(BF16)
                                S.group("pe", [(lambda j=j: T.transpose(pkb[:, j * 128:(j + 1) * 128],
                                                                        KHt[:, j * 128:(j + 1) * 128], identb[:]))
                                               for j in range(ntile)], reads=[rKHt, rmisc], writes=[rP[pk]])
                                yield
                                S.op("act", lambda: A.copy(KHT[:, t0 // 128:t0 // 128 + ntile, :],
                                                           pkb[:, :ntile * 128].rearrange("p (a b) -> p a b", b=128)),
                                     reads=[rP[pk]], writes=[rKHT])
                                yield
                            res_ = {}

                            def run_zip(g1, g2):
                                gens = [g for g in (g1, g2) if g is not None]
                                while gens:
                                    for g in list(gens):
                                        try:
                                            next(g)
                                        except StopIteration:
                                            gens.remove(g)
                            run_zip(prep_a(0), None)
                            for ti in range(len(TT)):
                                run_zip(prep_a(ti + 1) if ti + 1 < len(TT) else None, prep_b(ti))
                            W.after(kq)
                            ckpt("hg_prep")
                            order = (list(range(NXC, NCHK)) + list(range(NXC))) if fwd else list(range(NCHK - 1, -1, -1))
                            pos = {c: i for i, c in enumerate(order)}
                            for tt in range(18):
                                vb = tt % 2
                                S.op("dve", lambda vb=vb, tt=tt: V.tensor_tensor(
                                    Vx[vb][:], Vh[:, tt, :].unsqueeze(1).to_broadcast([128, CPT, 128]),
                                    cst[:, 896:896 + CPT].unsqueeze(2).to_broadcast([128, CPT, 128]), ALU.mult),
                                    reads=[rVh, rcst], writes=[rVx[vb]])
                                p = nextp(6, 8)
                                S.group("pe", [lambda tt=tt, vb=vb, p=p: T.matmul(
                                    P[p][:, :CPT * 128], KHT[:, tt, :], Vx[vb][:].rearrange("p a b -> p (a b)"), start=True, stop=True)],
                                    reads=[rKHT, rVx[vb]], writes=[rP[p]])
                                p0 = pos[tt * CPT]
                                if fwd:
                                    dst = dS[:, p0:p0 + CPT, :]
                                else:
                                    dst = dS[:, p0:p0 - CPT:-1, :] if p0 - CPT >= 0 else dS[:, p0::-1, :]
                                S.op("act", lambda dst=dst, p=p: A.copy(dst, P[p][:, :CPT * 128].rearrange("p (c v) -> p c v", v=128)),
                                     reads=[rP[p]], writes=[rdS, rdSh[0], rdSh[1]])
                            ckpt("hg_ds")
                            if fwd:
                                S.op("dve", lambda: V.tensor_copy(Dpos[:, NCC:NCHK], Dall[:, 0:NXC]), reads=[rDall], writes=[rDpos])
                                S.op("dve", lambda: V.tensor_copy(Dpos[:, 1:NCC], Dall[:, NXC + 1:NCHK]), reads=[rDall], writes=[rDpos])
                            else:
                                S.op("dve", lambda: V.tensor_copy(Dpos[:, 1:NCHK], Dall[:, NCHK - 2::-1]), reads=[rDall], writes=[rDpos])
                            S.op("dve", lambda: V.memset(Dpos[:, 0:1], 0.0), reads=[rDpos], writes=[rDpos])
                            for pp in range(1, NCHK):
                                for hf in range(2):
                                    vs = slice(hf * 64, hf * 64 + 64)
                                    S.op("dve", lambda pp=pp, vs=vs: V.scalar_tensor_tensor(
                                        dS[:, pp, vs], dS[:, pp - 1, vs], Dpos[:, pp:pp + 1], dS[:, pp, vs], ALU.mult, ALU.add),
                                        reads=[rdSh[hf], rDpos], writes=[rdSh[hf]])
                            ckpt("hg_scan")
                            mask = maskF if fwd else maskB
                            for g4 in range(5):
                                tts = list(range(g4 * 4, min(g4 * 4 + 4, 18)))
                                po = nextp(4, 6)
                                for tt in tts:
                                    off = (tt % 4) * 128
                                    fns = []
                                    for c in range(tt * CPT, (tt + 1) * CPT):
                                        pc = pos[c]
                                        lhs = zerob[:] if pc == 0 else dS[:, pc - 1, :]
                                        co = off + (c % CPT) * CH
                                        fns.append(lambda c=c, lhs=lhs, co=co, po=po: T.matmul(
                                            P[po][:, co:co + CH], lhs, QT[:, c * CH:(c + 1) * CH], start=(c % CPT == 0), stop=False,
                                            skip_group_check=True))
                                    S.group("pe", fns, reads=[rdS, rdSh[0], rdSh[1], rQT[g4], rmisc], writes=[rP[po]])
                                    pa = nextp(6, 8)
                                    S.group("pe", [lambda tt=tt, pa=pa: T.matmul(P[pa][:, 0:128], KT[:, tt * 128:(tt + 1) * 128],
                                                                                QT[:, tt * 128:(tt + 1) * 128], start=True, stop=True)],
                                            reads=[rKT[g4], rQT[g4]], writes=[rP[pa]])
                                    ab = tt % 2
                                    S.op("dve", lambda pa=pa, ab=ab, mask=mask: V.tensor_tensor(attm[ab][:], P[pa][:, 0:128], mask, ALU.mult),
                                         reads=[rP[pa], rcst], writes=[rattm[ab]])
                                    S.group("pe", [lambda tt=tt, ab=ab, off=off, po=po: T.matmul(
                                        P[po][:, off:off + 128], Vh[:, tt, :], attm[ab][:], start=False, stop=True, skip_group_check=True)],
                                        reads=[rVh, rattm[ab]], writes=[rP[po]])
                                nn = len(tts) * 128
                                t0 = g4 * 512
                                if fwd:
                                    S.op("act", lambda po=po, nn=nn, t0=t0: A.copy(o_acc[:, t0:t0 + nn], P[po][:, :nn]),
                                         reads=[rP[po]], writes=[ro_acc[g4]])
                                else:
                                    S.op("dve", lambda po=po, nn=nn, t0=t0: V.tensor_tensor(o_acc[:, t0:t0 + nn], o_acc[:, t0:t0 + nn],
                                                                                          P[po][:, :nn], ALU.add),
                                         reads=[rP[po], ro_acc[g4]], writes=[ro_acc[g4]])
                            if stop == "hg_o" + str(di):
                                dump("QT", QT[:], rQT); dump("KT", KT[:], rKT); dump("dS", dS[:].rearrange("p c v -> p (c v)"), [rdS])
                                dump("Dall", Dall[:], [rDall]); dump("Dpos", Dpos[:], [rDpos]); dump("oacc0_0", o_acc[:], ro_acc)
                            ckpt("hg_o" + str(di))
                        if f"oacc{l}_{h}" in dbg_d:
                            dump(f"oacc{l}_{h}", o_acc[:], ro_acc)
                        W1, rW1, Wq, rWq = Wt[0][0], rWt[0][0], Wt[0][4], rWt[0][4]
                        ro, rro = W1, rW1
                        ko, wo_, rwo = nextw()
                        kr, wr_, rwr = nextw()
                        gh = vv("hg", l)
                        for ti, (t0, n) in enumerate(TO):
                            S.op("act", lambda: A.activation(sqh[:, :n], o_acc[:, t0:t0 + n], AF.Square), reads=[ro_acc[ti]], writes=[rsqh])
                            p = nextp(0, 4)
                            S.group("pe", [lambda p=p: T.matmul(P[p][:, :n], onesb[:], sqh[:, :n], start=True, stop=True)],
                                    reads=[rsqh, rmisc], writes=[rP[p]])
                            S.op("act", lambda p=p: A.activation(ro[:, :n], P[p][:, :n], AF.Ln, bias=epsD[:, 1:2], scale=1.0 / 128),
                                 reads=[rP[p], rmisc], writes=[rro])
                            S.op("act", lambda: A.activation(ro[:, :n], ro[:, :n], AF.Exp, scale=-0.5), reads=[rro], writes=[rro])
                            S.op("dve", lambda: V.tensor_tensor(ro[:, :n], ro[:, :n], o_acc[:, t0:t0 + n], ALU.mult),
                                 reads=[rro, ro_acc[ti]], writes=[rro])
                            p2 = nextp(0, 4)
                            S.group("pe", [(lambda kc=kc, p2=p2: T.matmul(P[p2][:, :n], wo_[:, kc, :], hT[:, kc, t0:t0 + n],
                                                                         start=(kc == 0), stop=(kc == 7))) for kc in range(8)],
                                    reads=[rwo, rhT[ti]], writes=[rP[p2]])
                            S.op("act", lambda p2=p2: A.activation(Wq[:, :n], P[p2][:, :n], AF.Silu), reads=[rP[p2]], writes=[rWq])
                            S.op("dve", lambda: V.scalar_tensor_tensor(yh[:, t0:t0 + n], ro[:, :n], gh, Wq[:, :n], ALU.mult, ALU.mult),
                                 reads=[rro, rWq, rvecs], writes=[ryh[ti]])
                            col = 0 if t0 < NX else 1
                            for dj in range(8):
                                p3 = nextp(4, 8)
                                S.group("pe", [lambda p3=p3, dj=dj: T.matmul(P[p3][:, :n], wr_[:, dj, :], yh[:, t0:t0 + n], start=True, stop=True)],
                                        reads=[rwr, ryh[ti]], writes=[rP[p3]])
                                g1 = mod[:, 16 + dj, col:col + 1]
                                S.op("dve", lambda p3=p3, g1=g1, dj=dj: V.scalar_tensor_tensor(
                                    xT[:, dj, t0:t0 + n], P[p3][:, :n], g1, xT[:, dj, t0:t0 + n], ALU.mult, ALU.add),
                                    reads=[rP[p3], rmod, rxT[dj][ti]], writes=[rxT[dj][ti]])
                        if f"yh{l}_{h}" in dbg_d:
                            dump(f"yh{l}_{h}", yh[:], ryh)
                        W.after(kr)
                    S.barrier()
                if stop == "hgrn":
                    break
                ecm = ExitStack()
                em.enter_context(ecm)
                ymc = sbuf(ecm, "ymc", [128, 4, NT], BF16)
                with ExitStack() as ec:
                    rowo = (l % 2 == 0)
                    conv_tiles = TT if not last else TT[:4]
                    UPL = (32 * 79 + 31) if rowo else (62 * 64)
                    Upad = [sbuf(ec, f"Upad{i}", [128, UPL + 286], BF16) for i in range(2)]
                    Dg = [sbuf(ec, f"Dg{i}", [128, 31, 128], BF16) for i in range(2)]
                    sgt = [sbuf(ec, f"sgt{i}", [128, 512], F32) for i in range(2)]
                    cwb = sbuf(ec, "cwb", [128, 4 * 31], BF16)
                    rUp, rDg, rsgt = S.grid(2), S.grid(2), S.grid(2)
                    rcwb = S.res()
                    for i in range(2):
                        S.op("pool", lambda i=i: G.memset(Upad[i][:], 0.0), writes=[rUp[i]])
                    S.op("dve", lambda: V.tensor_copy(cwb[:], vecs[:, VOFF["convw"] + l * 124: VOFF["convw"] + (l + 1) * 124]),
                         reads=[rvecs], writes=[rcwb])

                    def uview(b, ti, k):
                        t0, n = TT[ti]
                        if t0 >= NX:
                            return Upad[b][:, UPL + k: UPL + k + NCX]
                        r0 = t0 // 64
                        if rowo:
                            return Upad[b][:, r0 * 79 + k: r0 * 79 + k + 8 * 79].rearrange("p (r c) -> p r c", c=79)[:, :, 0:64]
                        return Upad[b][:, (r0 + k) * 64: (r0 + k) * 64 + 512]

                    def pview(p, ti):
                        t0, n = TT[ti]
                        if t0 < NX and rowo:
                            return P[p][:, :n].rearrange("p (r c) -> p r c", c=64)
                        return P[p][:, :n]

                    def emit_glu(j):
                        b = j % 2
                        ka, wa_, rwa_ = nextw()
                        kg, wg_, rwg_ = nextw()
                        for ti in range(len(conv_tiles)):
                            t0, n = TT[ti]
                            pa = nextp(0, 4)
                            S.group("pe", [(lambda kc=kc, pa=pa: T.matmul(P[pa][:, :n], wa_[:, kc, :], hT[:, kc, t0:t0 + n],
                                                                         start=(kc == 0), stop=(kc == 7))) for kc in range(8)],
                                    reads=[rwa_, rhT[ti]], writes=[rP[pa]])
                            pg = nextp(0, 4)
                            S.group("pe", [(lambda kc=kc, pg=pg: T.matmul(P[pg][:, :n], wg_[:, kc, :], hT[:, kc, t0:t0 + n],
                                                                         start=(kc == 0), stop=(kc == 7))) for kc in range(8)],
                                    reads=[rwg_, rhT[ti]], writes=[rP[pg]])
                            sb_ = ti % 2
                            S.op("act", lambda pg=pg, sb_=sb_: A.activation(sgt[sb_][:, :n], P[pg][:, :n], AF.Sigmoid),
                                 reads=[rP[pg]], writes=[rsgt[sb_]])
                            sv = sgt[sb_][:, :n]
                            if t0 < NX and rowo:
                                sv = sv.rearrange("p (r c) -> p r c", c=64)
                            S.op("dve", lambda pa=pa, sv=sv, ti=ti, b=b: V.tensor_tensor(uview(b, ti, 15), pview(pa, ti), sv, ALU.mult),
                                 reads=[rP[pa], rsgt[sb_]], writes=[rUp[b]])
                        W.after(kg)
                        S.op("dve", lambda b=b, j=j: V.tensor_tensor(
                            Dg[b][:], identb[:].unsqueeze(1).to_broadcast([128, 31, 128]),
                            cwb[:, j * 31:(j + 1) * 31].unsqueeze(2).to_broadcast([128, 31, 128]), ALU.mult),
                            reads=[rmisc, rcwb], writes=[rDg[b]])

                    def emit_conv(j):
                        b = j % 2
                        cb = vv("convb", l * 4 + j)
                        for ti in range(len(conv_tiles)):
                            t0, n = TT[ti]
                            p = nextp(4, 8)
                            S.group("pe", [(lambda k_=k_, p=p, ti=ti, b=b: T.matmul(pview(p, ti), Dg[b][:, k_, :], uview(b, ti, k_),
                                                                                 start=(k_ == 0), stop=(k_ == 30))) for k_ in range(31)],
                                    reads=[rDg[b], rUp[b]], writes=[rP[p]])
                            S.op("act", lambda p=p, j=j, cb=cb: A.activation(ymc[:, j, t0:t0 + n], P[p][:, :n], AF.Identity, bias=cb, scale=1.0),
                                 reads=[rP[p], rvecs], writes=[rym[j][ti]])
                    emit_glu(0)
                    for j in range(4):
                        if j + 1 < 4:
                            emit_glu(j + 1)
                        emit_conv(j)
                    S.barrier()
                with ExitStack() as eln:
                    sq4 = sbuf(eln, "sq4", [128, 4, 512], BF16)
                    mu = sbuf(eln, "mu", [128, 512], F32)
                    msq = sbuf(eln, "msq", [128, 512], F32)
                    rsd = sbuf(eln, "rsd", [128, 512], F32)
                    lt = [sbuf(eln, f"lt{i}", [128, 512], F32) for i in range(2)]
                    rsq4, rmu, rmsq, rrsd = (S.res() for _ in range(4))
                    rlt = S.grid(2)
                    for ti, (t0, n) in enumerate(TO):
                        for j in range(4):
                            S.op("act", lambda j=j: A.activation(sq4[:, j, :n], ymc[:, j, t0:t0 + n], AF.Square),
                                 reads=[rym[j][ti]], writes=[rsq4])
                        p1 = nextp(0, 4)
                        S.group("pe", [(lambda j=j, p1=p1: T.matmul(P[p1][:, :n], onesb[:], ymc[:, j, t0:t0 + n], start=(j == 0), stop=(j == 3)))
                                       for j in range(4)], reads=[rym[j][ti] for j in range(4)] + [rmisc], writes=[rP[p1]])
                        p2 = nextp(0, 4)
                        S.group("pe", [(lambda j=j, p2=p2: T.matmul(P[p2][:, :n], onesb[:], sq4[:, j, :n], start=(j == 0), stop=(j == 3)))
                                       for j in range(4)], reads=[rsq4, rmisc], writes=[rP[p2]])
                        S.op("act", lambda p1=p1: A.mul(mu[:, :n], P[p1][:, :n], 1.0 / 512), reads=[rP[p1]], writes=[rmu])
                        S.op("dve", lambda: V.tensor_tensor(msq[:, :n], mu[:, :n], mu[:, :n], ALU.mult), reads=[rmu], writes=[rmsq])
                        S.op("dve", lambda p2=p2: V.scalar_tensor_tensor(rsd[:, :n], P[p2][:, :n], 1.0 / 512, msq[:, :n], ALU.mult, ALU.subtract),
                             reads=[rP[p2], rmsq], writes=[rrsd])
                        S.op("act", lambda: A.activation(rsd[:, :n], rsd[:, :n], AF.Ln, bias=epsD[:, 1:2], scale=1.0),
                             reads=[rrsd, rmisc], writes=[rrsd])
                        S.op("act", lambda: A.activation(rsd[:, :n], rsd[:, :n], AF.Exp, scale=-0.5), reads=[rrsd], writes=[rrsd])
                        for j in range(4):
                            b = j % 2
                            S.op("dve", lambda j=j, b=b: V.tensor_tensor(lt[b][:, :n], ymc[:, j, t0:t0 + n], mu[:, :n], ALU.subtract),
                                 reads=[rym[j][ti], rmu], writes=[rlt[b]])
                            S.op("dve", lambda b=b: V.tensor_tensor(lt[b][:, :n], lt[b][:, :n], rsd[:, :n], ALU.mult),
                                 reads=[rlt[b], rrsd], writes=[rlt[b]])
                            S.op("act", lambda j=j, b=b: A.activation(ymc[:, j, t0:t0 + n], lt[b][:, :n], AF.Silu,
                                                                      bias=vv("lnb", l * 4 + j), scale=vv("lng", l * 4 + j)),
                                 reads=[rlt[b], rvecs], writes=[rym[j][ti]])
                    S.barrier()
                dump(f"ymc{l}", ymc[:].rearrange("p a b -> p (a b)"), [r for rr in rym[:4] for r in rr])
                if stop == "conv":
                    break
                W2 = WChunks(ecm, "o", [wrow(j * 128) for j in range(4)], "act", nst=2, nbf=4)
                W2.prefetch()
                wo4 = [W2.get(j) for j in range(4)]
                for dj in range(8):
                    for ti, (t0, n) in enumerate(TO):
                        col = 0 if t0 < NX else 1
                        p = nextp(0, 4)
                        S.group("pe", [(lambda j=j, p=p, dj=dj: T.matmul(P[p][:, :n], wo4[j][0][:, dj, :], ymc[:, j, t0:t0 + n],
                                                                        start=(j == 0), stop=(j == 3))) for j in range(4)],
                                reads=[w_[1] for w_ in wo4] + [rym[j][ti] for j in range(4)], writes=[rP[p]])
                        g1 = mod[:, 16 + dj, col:col + 1]
                        S.op("dve", lambda p=p, g1=g1, dj=dj: V.scalar_tensor_tensor(
                            xT[:, dj, t0:t0 + n], P[p][:, :n], g1, xT[:, dj, t0:t0 + n], ALU.mult, ALU.add),
                            reads=[rP[p], rmod, rxT[dj][ti]], writes=[rxT[dj][ti]])
                S.barrier()
            dump(f"xmix{l}", xT[:].rearrange("p a b -> p (a b)"), [r for rr in rxT for r in rr])
            if stop == "mix":
                break

            with ExitStack() as eo:
                comb = sbuf(eo, "comb", [128, 18, NE], F32)
                rcomb = S.res()
                with ExitStack() as er:
                    h2f = sbuf(er, "h2f", [128, 8, 512], F32)
                    sq = sbuf(er, "sq2", [128, 8, 512], BF16)
                    rstd = sbuf(er, "rstd2", [128, 512], F32)
                    tmp = [sbuf(er, f"nt2_{i}", [128, 512], F32) for i in range(2)]
                    rsq, rrstd = S.res(), S.res()
                    rtmp = S.grid(2)
                    rh2 = S.res()
                    pl = 7
                    for ti, (t0, n) in enumerate(TO):
                        if True:
                            col = 0 if t0 < NX else 1
                            for c in range(8):
                                S.op("act", lambda c=c: A.activation(sq[:, c, :n], xT[:, c, t0:t0 + n], AF.Square),
                                     reads=[rxT[c][ti]], writes=[rsq])
                            p = nextp(0, 4)
                            S.group("pe", [(lambda c=c, p=p: T.matmul(P[p][:, :n], onesb[:], sq[:, c, :n], start=(c == 0), stop=(c == 7)))
                                           for c in range(8)], reads=[rsq, rmisc], writes=[rP[p]])
                            S.op("act", lambda p=p: A.activation(rstd[:, :n], P[p][:, :n], AF.Ln, bias=epsD[:, 0:1], scale=1.0),
                                 reads=[rP[p], rmisc], writes=[rrstd])
                            S.op("act", lambda: A.activation(rstd[:, :n], rstd[:, :n], AF.Exp, scale=-0.5), reads=[rrstd], writes=[rrstd])
                            for c in range(8):
                                b = c % 2
                                S.op("dve", lambda c=c, b=b: V.tensor_tensor(tmp[b][:, :n], xT[:, c, t0:t0 + n], rstd[:, :n], ALU.mult),
                                     reads=[rxT[c][ti], rrstd], writes=[rtmp[b]])
                                sc_ap = se[:, 1, c, col:col + 1]
                                sh_ap = mod[:, 24 + c, col:col + 1]
                                S.op("act", lambda c=c, b=b, sc_ap=sc_ap, sh_ap=sh_ap: A.activation(
                                    h2f[:, c, :n], tmp[b][:, :n], AF.Identity, bias=sh_ap, scale=sc_ap),
                                    reads=[rtmp[b], rse, rmod], writes=[rh2])
                                if False:
                                    S.op("pool", lambda c=c: G.tensor_copy(hT[:, c, t0:t0 + n], h2f[:, c, :n]),
                                         reads=[rh2], writes=[rhT[ti]])
                                else:
                                    S.op("act", lambda c=c, b=b, sc_ap=sc_ap, sh_ap=sh_ap: A.activation(
                                        hT[:, c, t0:t0 + n], tmp[b][:, :n], AF.Identity, bias=sh_ap, scale=sc_ap),
                                        reads=[rtmp[b], rse, rmod], writes=[rhT[ti]])
                            fns = []
                            for s_ in range(n // 128):
                                tt = t0 // 128 + s_
                                for kc in range(8):
                                    fns.append(lambda s_=s_, tt=tt, kc=kc: T.matmul(
                                        P[pl][:, tt * NE:(tt + 1) * NE], h2f[:, kc, s_ * 128:(s_ + 1) * 128], rw[:, kc, :],
                                        start=(kc == 0), stop=(kc == 7)))
                            S.group("pe", fns, reads=[rh2, rrw], writes=[rP[pl]])
                    ntl = sum(n for _, n in TO) // 128
                    NG = ntl * 4
                    sg_ = sbuf(er, "r_s", [128, ntl, NE], F32)
                    sbb = sbuf(er, "r_sb", [128, ntl, NE], F32)
                    ps6 = sbuf(er, "r_p6", [128, NG, 6], F32)
                    gs = sbuf(er, "r_gs", [128, NG], F32)
                    gm = sbuf(er, "r_gm", [128, ntl], F32)
                    ing = sbuf(er, "r_ing", [128, NG], F32)
                    sbm = sbuf(er, "r_sbm", [128, ntl, NE], F32)
                    sel = sbuf(er, "r_sel", [128, ntl, NE], F32)
                    m1 = sbuf(er, "r_m1", [128, ntl], F32)
                    rr_ = S.res()
                    R = dict(reads=[rr_], writes=[rr_])
                    S.op("act", lambda: A.activation(sg_[:].rearrange("p a b -> p (a b)"), P[pl][:, :ntl * NE], AF.Sigmoid),
                         reads=[rP[pl]], writes=[rr_])
                    rb_bc = vecs[:, VOFF["rbias"]:VOFF["rbias"] + NE].unsqueeze(1).to_broadcast([128, ntl, NE])
                    S.op("dve", lambda: V.tensor_tensor(sbb[:], sg_[:], rb_bc, ALU.add), reads=[rr_, rvecs], writes=[rr_])
                    g4v = sbb[:].rearrange("p a (g e) -> p (a g) e", e=4)
                    S.op("dve", lambda: V.tensor_tensor(ps6[:, :, 0:2], g4v[:, :, 0:4:2], g4v[:, :, 1:4:2], ALU.add), **R)
                    S.op("dve", lambda: V.tensor_tensor(ps6[:, :, 2:4], g4v[:, :, 0:2], g4v[:, :, 2:4], ALU.add), **R)
                    S.op("dve", lambda: V.tensor_tensor(ps6[:, :, 4:5], g4v[:, :, 0:1], g4v[:, :, 3:4], ALU.add), **R)
                    S.op("dve", lambda: V.tensor_tensor(ps6[:, :, 5:6], g4v[:, :, 1:2], g4v[:, :, 2:3], ALU.add), **R)
                    S.op("dve", lambda: V.tensor_reduce(gs[:], ps6[:], AX.X, ALU.max), **R)
                    S.op("dve", lambda: V.tensor_reduce(gm[:], gs[:].rearrange("p (a g) -> p a g", g=4), AX.X, ALU.max), **R)
                    S.op("dve", lambda: V.tensor_tensor(ing[:].rearrange("p (a g) -> p a g", g=4), gs[:].rearrange("p (a g) -> p a g", g=4),
                                                        gm[:].unsqueeze(2).to_broadcast([128, ntl, 4]), ALU.is_equal), **R)
                    S.op("dve", lambda: V.tensor_scalar(ing[:], ing[:], BIG, -BIG, ALU.mult, ALU.add), **R)
                    S.op("dve", lambda: V.tensor_tensor(sbm[:].rearrange("p a (g e) -> p (a g) e", e=4), g4v,
                                                        ing[:].unsqueeze(2).to_broadcast([128, NG, 4]), ALU.add), **R)
                    S.op("dve", lambda: V.tensor_reduce(m1[:], sbm[:], AX.X, ALU.max), **R)
                    S.op("dve", lambda: V.tensor_tensor(sel[:], sbm[:], m1[:].unsqueeze(2).to_broadcast([128, ntl, NE]), ALU.is_equal), **R)
                    S.op("dve", lambda: V.scalar_tensor_tensor(sbm[:], sel[:], -BIG, sbm[:], ALU.mult, ALU.add), **R)
                    S.op("dve", lambda: V.tensor_reduce(m1[:], sbm[:], AX.X, ALU.max), **R)
                    S.op("dve", lambda: V.tensor_tensor(sbm[:], sbm[:], m1[:].unsqueeze(2).to_broadcast([128, ntl, NE]), ALU.is_ge), **R)
                    S.op("dve", lambda: V.tensor_tensor(sel[:], sel[:], sbm[:], ALU.add), **R)
                    S.op("dve", lambda: V.tensor_tensor(sel[:], sel[:], sg_[:], ALU.mult), **R)
                    S.op("dve", lambda: V.tensor_reduce(m1[:], sel[:], AX.X, ALU.add), **R)
                    S.op("dve", lambda: V.reciprocal(m1[:], m1[:]), **R)
                    S.op("dve", lambda: V.tensor_tensor(comb[:, :ntl, :], sel[:], m1[:].unsqueeze(2).to_broadcast([128, ntl, NE]), ALU.mult),
                         reads=[rr_], writes=[rcomb])
                    S.barrier()
                comb_hi = sbuf(eo, "comb_hi", [128, 18, NE], BF16)
                comb_lo = sbuf(eo, "comb_lo", [128, 18, NE], BF16)
                comb_r = sbuf(eo, "comb_r", [128, 18, NE], F32)
                S.op("dve", lambda: V.tensor_copy(comb_hi[:], comb[:]), reads=[rcomb], writes=[rcomb])
                S.op("dve", lambda: V.tensor_tensor(comb_r[:], comb[:], comb_hi[:], ALU.subtract), reads=[rcomb], writes=[rcomb])
                S.op("dve", lambda: V.tensor_copy(comb_lo[:], comb_r[:]), reads=[rcomb], writes=[rcomb])
                dump(f"comb{l}", comb[:].rearrange("p a b -> p (a b)"), [rcomb])
                dump(f"h2_{l}", hT[:].rearrange("p a b -> p (a b)"), rhT)
                if stop == "router":
                    break
                NU = NE * 4
                NST, NBF = 4, 2
                stg = [sbuf(eo, f"stg{i}", [128, 2048], F32) for i in range(NST)]
                wbf = [sbuf(eo, f"wbf{i}", [128, 3, 2048], BF16) for i in range(NBF)]
                cg = sbuf(eo, "cg", [128, NT], F32)
                actb = [sbuf(eo, f"actb{i}", [128, 2, 512], BF16) for i in range(2)]
                sgm = [sbuf(eo, f"sgm{i}", [128, 512], F32) for i in range(2)]
                t1m = [sbuf(eo, f"t1m{i}", [128, 512], F32) for i in range(2)]
                rstg = S.grid(NST)
                rwbf = S.grid(NBF, 3)
                rcg = S.res()
                ractb, rsgm, rt1m = S.grid(2), S.grid(2), S.grid(2)
                dstg = [S.dsem() for _ in range(NST)]
                pieces = []
                for e in range(NE):
                    for q in range(4):
                        f0 = q * 256
                        pieces.append((wg_d[l, e, :, f0:f0 + 256].rearrange("(kc p) f -> p kc f", p=128), 0))
                        pieces.append((wu_d[l, e, :, f0:f0 + 256].rearrange("(kc p) f -> p kc f", p=128), 1))
                        pieces.append((wd_d[l, e, f0:f0 + 256, :].rearrange("(fc p) d -> p fc d", p=128), 2))
                pl_, pc_ = [0], [0]

                def p_load():
                    if pl_[0] >= len(pieces):
                        return
                    i = pl_[0] % NST
                    src, kind = pieces[pl_[0]]
                    dst = stg[i][:].rearrange("p (a b) -> p a b", a=(8 if kind < 2 else 2))
                    S.dma("sp", dst, src, dstg[i], writes=[rstg[i]])
                    pl_[0] += 1

                def p_cast():
                    if pc_[0] >= len(pieces):
                        return
                    k = pc_[0]
                    i = k % NST
                    u, kind = k // 3, k % 3
                    j = u % NBF
                    S.op("act", lambda i=i, j=j, kind=kind: A.copy(wbf[j][:, kind, :], stg[i][:]),
                         reads=[rstg[i]], writes=[rwbf[j][kind]])
                    pc_[0] += 1
                    p_load()
                for _ in range(NST):
                    p_load()
                for _ in range(3):
                    p_cast()
                def emit_cg(e):
                    for g4 in range((ntl + 3) // 4):
                        tts = list(range(g4 * 4, min(g4 * 4 + 4, ntl)))
                        p = 7
                        fns = []
                        for tt in tts:
                            for hl, cb_ in enumerate((comb_hi, comb_lo)):
                                fns.append(lambda tt=tt, hl=hl, cb_=cb_: T.matmul(
                                    P[p][:, (tt % 4) * 128:(tt % 4 + 1) * 128],
                                    cb_[:, tt, e:e + 1].to_broadcast([128, 128]), identb[:], start=(hl == 0), stop=(hl == 1)))
                        S.group("pe", fns, reads=[rcomb, rmisc], writes=[rP[p]])
                        nn = len(tts) * 128
                        S.op("dve", lambda p=p, g4=g4, nn=nn: V.tensor_copy(cg[:, g4 * 512:g4 * 512 + nn], P[p][:, :nn]),
                             reads=[rP[p]], writes=[rcg])

                def emit_gu(u, ti, ab):
                    e, q = u // 4, u % 4
                    j = u % NBF
                    t0, n = TO[ti]
                    wgt = wbf[j][:, 0, :].rearrange("p (kc f) -> p kc f", kc=8)
                    wut = wbf[j][:, 1, :].rearrange("p (kc f) -> p kc f", kc=8)
                    if q == 0 and ti == 0:
                        emit_cg(e)
                    for fc in range(2):
                        pg = nextp(0, 4)
                        S.group("pe", [(lambda kc=kc, pg=pg, fc=fc: T.matmul(P[pg][:, :n], wgt[:, kc, fc * 128:(fc + 1) * 128],
                                                                           hT[:, kc, t0:t0 + n], start=(kc == 0), stop=(kc == 7)))
                                       for kc in range(8)], reads=[rwbf[j][0], rhT[ti]], writes=[rP[pg]])
                        pu = nextp(0, 4)
                        S.group("pe", [(lambda kc=kc, pu=pu, fc=fc: T.matmul(P[pu][:, :n], wut[:, kc, fc * 128:(fc + 1) * 128],
                                                                           hT[:, kc, t0:t0 + n], start=(kc == 0), stop=(kc == 7)))
                                       for kc in range(8)], reads=[rwbf[j][1], rhT[ti]], writes=[rP[pu]])
                        S.op("act", lambda pg=pg, fc=fc: A.activation(sgm[fc][:, :n], P[pg][:, :n], AF.Silu),
                             reads=[rP[pg]], writes=[rsgm[fc]])
                        S.op("dve", lambda pu=pu, fc=fc: V.tensor_tensor(t1m[fc][:, :n], P[pu][:, :n], sgm[fc][:, :n], ALU.mult),
                             reads=[rP[pu], rsgm[fc]], writes=[rt1m[fc]])
                        S.op("pool", lambda fc=fc, ab=ab: G.tensor_tensor(actb[ab][:, fc, :n], t1m[fc][:, :n], cg[:, t0:t0 + n], ALU.mult),
                             reads=[rt1m[fc], rcg], writes=[ractb[ab]])

                def emit_dn(u, ti, ab):
                    j = u % NBF
                    t0, n = TO[ti]
                    col = 0 if t0 < NX else 1
                    wdt = wbf[j][:, 2, :].rearrange("p (fc d) -> p fc d", fc=2)
                    for dj in range(8):
                        po = nextp(4, 8)
                        S.group("pe", [(lambda fc=fc, po=po, dj=dj, ab=ab: T.matmul(
                            P[po][:, :n], wdt[:, fc, dj * 128:(dj + 1) * 128], actb[ab][:, fc, :n], start=(fc == 0), stop=(fc == 1)))
                            for fc in range(2)], reads=[rwbf[j][2], ractb[ab]], writes=[rP[po]])
                        g2 = mod[:, 40 + dj, col:col + 1]
                        S.op("dve", lambda po=po, g2=g2, dj=dj: V.scalar_tensor_tensor(
                            xT[:, dj, t0:t0 + n], P[po][:, :n], g2, xT[:, dj, t0:t0 + n], ALU.mult, ALU.add),
                            reads=[rP[po], rmod, rxT[dj][ti]], writes=[rxT[dj][ti]])

                items = [(u, ti) for u in range(NU) for ti in range(len(TO))]
                prev = None
                for k_, (u, ti) in enumerate(items):
                    emit_gu(u, ti, k_ % 2)
                    if not MOE_PIPE:
                        emit_dn(u, ti, k_ % 2)
                    elif prev is not None:
                        emit_dn(*prev)
                    if ti == 0:
                        for _ in range(3):
                            p_cast()
                    prev = (u, ti, k_ % 2)
                if MOE_PIPE:
                    emit_dn(*prev)
                S.barrier()
            dump(f"xout{l}", xT[:].rearrange("p a b -> p (a b)"), [r for rr in rxT for r in rr])

        if stop is None:
            with ExitStack() as ef:
                sq = sbuf(ef, "sqf", [128, 8, 512], BF16)
                rstd = sbuf(ef, "rstdf", [128, 512], F32)
                tmp = [sbuf(ef, f"ntf{i}", [128, 512], F32) for i in range(2)]
                yT = sbuf(ef, "yT", [128, 8, 512], F32)
                yo = [sbuf(ef, f"yo{i}", [128, D], F32) for i in range(2)]
                gfs = sbuf(ef, "gfs", [128, 8], F32)
                rsq, rrstd, ryT, rgfs = (S.res() for _ in range(4))
                rtmp, ryo = S.grid(2), S.grid(2)
                dyo = [S.dsem(), S.dsem()]
                S.op("dve", lambda: V.tensor_scalar(gfs[:], vecs[:, VOFF["fing"]:VOFF["fing"] + 8], float(np.sqrt(D)), None, ALU.mult),
                     reads=[rvecs], writes=[rgfs])
                for ti, (t0, n) in enumerate(TT[:4]):
                    for c in range(8):
                        S.op("act", lambda c=c: A.activation(sq[:, c, :], xT[:, c, t0:t0 + n], AF.Square), reads=[rxT[c][ti]], writes=[rsq])
                    p = nextp(0, 4)
                    S.group("pe", [(lambda c=c, p=p: T.matmul(P[p][:], onesb[:], sq[:, c, :], start=(c == 0), stop=(c == 7)))
                                   for c in range(8)], reads=[rsq, rmisc], writes=[rP[p]])
                    S.op("act", lambda p=p: A.activation(rstd[:], P[p][:], AF.Ln, bias=epsD[:, 0:1], scale=1.0),
                         reads=[rP[p], rmisc], writes=[rrstd])
                    S.op("act", lambda: A.activation(rstd[:], rstd[:], AF.Exp, scale=-0.5), reads=[rrstd], writes=[rrstd])
                    for c in range(8):
                        b = c % 2
                        S.op("dve", lambda c=c, b=b: V.tensor_tensor(tmp[b][:], xT[:, c, t0:t0 + n], rstd[:], ALU.mult),
                             reads=[rxT[c][ti], rrstd], writes=[rtmp[b]])
                        S.op("act", lambda c=c, b=b: A.activation(yT[:, c, :], tmp[b][:], AF.Copy, scale=gfs[:, c:c + 1]),
                             reads=[rtmp[b], rgfs], writes=[ryT])
                    for j in range(4):
                        tt = ti * 4 + j
                        b = tt % 2
                        for half in range(2):
                            pp = nextp(4, 8)
                            S.group("pe", [(lambda c=c, pp=pp, j=j: T.transpose(P[pp][:, (c % 4) * 128:(c % 4 + 1) * 128],
                                                                               yT[:, c, j * 128:(j + 1) * 128], ident))
                                           for c in range(half * 4, half * 4 + 4)], reads=[ryT, rcst], writes=[rP[pp]])
                            if half == 0:
                                S.op("act", lambda pp=pp, b=b: A.copy(yo[b][:, 0:512], P[pp][:]), reads=[rP[pp]], writes=[ryo[b]])
                            else:
                                S.op("dve", lambda pp=pp, b=b: V.tensor_copy(yo[b][:, 512:1024], P[pp][:]), reads=[rP[pp]], writes=[ryo[b]])
                        S.dma("sp", y_d[tt * 128:(tt + 1) * 128, :], yo[b][:], dyo[b], reads=[ryo[b]])
                S.barrier()
        S.barrier()
        print("[kernel] semaphore waits:", S.n_wait, "fused into instructions:", S.n_fused)
    return nc


def _chunked(v, n):
    return np.ascontiguousarray(np.asarray(v, np.float32).reshape(n, 128).T)


def make_consts():
    c = np.zeros((128, NCST), np.float32)
    c[:, 0:128] = np.eye(128, dtype=np.float32)
    s = np.arange(128)[:, None]
    t = np.arange(128)[None, :]
    same = (s // CH) == (t // CH)
    c[:, 128:256] = (same & (s <= t)).astype(np.float32)
    c[:, 256:384] = (same & (s >= t)).astype(np.float32)
    m = np.ones(512, np.float32)
    m[::CH] = 0.0
    c[:, 384:896] = m[None, :]
    for i in range(4):
        c[32 * i:32 * i + 32, 896 + i] = 1.0
    return c


def make_in_maps(inputs, cores):
    f = lambda k: np.asarray(inputs[k], np.float32)
    vecs = np.zeros((128, NV), np.float32)

    def put(name, arr):
        arr = np.asarray(arr, np.float32)
        vecs[:, VOFF[name]:VOFF[name] + arr.shape[1]] = arr
    put("n1g", np.concatenate([_chunked(f("norm1_g")[l], 8) for l in range(DEPTH)], 1))
    put("n2g", np.concatenate([_chunked(f("norm2_g")[l], 8) for l in range(DEPTH)], 1))
    put("fing", _chunked(f("final_norm_g"), 8))
    put("convb", np.concatenate([_chunked(f("conv_b")[l], 4) for l in range(DEPTH)], 1))
    put("lng", np.concatenate([_chunked(f("conv_ln_g")[l], 4) for l in range(DEPTH)], 1))
    put("lnb", np.concatenate([_chunked(f("conv_ln_b")[l], 4) for l in range(DEPTH)], 1))
    put("hg", np.stack([f("hgrn_norm_g")[l] for l in range(DEPTH)], 1))
    put("lbf", np.concatenate([_chunked(f("lb_fwd")[l], 4) for l in range(DEPTH)], 1))
    put("lbb", np.concatenate([_chunked(f("lb_bwd")[l], 4) for l in range(DEPTH)], 1))
    cw = f("conv_w")
    cwr = cw.reshape(DEPTH, 31, 4, 128).transpose(3, 0, 2, 1).reshape(128, DEPTH * 4 * 31)
    put("convw", cwr)
    put("rbias", np.broadcast_to(f("router_bias")[None, :], (128, NE)))
    bada = np.stack([_chunked(f("b_ada")[l], 48) for l in range(DEPTH)], 0)
    rwr = np.ascontiguousarray(f("router_w").reshape(8, 128, NE).transpose(1, 0, 2).reshape(128, 8 * NE))
    consts = make_consts()
    shared = {"w_ada": f("w_ada"), "b_ada_r": bada, "vecs": vecs, "consts": consts, "w_in": f("w_in"),
              "w_out": f("w_out"), "router_w_r": rwr, "w_gate": f("w_gate"), "w_up": f("w_up"), "w_down": f("w_down")}
    maps = []
    cc = f("c_ctx")
    for b in cores:
        c2 = np.stack([_chunked(f("c")[b], 8), _chunked(cc, 8)], 2).reshape(128, 16)
        m = dict(shared)
        m["x"] = np.ascontiguousarray(f("x")[b])
        m["ctx"] = np.ascontiguousarray(f("ctx")[b])
        m["c2"] = np.ascontiguousarray(c2)
        maps.append(m)
    return maps


_NC_CACHE = {}


def kernel(**inputs):
    if "nc" not in _NC_CACHE:
        _NC_CACHE["nc"] = build_program()
    nc = _NC_CACHE["nc"]
    maps = make_in_maps(inputs, list(range(8)))
    res = run_bass_kernel_spmd(nc, maps, core_ids=list(range(8)))
    return np.stack([np.asarray(r["y"], np.float32) for r in res.results], 0)
```
